# Optimizing a Trainium2 kernel written in Bass

```python
import jax, jax.numpy as jnp
from jax import lax
import numpy as np

D_MODEL = 1024
BATCH = 4
SEQ = 8192
DEPTH = 4

MEM_LEN = 256

POOL_W = D_MODEL // 4
NSA_W = D_MODEL // 4
SGU_W = D_MODEL // 4
CONV_W = D_MODEL - POOL_W - NSA_W - SGU_W
D_MIX = POOL_W + NSA_W + SGU_W + CONV_W

POOL_GROUPS = 4
POOL_WINDOWS = (2, 4, 8, 16)
POOL_GW = POOL_W // POOL_GROUPS

NSA_HEADS = 4
NSA_DH = NSA_W // NSA_HEADS
CMP_BLOCK = 32
CMP_STRIDE = 16
CMP_HIDDEN = 256
SEL_BLOCK = 64
SEL_TOPK = 16
WINDOW = 512
Q_BLOCK = 128
FORCE_SCORE = 1e4

SGU_GROUPS = 4
SGU_GW = SGU_W // SGU_GROUPS
SGU_CHUNK = 128

CONV_WIDTH = 31

X_HEADS = 4
X_DH = D_MODEL // X_HEADS

D_FF = 2816
N_EXPERTS = 8
TOP_K = 2
D_FF_EXPERT = 3584
MOE_BLOCK = 512
N_DENSE = (DEPTH + 1) // 2
N_MOE = DEPTH // 2

ALPHA = (2 * DEPTH) ** 0.25
BETA = (8 * DEPTH) ** -0.25
LN_EPS = 1e-5
NEG_INF = -1e30

IN_SPLITS = (POOL_W, NSA_W, 6 * NSA_DH, 3 * NSA_HEADS, 2 * SGU_W, 2 * CONV_W)
D_IN = sum(IN_SPLITS)
IN_OFFSETS = tuple(sum(IN_SPLITS[:i + 1]) for i in range(len(IN_SPLITS) - 1))

kernel_name = "hybrid_pool_nsa_sgu_conv_moe_trunk"


def layer_norm(x, g, b):
    xf = x.astype(jnp.float32)
    mu = jnp.mean(xf, axis=-1, keepdims=True)
    var = jnp.mean(jnp.square(xf - mu), axis=-1, keepdims=True)
    y = (xf - mu) * lax.rsqrt(var + LN_EPS)
    return (y * g.astype(jnp.float32) + b.astype(jnp.float32)).astype(x.dtype)


def masked_softmax(s, mask):
    p = jax.nn.softmax(jnp.where(mask, s, NEG_INF), axis=-1)
    return jnp.where(mask, p, 0.0)


def pool_mixer(a, w, scale):
    b, t, _ = a.shape
    af = a.astype(jnp.float32).reshape(b, t, POOL_GROUPS, POOL_GW)
    cs = jnp.cumsum(af, axis=1)
    pos = jnp.arange(t)
    outs = []
    for g, win in enumerate(POOL_WINDOWS):
        c = cs[:, :, g]
        prev = jnp.pad(c, ((0, 0), (win, 0), (0, 0)))[:, :t]
        cnt = jnp.minimum(pos + 1, win).astype(jnp.float32)[None, :, None]
        outs.append((c - prev) / cnt - af[:, :, g])
    d = jnp.stack(outs, axis=2).astype(a.dtype)
    y = jnp.einsum('btgc,gcd->btgd', d, w).reshape(b, t, POOL_W)
    return y * scale


def compress_kv(raw, pe, w1, w2):
    b, t, dk = raw.shape
    n_chunk = t // CMP_STRIDE
    per = CMP_BLOCK // CMP_STRIDE
    nc = n_chunk - per + 1
    c = raw.reshape(b, n_chunk, CMP_STRIDE, dk)
    blocks = jnp.concatenate([c[:, p:p + nc] for p in range(per)], axis=2) + pe
    h = jax.nn.gelu(blocks.reshape(b, nc, CMP_BLOCK * dk) @ w1)
    return h @ w2


def nsa_mixer(q, kv, gate_logits, pe_k, wk1, wk2, pe_v, wv1, wv2):
    b, t, _ = q.shape
    qh = q.reshape(b, t, NSA_HEADS, NSA_DH)
    k_cmp, v_cmp, k_sel, v_sel, k_win, v_win = jnp.split(kv, 6, axis=-1)
    kc = compress_kv(k_cmp, pe_k, wk1, wk2)
    vc = compress_kv(v_cmp, pe_v, wv1, wv2)
    nc = kc.shape[1]
    ns = t // SEL_BLOCK
    n_sel = min(SEL_TOPK, ns)
    ks_blocks = k_sel.reshape(b, ns, SEL_BLOCK, NSA_DH)
    vs_blocks = v_sel.reshape(b, ns, SEL_BLOCK, NSA_DH)
    kw_pad = jnp.pad(k_win, ((0, 0), (WINDOW, 0), (0, 0)))
    vw_pad = jnp.pad(v_win, ((0, 0), (WINDOW, 0), (0, 0)))
    gates = jax.nn.sigmoid(gate_logits.astype(jnp.float32)).reshape(b, t, NSA_HEADS, 3)
    scale = NSA_DH ** -0.5
    cmp_end = jnp.arange(nc) * CMP_STRIDE + CMP_BLOCK - 1
    sel_ids = jnp.arange(ns)
    ratio = SEL_BLOCK // CMP_STRIDE
    lead = CMP_BLOCK // CMP_STRIDE - 1
    tail = ratio * ns - nc

    def query_block(i):
        q0 = i * Q_BLOCK
        tq = q0 + jnp.arange(Q_BLOCK)
        qb = lax.dynamic_slice_in_dim(qh, q0, Q_BLOCK, axis=1)
        s = jnp.einsum('bqhd,bnd->bhqn', qb, kc).astype(jnp.float32) * scale
        p_c = masked_softmax(s, cmp_end[None, :] <= tq[:, None])
        o_c = jnp.einsum('bhqn,bnd->bqhd', p_c.astype(vc.dtype), vc)
        imp = jnp.pad(p_c.sum(axis=1), ((0, 0), (0, 0), (lead, tail)))
        imp_sel = imp[..., :ratio * ns].reshape(b, Q_BLOCK, ns, ratio).sum(-1)
        for p in range(lead):
            imp_sel = imp_sel + imp[..., ratio + p:ratio + p + ratio * ns:ratio]
        cur = tq // SEL_BLOCK
        valid = sel_ids[None, :] <= cur[:, None]
        forced = ((sel_ids[None, :] == 0) | (sel_ids[None, :] == cur[:, None])
                  | (sel_ids[None, :] == cur[:, None] - 1))
        score = jnp.where(valid, imp_sel + FORCE_SCORE * forced, -1.0)
        top_s, idx = lax.top_k(score, n_sel)
        ks = jax.vmap(lambda kb, ib: kb[ib])(ks_blocks, idx)
        vs = jax.vmap(lambda vb, ib: vb[ib])(vs_blocks, idx)
        kpos = idx[..., None] * SEL_BLOCK + jnp.arange(SEL_BLOCK)
        m_s = (top_s >= 0.0)[..., None] & (kpos <= tq[None, :, None, None])
        s = jnp.einsum('bqhd,bqnkd->bhqnk', qb, ks).astype(jnp.float32) * scale
        s = s.reshape(b, NSA_HEADS, Q_BLOCK, n_sel * SEL_BLOCK)
        m_s = m_s.reshape(b, 1, Q_BLOCK, n_sel * SEL_BLOCK)
        p_s = masked_softmax(s, m_s).reshape(b, NSA_HEADS, Q_BLOCK, n_sel, SEL_BLOCK)
        o_s = jnp.einsum('bhqnk,bqnkd->bqhd', p_s.astype(vs.dtype), vs)
        kw = lax.dynamic_slice_in_dim(kw_pad, q0, WINDOW + Q_BLOCK, axis=1)
        vw = lax.dynamic_slice_in_dim(vw_pad, q0, WINDOW + Q_BLOCK, axis=1)
        spos = q0 - WINDOW + jnp.arange(WINDOW + Q_BLOCK)
        m_w = ((spos[None, :] >= 0) & (spos[None, :] <= tq[:, None])
               & (spos[None, :] > tq[:, None] - WINDOW))
        s = jnp.einsum('bqhd,bkd->bhqk', qb, kw).astype(jnp.float32) * scale
        p_w = masked_softmax(s, m_w)
        o_w = jnp.einsum('bhqk,bkd->bqhd', p_w.astype(vw.dtype), vw)
        g = lax.dynamic_slice_in_dim(gates, q0, Q_BLOCK, axis=1)
        o = g[..., 0:1] * o_c + g[..., 1:2] * o_s + g[..., 2:3] * o_w
        return o.astype(qh.dtype)

    out = lax.map(query_block, jnp.arange(t // Q_BLOCK))
    return out.transpose(1, 0, 2, 3, 4).reshape(b, t, NSA_W)


def sgu_mixer(uv, ln_g, ln_b, w_s, b_s):
    b, t, _ = uv.shape
    u, v = jnp.split(jax.nn.gelu(uv), 2, axis=-1)
    v = layer_norm(v, ln_g, ln_b)
    v = v.reshape(b, t // SGU_CHUNK, SGU_CHUNK, SGU_GROUPS, SGU_GW)
    causal = jnp.tril(jnp.ones((SGU_CHUNK, SGU_CHUNK), dtype=bool))
    w = jnp.where(causal, w_s, 0.0)
    mix = jnp.einsum('gij,bnjgc->bnigc', w, v) + b_s.T[None, None, :, :, None]
    return u * mix.reshape(b, t, SGU_W)


def conv_mixer(ag, w_dw, b_dw, ln_g, ln_b, w_pw):
    a, g = jnp.split(ag, 2, axis=-1)
    h = a * jax.nn.sigmoid(g)
    h = lax.conv_general_dilated(
        h, w_dw[:, None, :], window_strides=(1,), padding=[(CONV_WIDTH - 1, 0)],
        dimension_numbers=("NWC", "WIO", "NWC"), feature_group_count=CONV_W) + b_dw
    h = jax.nn.silu(layer_norm(h, ln_g, ln_b))
    return h @ w_pw


def hybrid_mixer(h, w_in, pool_w, pool_scale, pe_k, wk1, wk2, pe_v, wv1, wv2,
                 sgu_ln_g, sgu_ln_b, sgu_w, sgu_b, conv_w, conv_b, conv_ln_g,
                 conv_ln_b, conv_pw, w_out):
    z = h @ w_in
    a, q, kv, gl, uv, ag = jnp.split(z, IN_OFFSETS, axis=-1)
    y = jnp.concatenate([
        pool_mixer(a, pool_w, pool_scale),
        nsa_mixer(q, kv, gl, pe_k, wk1, wk2, pe_v, wv1, wv2),
        sgu_mixer(uv, sgu_ln_g, sgu_ln_b, sgu_w, sgu_b),
        conv_mixer(ag, conv_w, conv_b, conv_ln_g, conv_ln_b, conv_pw),
    ], axis=-1)
    return y @ w_out


def memory_cross_attention(h, mem, wq, wkv, wo):
    b, t, d = h.shape
    q = (h @ wq).reshape(b, t, X_HEADS, X_DH)
    k, v = jnp.split(mem @ wkv, 2, axis=-1)
    k = k.reshape(b, -1, X_HEADS, X_DH)
    v = v.reshape(b, -1, X_HEADS, X_DH)
    s = jnp.einsum('bthd,bmhd->bhtm', q, k).astype(jnp.float32) * (X_DH ** -0.5)
    p = jax.nn.softmax(s, axis=-1)
    o = jnp.einsum('bhtm,bmhd->bthd', p.astype(v.dtype), v).reshape(b, t, d)
    return o @ wo


def swiglu(h, w13, w2):
    g, u = jnp.split(h @ w13, 2, axis=-1)
    return (jax.nn.silu(g) * u) @ w2


def moe_swiglu(h, router_w, w13, w2):
    n, d = h.shape
    logits = (h @ router_w).astype(jnp.float32)
    top_v, top_e = lax.top_k(logits, TOP_K)
    gate = jax.nn.softmax(top_v, axis=-1)
    flat_e = top_e.reshape(-1)
    flat_tok = jnp.arange(n * TOP_K, dtype=jnp.int32) // TOP_K
    order = jnp.argsort(flat_e)
    se = flat_e[order]
    counts = jnp.bincount(flat_e, length=N_EXPERTS)
    start = jnp.cumsum(counts) - counts
    pcounts = (counts + MOE_BLOCK - 1) // MOE_BLOCK * MOE_BLOCK
    pend = jnp.cumsum(pcounts)
    pstart = pend - pcounts
    dest = pstart[se] + jnp.arange(n * TOP_K) - start[se]
    m_pad = -(-(n * TOP_K) // MOE_BLOCK) * MOE_BLOCK + N_EXPERTS * MOE_BLOCK
    tok = jnp.full((m_pad,), n, jnp.int32).at[dest].set(flat_tok[order])
    gw = jnp.zeros((m_pad,), jnp.float32).at[dest].set(gate.reshape(-1)[order])
    n_blk = m_pad // MOE_BLOCK
    blk_e = jnp.minimum(jnp.searchsorted(pend, jnp.arange(n_blk) * MOE_BLOCK, side='right'),
                        N_EXPERTS - 1)
    h_pad = jnp.concatenate([h, jnp.zeros((1, d), h.dtype)], axis=0)

    def expert_block(args):
        tb, e = args
        g, u = jnp.split(h_pad[tb] @ w13[e], 2, axis=-1)
        return (jax.nn.silu(g) * u) @ w2[e]

    y = lax.map(expert_block, (tok.reshape(n_blk, MOE_BLOCK), blk_e))
    y = y.reshape(m_pad, d) * gw[:, None].astype(h.dtype)
    return jax.ops.segment_sum(y, tok, num_segments=n + 1)[:n]


def setup_inputs(seed: int = 0) -> dict:
    key = jax.random.key(seed)
    ks = iter(jax.random.split(key, 48))
    f32 = jnp.float32
    L, ND, NM, dk = DEPTH, N_DENSE, N_MOE, NSA_DH

    def nrm(shape, scale):
        return jax.random.normal(next(ks), shape, f32) * scale

    def gain(shape):
        return 1.0 + nrm(shape, 0.05)

    def bias(shape):
        return nrm(shape, 0.02)

    return {
        "x": nrm((BATCH, SEQ, D_MODEL), 1.0),
        "mem": nrm((BATCH, MEM_LEN, D_MODEL), 1.0),
        "w_in": nrm((L, D_MODEL, D_IN), D_MODEL ** -0.5),
        "pool_w": nrm((L, POOL_GROUPS, POOL_GW, POOL_GW), POOL_GW ** -0.5),
        "pool_scale": 1.0 + nrm((L, POOL_W), 0.1),
        "cmp_pe_k": nrm((L, CMP_BLOCK, dk), 0.1),
        "cmp_k_w1": nrm((L, CMP_BLOCK * dk, CMP_HIDDEN), (CMP_BLOCK * dk) ** -0.5),
        "cmp_k_w2": nrm((L, CMP_HIDDEN, dk), CMP_HIDDEN ** -0.5),
        "cmp_pe_v": nrm((L, CMP_BLOCK, dk), 0.1),
        "cmp_v_w1": nrm((L, CMP_BLOCK * dk, CMP_HIDDEN), (CMP_BLOCK * dk) ** -0.5),
        "cmp_v_w2": nrm((L, CMP_HIDDEN, dk), CMP_HIDDEN ** -0.5),
        "sgu_ln_g": gain((L, SGU_W)),
        "sgu_ln_b": bias((L, SGU_W)),
        "sgu_w": nrm((L, SGU_GROUPS, SGU_CHUNK, SGU_CHUNK), SGU_CHUNK ** -0.5),
        "sgu_b": 1.0 + nrm((L, SGU_GROUPS, SGU_CHUNK), 0.1),
        "conv_w": nrm((L, CONV_WIDTH, CONV_W), CONV_WIDTH ** -0.5),
        "conv_b": bias((L, CONV_W)),
        "conv_ln_g": gain((L, CONV_W)),
        "conv_ln_b": bias((L, CONV_W)),
        "conv_pw": nrm((L, CONV_W, CONV_W), CONV_W ** -0.5),
        "w_out": nrm((L, D_MIX, D_MODEL), BETA * D_MIX ** -0.5),
        "ln1_g": gain((L, D_MODEL)),
        "ln1_b": bias((L, D_MODEL)),
        "xq_w": nrm((L, D_MODEL, D_MODEL), D_MODEL ** -0.5),
        "xkv_w": nrm((L, D_MODEL, 2 * D_MODEL), D_MODEL ** -0.5),
        "xo_w": nrm((L, D_MODEL, D_MODEL), BETA * D_MODEL ** -0.5),
        "ln2_g": gain((L, D_MODEL)),
        "ln2_b": bias((L, D_MODEL)),
        "ffn_w13": nrm((ND, D_MODEL, 2 * D_FF), D_MODEL ** -0.5),
        "ffn_w2": nrm((ND, D_FF, D_MODEL), BETA * D_FF ** -0.5),
        "router_w": nrm((NM, D_MODEL, N_EXPERTS), D_MODEL ** -0.5),
        "exp_w13": nrm((NM, N_EXPERTS, D_MODEL, 2 * D_FF_EXPERT), D_MODEL ** -0.5),
        "exp_w2": nrm((NM, N_EXPERTS, D_FF_EXPERT, D_MODEL), BETA * D_FF_EXPERT ** -0.5),
        "ln3_g": gain((L, D_MODEL)),
        "ln3_b": bias((L, D_MODEL)),
    }


def reference(x, mem, w_in, pool_w, pool_scale, cmp_pe_k, cmp_k_w1, cmp_k_w2,
              cmp_pe_v, cmp_v_w1, cmp_v_w2, sgu_ln_g, sgu_ln_b, sgu_w, sgu_b,
              conv_w, conv_b, conv_ln_g, conv_ln_b, conv_pw, w_out, ln1_g, ln1_b,
              xq_w, xkv_w, xo_w, ln2_g, ln2_b, ffn_w13, ffn_w2, router_w,
              exp_w13, exp_w2, ln3_g, ln3_b):
    b, t, d = x.shape
    for l in range(DEPTH):
        mix = hybrid_mixer(x, w_in[l], pool_w[l], pool_scale[l], cmp_pe_k[l],
                           cmp_k_w1[l], cmp_k_w2[l], cmp_pe_v[l], cmp_v_w1[l],
                           cmp_v_w2[l], sgu_ln_g[l], sgu_ln_b[l], sgu_w[l], sgu_b[l],
                           conv_w[l], conv_b[l], conv_ln_g[l], conv_ln_b[l],
                           conv_pw[l], w_out[l])
        x = layer_norm(ALPHA * x + mix, ln1_g[l], ln1_b[l])
        xa = memory_cross_attention(x, mem, xq_w[l], xkv_w[l], xo_w[l])
        x = layer_norm(ALPHA * x + xa, ln2_g[l], ln2_b[l])
        if l % 2 == 0:
            f = swiglu(x, ffn_w13[l // 2], ffn_w2[l // 2])
        else:
            f = moe_swiglu(x.reshape(b * t, d), router_w[l // 2], exp_w13[l // 2],
                           exp_w2[l // 2]).reshape(b, t, d)
        x = layer_norm(ALPHA * x + f, ln3_g[l], ln3_b[l])
    return x
```

```python
import numpy as np
from contextlib import ExitStack
import concourse.bass as bass
import concourse.mybir as mybir
from concourse.bass_utils import run_bass_kernel_spmd

F32 = mybir.dt.float32
BF16 = mybir.dt.bfloat16
ALU = mybir.AluOpType
AF = mybir.ActivationFunctionType
AX = mybir.AxisListType

D = 1024
NCH = 8
DEPTH = 4
MEM = 256
D_IN = 1932
D_FF = 2816
D_FFE = 3584
NEXP = 8
ALPHA = (2 * DEPTH) ** 0.25
EPS = 1e-5
NEGM = -30000.0
TT = 512
GELU_NATIVE = True

ENGS = ("pe", "act", "dve", "pool", "sp")
N_DMA_SEMS = 40


class Em:
    def __init__(self, nc, stack):
        self.nc = nc
        self.prog = {e: [] for e in ENGS}
        self.cnt = {e: 0 for e in ENGS}
        self.waited = {e: {} for e in ENGS}
        self.res = {}
        self.sem = {e: stack.enter_context(nc.semaphore("s_" + e)) for e in ENGS}
        self.dsem = [stack.enter_context(nc.semaphore("d%d" % i)) for i in range(N_DMA_SEMS)]
        self.ndma = 0
        self.nwaits = 0
        self.dlast = {}

    def _st(self, key):
        st = self.res.get(key)
        if st is None:
            st = {"w": None, "r": {}}
            self.res[key] = st
        return st

    def _need(self, eng, tok, same_ok):
        if tok is None:
            return
        if tok[0] == "e":
            _, e2, k = tok
            if e2 == eng and same_ok:
                return
            if self.waited[eng].get(e2, 0) >= k:
                return
            self.waited[eng][e2] = k
            self.prog[eng].append(("wait", self.sem[e2], k))
            self.nwaits += 1
        else:
            _, si, target = tok
            if self.waited[eng].get(("d", si), 0) >= target:
                return
            self.waited[eng][("d", si)] = target
            self.prog[eng].append(("wait", self.dsem[si], target))
            self.nwaits += 1

    def _deps(self, eng, reads, writes):
        for r in reads:
            self._need(eng, self._st(r)["w"], same_ok=(eng == "pe"))
        for w in writes:
            st = self._st(w)
            self._need(eng, st["w"], same_ok=True)
            for tok in st["r"].values():
                self._need(eng, tok, same_ok=True)

    def _commit(self, tok, rkey, reads, writes):
        for r in reads:
            self._st(r)["r"][rkey] = tok
        for w in writes:
            st = self._st(w)
            st["w"] = tok
            st["r"] = {}

    @staticmethod
    def _excl(reads, writes):
        ex = [k for k in reads if (isinstance(k, tuple) and k[0] in ("pb", "acc")) or k == "imp"]
        if not ex:
            return reads, writes
        return [k for k in reads if k not in ex], list(writes) + [k for k in ex if k not in writes]

    def op(self, eng, fn, reads=(), writes=()):
        reads, writes = self._excl(list(reads), list(writes))
        self._deps(eng, reads, writes)
        self.cnt[eng] += 1
        tok = ("e", eng, self.cnt[eng])
        self.prog[eng].append(("op", fn, self.sem[eng]))
        self._commit(tok, eng, reads, writes)
        return tok

    def dma(self, q, out, in_, reads=(), writes=(), **kw):
        k = self.ndma
        self.ndma += 1
        si = k % N_DMA_SEMS
        target = 16 * (k // N_DMA_SEMS + 1)
        if target > 16:
            self._need(q, ("d", si, target - 16), same_ok=False)
        self._deps(q, reads, writes)
        tok = ("d", si, target)
        self.dlast[si] = tok
        self.prog[q].append(("dma", out, in_, self.dsem[si], kw))
        self._commit(tok, ("dq", si), reads, writes)
        return tok

    def barrier(self, engs=ENGS):
        for e in engs:
            for e2 in ENGS:
                if e2 != e and self.cnt[e2] > 0:
                    self._need(e, ("e", e2, self.cnt[e2]), same_ok=False)
            for tok in self.dlast.values():
                self._need(e, tok, same_ok=False)

    def replay(self):
        nc = self.nc
        engmap = {"pe": "tensor", "act": "scalar", "dve": "vector", "pool": "gpsimd", "sp": "sync"}
        with nc.Block() as block:
            for e in ENGS:
                prog = self.prog[e]

                def body(engobj, prog=prog):
                    for it in prog:
                        if it[0] == "wait":
                            engobj.wait_ge(it[1], it[2])
                        elif it[0] == "op":
                            it[1](engobj).then_inc(it[2], 1)
                        else:
                            engobj.dma_start(out=it[1], in_=it[2], **it[4]).then_inc(it[3], 16)

                getattr(block, engmap[e])(body)


PCOL = {}
_off = 0
for _n, _w in [("ln1g", 8), ("ln1b", 8), ("ln2g", 8), ("ln2b", 8), ("ln3g", 8), ("ln3b", 8),
               ("pool_scale", 2), ("conv_w", 62), ("conv_b", 2), ("conv_lng", 2), ("conv_lnb", 2),
               ("peT", 64)]:
    PCOL[_n] = _off
    _off += _w
NP = _off
CF = {"ident": 0, "triu": 128, "invw": 256, "invc": 258, "VW": 290, "CW": 546}
CF32_W = 802


class Builder:
    def __init__(self, T, depth, dbg=None):
        self.T = T
        self.depth = depth
        self.dbg = dbg or {}
        self.NT = T // TT
        self.NQ = T // 128
        self.nc = bass.Bass("TRN2", target_bir_lowering=False)
        self.uid = 0
        self.dumped = set()

    def sb(self, st, name, shape, dt):
        self.uid += 1
        return st.enter_context(self.nc.sbuf_tensor("%s_%d" % (name, self.uid), shape, dt))

    def din(self, name, shape, dt=F32):
        return self.nc.dram_tensor(name, list(shape), dt, kind="ExternalInput").ap()

    def dscr(self, name, shape, dt):
        return self.nc.dram_tensor(name, list(shape), dt, kind="Internal").ap()

    def mm(self, ps_ap, pskey, pairs):
        n = len(pairs)
        for i, (l, r, rd) in enumerate(pairs):
            self.em.op("pe", lambda e, l=l, r=r, i=i: e.matmul(ps_ap, lhsT=l, rhs=r, start=(i == 0), stop=(i == n - 1)),
                       reads=rd, writes=[pskey])

    def dump(self, name, ap, shape, dt, keys):
        if "dumps" not in self.dbg or name in self.dumped:
            return
        self.dumped.add(name)
        d = self.nc.dram_tensor("dbg_" + name, list(shape), dt, kind="ExternalOutput").ap()
        self.em.dma("sp", d, ap, reads=keys, writes=["dbg_" + name])

    def pbcopy(self, st, bank, keys):
        t = self.sb(st, "pbc", [128, 512], F32)
        self.em.op("dve", lambda e: e.memset(t[:], 0.0), writes=["pbc%d" % bank])
        n = 512 if bank == 3 else 260
        self.em.op("act", lambda e: e.copy(out=t[:, 0:n], in_=self.pb[bank][:, 0:n]), reads=keys, writes=["pbc%d" % bank])
        return t[:]

    def bank(self):
        b = self.rot[self.roti % len(self.rot)]
        self.roti += 1
        return b

    def ln_fm(self, r, rkey, nch, gcol, bcol, N, outs, func=AF.Identity):
        em = self.em
        W = self.lnw
        ones = self.ones_d[nch * 128]
        for c in range(nch):
            em.op("act", lambda e, c=c: e.activation(out=W["sq"][:, c, :N], in_=r[:, c, :N], func=AF.Square),
                  reads=[(rkey, c)], writes=[("ln_sq", c)])
            em.op("pool", lambda e, c=c: e.tensor_copy(out=W["rb"][:, c, :N], in_=r[:, c, :N]),
                  reads=[(rkey, c)], writes=[("ln_rb", c)])
        b0, b1 = self.bank(), self.bank()
        self.mm(self.pb[b0][:, :N], ("pb", b0), [(ones[:], W["rb"][:, c, :N], ["ones", ("ln_rb", c)]) for c in range(nch)])
        self.mm(self.pb[b1][:, :N], ("pb", b1), [(ones[:], W["sq"][:, c, :N], ["ones", ("ln_sq", c)]) for c in range(nch)])
        mean, rstd, tmp = W["mean"], W["rstd"], W["tmp"]
        em.op("dve", lambda e: e.tensor_copy(out=mean[:, :N], in_=self.pb[b0][:, :N]), reads=[("pb", b0)], writes=["ln_mean"])
        em.op("dve", lambda e: e.tensor_tensor(out=tmp[:, :N], in0=mean[:, :N], in1=mean[:, :N], op=ALU.mult), reads=["ln_mean"], writes=[("ln_tmp", 0)])
        em.op("dve", lambda e: e.tensor_tensor(out=rstd[:, :N], in0=self.pb[b1][:, :N], in1=tmp[:, :N], op=ALU.subtract),
              reads=[("pb", b1), ("ln_tmp", 0)], writes=["ln_rstd"])
        em.op("act", lambda e: e.activation(out=rstd[:, :N], in_=rstd[:, :N], func=AF.Sqrt, bias=self.eps_sb[:], scale=1.0),
              reads=["ln_rstd", "consts"], writes=["ln_rstd"])
        em.op("dve", lambda e: e.reciprocal(out=rstd[:, :N], in_=rstd[:, :N]), reads=["ln_rstd"], writes=["ln_rstd"])
        P = self.par
        for c in range(nch):
            tmpc = W["tmp"] if c % 2 == 0 else W["tmp2"]
            tk = ("ln_tmp", c % 2)
            em.op("dve", lambda e, c=c, tmpc=tmpc: e.tensor_tensor(out=tmpc[:, :N], in0=r[:, c, :N], in1=mean[:, :N], op=ALU.subtract),
                  reads=[(rkey, c), "ln_mean"], writes=[tk])
            em.op("dve", lambda e, c=c, tmpc=tmpc: e.tensor_tensor(out=tmpc[:, :N], in0=tmpc[:, :N], in1=rstd[:, :N], op=ALU.mult),
                  reads=[tk, "ln_rstd"], writes=[tk])
            if func != AF.Identity:
                em.op("act", lambda e, c=c, tmpc=tmpc: e.activation(out=tmpc[:, :N], in_=tmpc[:, :N], func=AF.Identity,
                                                                   scale=P[:, gcol + c:gcol + c + 1], bias=P[:, bcol + c:bcol + c + 1]),
                      reads=[tk, "par"], writes=[tk])
                for (ot, okey) in outs:
                    em.op("act", lambda e, c=c, ot=ot, tmpc=tmpc: e.activation(out=ot[:, c, :N], in_=tmpc[:, :N], func=func), reads=[tk], writes=[(okey, c)])
                continue
            for (ot, okey) in outs:
                em.op("act", lambda e, c=c, ot=ot, tmpc=tmpc: e.activation(out=ot[:, c, :N], in_=tmpc[:, :N], func=func,
                                                                          scale=P[:, gcol + c:gcol + c + 1], bias=P[:, bcol + c:bcol + c + 1]),
                      reads=[tk, "par"], writes=[(okey, c)])

    def build(self):
        nc = self.nc
        T, L = self.T, self.depth
        I = {}
        I["xT"] = self.din("xT", [D, T])
        I["memT"] = self.din("memT", [D, MEM])
        I["w_in"] = self.din("w_in", [DEPTH, D, D_IN])
        I["w_out"] = self.din("w_out", [DEPTH, D, D])
        I["xq_w"] = self.din("xq_w", [DEPTH, D, D])
        I["xkv_w"] = self.din("xkv_w", [DEPTH, D, 2 * D])
        I["xo_w"] = self.din("xo_w", [DEPTH, D, D])
        I["ffn_w13"] = self.din("ffn_w13", [2, D, 2 * D_FF])
        I["ffn_w2"] = self.din("ffn_w2", [2, D_FF, D])
        I["router_w"] = self.din("router_w", [2, D, NEXP])
        I["exp_w13"] = self.din("exp_w13", [2, NEXP, D, 2 * D_FFE])
        I["exp_w2"] = self.din("exp_w2", [2, NEXP, D_FFE, D])
        I["params"] = self.din("params", [DEPTH, 128, NP])
        for nm in ("cmp_k", "cmp_v"):
            I[nm + "_w1"] = self.din(nm + "_w1", [DEPTH, 2048, 256])
            I[nm + "_w2"] = self.din(nm + "_w2", [DEPTH, 256, 64])
        I["conv_pw"] = self.din("conv_pw", [DEPTH, 256, 256])
        I["pool_w"] = self.din("pool_w", [DEPTH, 4, 64, 64])
        I["sgu_wT"] = self.din("sgu_wT", [DEPTH, 4, 128, 128])
        I["sgu_b"] = self.din("sgu_b", [DEPTH, 4, 128])
        I["sgu_ln_g"] = self.din("sgu_ln_g", [DEPTH, 256])
        I["sgu_ln_b"] = self.din("sgu_ln_b", [DEPTH, 256])
        I["cf32"] = self.din("cf32", [128, CF32_W])
        NKT = T // 128
        self.CB = {"Gm": 0, "Mimp": 17 * 128, "Caus": 21 * 128, "Wlow": 22 * 128, "E": 23 * 128}
        self.CBW = (23 + NKT) * 128
        I["cb"] = self.din("cb", [128, self.CBW])
        self.I = I
        self.out = nc.dram_tensor("outT", [D, T], F32, kind="ExternalOutput").ap()
        self.XA = self.dscr("XA", [D, T], F32)
        self.XB = self.dscr("XB", [D, T], F32)
        if "dump_y" in self.dbg:
            self.Y = nc.dram_tensor("Y", [128, NCH, T], BF16, kind="ExternalOutput").ap()
        else:
            self.Y = self.dscr("Y", [128, NCH, T], BF16)
        self.QT = self.dscr("QT", [64, 4, T], BF16)
        if "y_in" in self.dbg:
            self.Yin = self.din("y_in", [128, NCH, T], BF16)

        with ExitStack() as st:
            self.em = em = Em(nc, st)
            self.pb = [st.enter_context(nc.psum_tensor("pb%d" % i, [128, 512], F32)) for i in range(8)]
            self.rot = list(range(8))
            self.roti = 0
            self.ones_d = {1024: self.sb(st, "ones1024", [128, 128], BF16), 256: self.sb(st, "ones256", [128, 128], BF16)}
            self.ones1 = self.sb(st, "ones1", [128, 128], BF16)
            self.eps_sb = self.sb(st, "eps", [128, 1], F32)
            self.cf = self.sb(st, "cf", [128, CF32_W], F32)
            em.dma("sp", self.cf[:], I["cf32"], writes=["ident"])
            self.ident = self.cf[:, 0:128]
            self.identb = self.sb(st, "identb", [128, 128], BF16)
            em.op("act", lambda e: e.copy(out=self.identb[:], in_=self.cf[:, 0:128]), reads=["ident"], writes=["identb"])
            em.op("dve", lambda e: e.memset(self.ones_d[1024][:], 1.0 / 1024), writes=["ones"])
            em.op("dve", lambda e: e.memset(self.ones_d[256][:], 1.0 / 256), writes=["ones"])
            em.op("dve", lambda e: e.memset(self.ones1[:], 1.0), writes=["ones"])
            em.op("dve", lambda e: e.memset(self.eps_sb[:], EPS), writes=["consts"])
            self.par = self.sb(st, "par", [128, NP], F32)
            for l in range(L):
                em.dma("sp", self.par[:], I["params"][l], writes=["par"])
                xcur = I["xT"] if l == 0 else self.XB
                xcur_key = "xT" if l == 0 else "XB"
                last = (l == L - 1)
                if "y_in" not in self.dbg:
                    with ExitStack() as stn:
                        NQ = self.NQ
                        N = {"KselT": self.sb(stn, "KselT", [64, T], BF16), "KwinT": self.sb(stn, "KwinT", [64, T], BF16),
                             "Vsel": self.sb(stn, "Vsel", [128, NQ, 65], BF16), "Vwin": self.sb(stn, "Vwin", [128, NQ, 65], BF16),
                             "gates": self.sb(stn, "gates", [128, NQ, 12], F32), "kcT": self.sb(stn, "kcT", [64, 512], BF16),
                             "vcT": self.sb(stn, "vcT", [64, 512], BF16), "vca": self.sb(stn, "vca", [128, 4, 65], BF16)}
                        self.phase_a(l, xcur, xcur_key, N)
                        em.barrier()
                        if "skip_b" not in self.dbg:
                            self.phase_b(l, N)
                            em.barrier()
                Ysrc = self.Yin if "y_in" in self.dbg else self.Y
                self.phase_c(l, xcur, xcur_key, Ysrc)
                em.barrier()
                self.phase_d(l, self.out if last else self.XB, "out" if last else "XB")
                em.barrier()
            em.barrier(engs=("sp",))
            em.replay()
        return nc

    def gelu(self, x_ap, tmp_ap, xkey, tkey):
        em = self.em
        if GELU_NATIVE:
            em.op("act", lambda e: e.activation(out=x_ap, in_=x_ap, func=AF.Gelu_apprx_tanh), reads=[xkey], writes=[xkey])
            return
        em.op("dve", lambda e: e.tensor_tensor(out=tmp_ap, in0=x_ap, in1=x_ap, op=ALU.mult), reads=[xkey], writes=[tkey])
        em.op("dve", lambda e: e.tensor_scalar(out=tmp_ap, in0=tmp_ap, scalar1=0.044715, scalar2=1.0, op0=ALU.mult, op1=ALU.add), reads=[tkey], writes=[tkey])
        em.op("dve", lambda e: e.tensor_tensor(out=tmp_ap, in0=tmp_ap, in1=x_ap, op=ALU.mult), reads=[xkey, tkey], writes=[tkey])
        em.op("act", lambda e: e.activation(out=tmp_ap, in_=tmp_ap, func=AF.Sigmoid, scale=1.5957691216057308), reads=[tkey], writes=[tkey])
        em.op("dve", lambda e: e.tensor_tensor(out=x_ap, in0=tmp_ap, in1=x_ap, op=ALU.mult), reads=[xkey, tkey], writes=[xkey])

    def phase_a(self, l, xcur, xkey, N):
        nc, em, I, T = self.nc, self.em, self.I, self.T
        P = PCOL
        par = self.par
        with ExitStack() as st:
            sb = lambda n, s, d: self.sb(st, n, s, d)
            win = sb("win", [128, NCH, D_IN], BF16)
            xbf = [sb("xbf0", [128, NCH, TT], BF16)]
            w1 = [sb("w1_%d" % i, [64, 32, 256], BF16) for i in range(2)]
            w2c = [sb("w2c_%d" % i, [128, 2, 64], BF16) for i in range(2)]
            peT = sb("peT", [64, 64], BF16)
            cb = sb("cb", [128, 4], F32)
            cmp_h = [sb("cmp_h%d" % i, [64, 16 + TT], BF16) for i in range(2)]
            hid = sb("hid", [128, 2, 32], BF16)
            hpre = sb("hpre", [128, 32], F32)
            htmp = sb("htmp", [128, 32], F32)
            a_h = sb("a_h", [128, 2, 16 + TT], F32)
            S1 = sb("S1", [128, 16 + TT], F32)
            S2 = sb("S2", [128, 16 + TT], F32)
            Ssel = sb("Ssel", [128, 16 + TT], F32)
            dpool = sb("dpool", [128, 2, TT], BF16)
            PW = sb("PW", [128, 2, 128], BF16)
            u_sb = sb("u_sb", [128, 2, TT], F32)
            gtmp = sb("gtmp", [128, TT if not GELU_NATIVE else 2], F32)
            h_h = sb("h_h", [128, 2, 32 + TT], BF16)
            Dg = sb("Dg", [128, 2, 31, 128], BF16)
            cacc = sb("cacc", [128, 2, TT], F32)
            hcb = sb("hcb", [128, 2, TT], BF16)
            pw = sb("pw", [128, 2, 256], BF16)
            self.lnw = {"sq": sb("lnsq", [128, 2, TT], BF16), "rb": sb("lnrb", [128, 2, TT], BF16),
                        "mean": sb("lnmean", [128, TT], F32), "rstd": sb("lnrstd", [128, TT], F32), "tmp": sb("lntmp", [128, TT], F32), "tmp2": sb("lntmp2", [128, TT], F32)}
            sgm = self.lnw["tmp2"]
            mtmp = self.lnw["tmp"][:, 0:128]
            ybuf = sb("ybuf", [128, NCH, TT], BF16)
            qst = sb("qst", [64, 4, TT], BF16)
            vg = sb("vg", [128, 256], F32)
            vt = sb("vt", [128, 256], F32)
            vs1 = sb("vs1", [128, 1], F32)
            vs2 = sb("vs2", [128, 1], F32)
            vpad = [[sb("vpad%d_%d" % (s4, i), [128, 2, 128], BF16) for i in range(2)] for s4 in range(4)]
            WsTf = Ssel[:, 0:512].rearrange("p (g i) -> p g i", g=4)
            WsT = sb("WsT", [128, 4, 128], BF16)
            Btab = sb("Btab", [128, 2, 128], F32)
            Gbc = sb("Gbc", [128, 256], F32)
            Bbc = sb("Bbc", [128, 256], F32)

            em.dma("pool", win[:], I["w_in"][l].rearrange("(c p) n -> p c n", p=128), writes=["win"])
            for i, nm in enumerate(("cmp_k", "cmp_v")):
                em.dma("pool", w1[i][:], I[nm + "_w1"][l].rearrange("(p d) n -> d p n", d=64), writes=[("w1", i)])
                em.dma("pool", w2c[i][:], I[nm + "_w2"][l].rearrange("(c p) n -> p c n", p=128), writes=[("w2c", i)])
            em.dma("pool", pw[:], I["conv_pw"][l].rearrange("(c p) n -> p c n", p=128), writes=["pw"])
            em.op("dve", lambda e: e.memset(PW[:], 0.0), writes=["PW"])
            for c in range(2):
                for k in range(31):
                    em.op("dve", lambda e, c=c, k=k: e.tensor_single_scalar(out=Dg[:, c, k, :], in_=self.identb[:], scalar=par[:, P["conv_w"] + c * 31 + k:P["conv_w"] + c * 31 + k + 1], op=ALU.mult),
                          reads=["identb", "par"], writes=["Dg"])
            for g in range(4):
                em.dma("pool", PW[(g % 2) * 64:(g % 2) * 64 + 64, g // 2, (g % 2) * 64:(g % 2) * 64 + 64], I["pool_w"][l, g], writes=["PW"])
            em.dma("sp", WsTf, I["sgu_wT"][l].rearrange("g j i -> j g i"), writes=["WsTf", "Ssel_lo", "Ssel_hi"])
            for g in range(4):
                em.op("dve", lambda e, g=g: e.tensor_tensor(out=WsT[:, g, :], in0=WsTf[:, g, :], in1=self.cf[:, CF["triu"]:CF["triu"] + 128], op=ALU.mult),
                      reads=["WsTf", "ident"], writes=["WsT"])
                em.dma("sp", Btab[(g % 2) * 64:(g % 2) * 64 + 64, g // 2, :], I["sgu_b"][l, g].partition_broadcast(64), writes=["Btab"])
            em.dma("sp", Gbc[:], I["sgu_ln_g"][l].partition_broadcast(128), writes=["Gbc"])
            em.dma("sp", Bbc[:], I["sgu_ln_b"][l].partition_broadcast(128), writes=["Gbc"])
            em.op("act", lambda e: e.copy(out=peT[:], in_=par[0:64, P["peT"]:P["peT"] + 64]), reads=["par"], writes=["peT"])
            for i in range(2):
                for hc in range(2):
                    b = self.bank()
                    self.mm(self.pb[b][:, 0:1], ("pb", b), [(w1[i][:, p, hc * 128:(hc + 1) * 128], peT[:, i * 32 + p:i * 32 + p + 1], [("w1", i), "peT"]) for p in range(32)])
                    em.op("act", lambda e, i=i, hc=hc, b=b: e.copy(out=cb[:, i * 2 + hc:i * 2 + hc + 1], in_=self.pb[b][:, 0:1]), reads=[("pb", b)], writes=["cb"])
            em.op("dve", lambda e: e.memset(a_h[:, :, 0:16], 0.0), writes=["a_halo"])
            em.op("dve", lambda e: e.memset(h_h[:, :, 0:32], 0.0), writes=["h_halo0", "h_halo1"])
            for i in range(2):
                em.op("dve", lambda e, i=i: e.memset(cmp_h[i][:, 0:16], 0.0), writes=[("cmp_halo", i)])
                for s4 in range(4):
                    em.op("dve", lambda e, i=i, s4=s4: e.memset(vpad[s4][i][:], 0.0), writes=[("vpad", s4, i)])
            em.op("dve", lambda e: e.memset(N["kcT"][:], 0.0), writes=["kcT"])
            em.op("dve", lambda e: e.memset(N["vcT"][:], 0.0), writes=["vcT"])
            em.op("dve", lambda e: e.memset(N["Vsel"][:, :, 64:65], 1.0), writes=["Vsel"])
            em.op("dve", lambda e: e.memset(N["Vwin"][:, :, 64:65], 1.0), writes=["Vwin"])
            em.op("dve", lambda e: e.memset(N["vca"][:, :, 64:65], 1.0), writes=["vca"])

            def fm(col, width, xb, xk):
                b = self.bank()
                self.mm(self.pb[b][0:width, :], ("pb", b), [(win[:, k, col:col + width], xb[:, k, :], ["win", xk]) for k in range(NCH)])
                return b

            for t in range(self.NT):
                sl = 0
                c0 = t * TT
                xb, xk = xbf[sl], ("xbf", sl)
                em.dma("pool", xb[:], xcur[:, c0:c0 + TT].rearrange("(c p) t -> p c t", p=128), reads=[(xkey, t)], writes=[xk])
                for pr in range(2):
                    b = fm(pr * 128, 128, xb, xk)
                    em.op("act", lambda e, pr=pr, b=b: e.copy(out=a_h[:, pr, 16:], in_=self.pb[b][:]), reads=[("pb", b)], writes=[("a_h", pr)])
                for pr in range(2):
                    A = a_h[:, pr, :]
                    rdA = [("a_h", pr), "a_halo"]
                    n = 16 + TT
                    lo, hi = slice(0, 64), slice(64, 128)
                    if pr == 0:
                        em.op("dve", lambda e, A=A: e.tensor_tensor(out=Ssel[lo, 1:n], in0=A[lo, 1:n], in1=A[lo, 0:n - 1], op=ALU.add), reads=rdA, writes=["Ssel_lo"])
                        em.op("dve", lambda e, A=A: e.tensor_tensor(out=S1[hi, 1:n], in0=A[hi, 1:n], in1=A[hi, 0:n - 1], op=ALU.add), reads=rdA, writes=["S1_hi"])
                        em.op("dve", lambda e: e.tensor_tensor(out=Ssel[hi, 3:n], in0=S1[hi, 3:n], in1=S1[hi, 1:n - 2], op=ALU.add), reads=["S1_hi"], writes=["Ssel_hi"])
                    else:
                        em.op("dve", lambda e, A=A: e.tensor_tensor(out=S1[:, 1:n], in0=A[:, 1:n], in1=A[:, 0:n - 1], op=ALU.add), reads=rdA, writes=["S1_hi", "S1_lo"])
                        em.op("dve", lambda e: e.tensor_tensor(out=S2[:, 3:n], in0=S1[:, 3:n], in1=S1[:, 1:n - 2], op=ALU.add), reads=["S1_hi", "S1_lo"], writes=["S2"])
                        em.op("dve", lambda e: e.tensor_tensor(out=Ssel[lo, 7:n], in0=S2[lo, 7:n], in1=S2[lo, 3:n - 4], op=ALU.add), reads=["S2"], writes=["Ssel_lo"])
                        em.op("dve", lambda e: e.tensor_tensor(out=S1[hi, 7:n], in0=S2[hi, 7:n], in1=S2[hi, 3:n - 4], op=ALU.add), reads=["S2"], writes=["S1_hi"])
                        em.op("dve", lambda e: e.tensor_tensor(out=Ssel[hi, 15:n], in0=S1[hi, 15:n], in1=S1[hi, 7:n - 8], op=ALU.add), reads=["S1_hi"], writes=["Ssel_hi"])
                    em.op("dve", lambda e, pr=pr, A=A: e.scalar_tensor_tensor(out=dpool[:, pr, :], in0=Ssel[:, 16:], scalar=self.cf[:, CF["invw"] + pr:CF["invw"] + pr + 1], in1=A[:, 16:], op0=ALU.mult, op1=ALU.subtract),
                          reads=["Ssel_lo", "Ssel_hi", "ident"] + rdA, writes=[("dpool", pr)])
                    if t == 0:
                        em.op("dve", lambda e, pr=pr: e.tensor_tensor(out=Ssel[:, 0:16], in0=Ssel[:, 16:32], in1=self.cf[:, CF["invc"] + pr * 16:CF["invc"] + pr * 16 + 16], op=ALU.mult),
                              reads=["Ssel_lo", "Ssel_hi", "ident", ("dpool", pr)], writes=["Ssel_lo", "Ssel_hi"])
                        em.op("dve", lambda e, pr=pr, A=A: e.tensor_tensor(out=dpool[:, pr, 0:16], in0=Ssel[:, 0:16], in1=A[:, 16:32], op=ALU.subtract),
                              reads=["Ssel_lo", "Ssel_hi", ("dpool", pr)] + rdA, writes=[("dpool", pr)])
                    b = self.bank()
                    self.mm(self.pb[b][:], ("pb", b), [(PW[:, pr, :], dpool[:, pr, :], ["PW", ("dpool", pr)])])
                    em.op("act", lambda e, pr=pr, b=b: e.activation(out=ybuf[:, pr, :], in_=self.pb[b][:], func=AF.Identity, scale=par[:, P["pool_scale"] + pr:P["pool_scale"] + pr + 1]),
                          reads=[("pb", b), "par"], writes=[("ybuf", pr)])
                em.op("dve", lambda e: e.tensor_copy(out=a_h[:, :, 0:16], in_=a_h[:, :, TT:TT + 16]), reads=[("a_h", 0), ("a_h", 1)], writes=["a_halo"])
                for pr in range(2):
                    b = fm(908 + pr * 128, 128, xb, xk)
                    em.op("act", lambda e, pr=pr, b=b: e.copy(out=u_sb[:, pr, :], in_=self.pb[b][:]), reads=[("pb", b)], writes=[("u_sb", pr)])
                    self.gelu(u_sb[:, pr, :], gtmp[:], ("u_sb", pr), "gtmp")
                self.dump("u_sb", u_sb[:], [128, 2, TT], F32, [("u_sb", 0), ("u_sb", 1)])
                for s4 in range(4):
                    ts = slice(s4 * 128, (s4 + 1) * 128)
                    b = self.bank()
                    self.mm(self.pb[b][:, 0:256], ("pb", b), [(xb[:, k, ts], win[:, k, 1164:1420], ["win", xk]) for k in range(NCH)])
                    em.op("act", lambda e, b=b: e.copy(out=vg[:], in_=self.pb[b][:, 0:256]), reads=[("pb", b)], writes=["vg"])
                    self.dump("vg_pre", vg[:], [128, 256], F32, ["vg"])
                    self.gelu(vg[:], vt[:], "vg", "vt")
                    self.dump("vg_gelu", vg[:], [128, 256], F32, ["vg"])
                    em.op("dve", lambda e: e.tensor_reduce(out=vs1[:], in_=vg[:], axis=AX.X, op=ALU.add), reads=["vg"], writes=["vs1"])
                    em.op("dve", lambda e: e.tensor_single_scalar(out=vs1[:], in_=vs1[:], scalar=-1.0 / 256, op=ALU.mult), reads=["vs1"], writes=["vs1"])
                    em.op("dve", lambda e: e.tensor_single_scalar(out=vg[:], in_=vg[:], scalar=vs1[:, 0:1], op=ALU.add), reads=["vg", "vs1"], writes=["vg"])
                    em.op("dve", lambda e: e.tensor_tensor(out=vt[:], in0=vg[:], in1=vg[:], op=ALU.mult), reads=["vg"], writes=["vt"])
                    em.op("dve", lambda e: e.tensor_reduce(out=vs2[:], in_=vt[:], axis=AX.X, op=ALU.add), reads=["vt"], writes=["vs2"])
                    em.op("dve", lambda e: e.tensor_scalar(out=vs2[:], in0=vs2[:], scalar1=1.0 / 256, scalar2=EPS, op0=ALU.mult, op1=ALU.add), reads=["vs2"], writes=["vs2"])
                    em.op("act", lambda e: e.activation(out=vs2[:], in_=vs2[:], func=AF.Sqrt), reads=["vs2"], writes=["vs2"])
                    em.op("dve", lambda e: e.reciprocal(out=vs2[:], in_=vs2[:]), reads=["vs2"], writes=["vs2"])
                    em.op("dve", lambda e: e.scalar_tensor_tensor(out=vg[:], in0=vg[:], scalar=vs2[:, 0:1], in1=Gbc[:], op0=ALU.mult, op1=ALU.mult), reads=["vg", "vs2", "Gbc"], writes=["vg"])
                    self.dump("vg_ln", vg[:], [128, 256], F32, ["vg"])
                    self.dump("vs2", vs2[:], [128, 1], F32, ["vs2"])
                    for g in range(4):
                        em.op("dve", lambda e, g=g, s4=s4: e.tensor_tensor(out=vpad[s4][g % 2][:, g // 2, (g % 2) * 64:(g % 2) * 64 + 64], in0=vg[:, g * 64:(g + 1) * 64], in1=Bbc[:, g * 64:(g + 1) * 64], op=ALU.add),
                              reads=["vg", "Gbc"], writes=[("vpad", s4, g % 2)])
                for h in range(4):
                    b = fm(256 + h * 64, 64, xb, xk)
                    em.op("act", lambda e, h=h, b=b: e.copy(out=qst[:, h, :], in_=self.pb[b][0:64, :]), reads=[("pb", b)], writes=["qst"])
                em.dma("sp", self.QT[:, :, c0:c0 + TT], qst[:], reads=["qst"], writes=[("QT", t)])
                for (col, dst, key) in ((640, N["KselT"], "KselT"), (768, N["KwinT"], "KwinT")):
                    b = fm(col, 64, xb, xk)
                    em.op("act", lambda e, dst=dst, b=b, c0=c0: e.copy(out=dst[:, c0:c0 + TT], in_=self.pb[b][0:64, :]), reads=[("pb", b)], writes=[(key, t)])
                for i in range(2):
                    b = fm(512 + i * 64, 64, xb, xk)
                    em.op("act", lambda e, i=i, b=b: e.copy(out=cmp_h[i][:, 16:], in_=self.pb[b][0:64, :]), reads=[("pb", b)], writes=[("cmp_h", i)])
                j0 = 1 if t == 0 else 0
                nb = 32 - j0
                col0 = 0 if t == 0 else 32 * t - 1
                for i in range(2):
                    for hc in range(2):
                        b = self.bank()
                        self.mm(self.pb[b][:, 0:nb], ("pb", b),
                                [(w1[i][:, p, hc * 128:(hc + 1) * 128], cmp_h[i][:, 16 * j0 + p:16 * j0 + p + 16 * (nb - 1) + 1:16], [("w1", i), ("cmp_h", i), ("cmp_halo", i)]) for p in range(32)])
                        em.op("dve", lambda e, i=i, hc=hc, b=b, nb=nb: e.tensor_single_scalar(out=hpre[:, 0:nb], in_=self.pb[b][:, 0:nb], scalar=cb[:, i * 2 + hc:i * 2 + hc + 1], op=ALU.add),
                              reads=[("pb", b), "cb"], writes=["hpre"])
                        self.gelu(hpre[:, 0:nb], htmp[:, 0:nb], "hpre", "htmp")
                        em.op("act", lambda e, hc=hc, nb=nb: e.copy(out=hid[:, hc, 0:nb], in_=hpre[:, 0:nb]), reads=["hpre"], writes=[("hid", hc)])
                    b = self.bank()
                    self.mm(self.pb[b][0:64, 0:nb], ("pb", b), [(w2c[i][:, hc, :], hid[:, hc, 0:nb], [("w2c", i), ("hid", hc)]) for hc in range(2)])
                    dst, key = (N["kcT"], "kcT") if i == 0 else (N["vcT"], "vcT")
                    em.op("act", lambda e, dst=dst, b=b, col0=col0, nb=nb: e.copy(out=dst[:, col0:col0 + nb], in_=self.pb[b][0:64, 0:nb]), reads=[("pb", b)], writes=[key])
                    em.op("dve", lambda e, i=i: e.tensor_copy(out=cmp_h[i][:, 0:16], in_=cmp_h[i][:, TT:TT + 16]), reads=[("cmp_h", i)], writes=[("cmp_halo", i)])
                for s4 in range(4):
                    qt = t * 4 + s4
                    ts = slice(s4 * 128, (s4 + 1) * 128)
                    b = self.bank()
                    self.mm(self.pb[b][:, 0:204], ("pb", b), [(xb[:, k, ts], win[:, k, 704:908], ["win", xk]) for k in range(NCH)])
                    em.op("act", lambda e, qt=qt, b=b: e.copy(out=N["Vsel"][:, qt, 0:64], in_=self.pb[b][:, 0:64]), reads=[("pb", b)], writes=["Vsel"])
                    em.op("act", lambda e, qt=qt, b=b: e.copy(out=N["Vwin"][:, qt, 0:64], in_=self.pb[b][:, 128:192]), reads=[("pb", b)], writes=["Vwin"])
                    em.op("act", lambda e, qt=qt, b=b: e.activation(out=N["gates"][:, qt, :], in_=self.pb[b][:, 192:204], func=AF.Sigmoid), reads=[("pb", b)], writes=["gates"])
                for c in range(2):
                    ba = fm(1420 + c * 128, 128, xb, xk)
                    bg = fm(1676 + c * 128, 128, xb, xk)
                    em.op("act", lambda e, bg=bg: e.activation(out=sgm[:], in_=self.pb[bg][:], func=AF.Sigmoid), reads=[("pb", bg)], writes=[("ln_tmp", 1)])
                    em.op("dve", lambda e, c=c, ba=ba: e.tensor_tensor(out=h_h[:, c, 32:], in0=self.pb[ba][:], in1=sgm[:], op=ALU.mult), reads=[("pb", ba), ("ln_tmp", 1)], writes=[("h_h", c)])
                    ceng = "dve"
                    b = self.bank()
                    self.mm(self.pb[b][:], ("pb", b), [(Dg[:, c, k, :], h_h[:, c, 2 + k:2 + k + TT], ["Dg", ("h_h", c), ("h_halo%d" % c)]) for k in range(31)])
                    em.op("act", lambda e, c=c, b=b: e.activation(out=cacc[:, c, :], in_=self.pb[b][:], func=AF.Identity, bias=par[:, P["conv_b"] + c:P["conv_b"] + c + 1], scale=1.0),
                          reads=[("pb", b), "par"], writes=[("cacc", c)])
                    em.op(ceng, lambda e, c=c: e.tensor_copy(out=h_h[:, c, 0:32], in_=h_h[:, c, TT:TT + 32]), reads=[("h_h", c)], writes=[("h_halo%d" % c)])
                self.dump("h_h", h_h[:], [128, 2, 32 + TT], BF16, [("h_h", 0), ("h_h", 1)])
                self.dump("cacc", cacc[:], [128, 2, TT], F32, [("cacc", 0), ("cacc", 1)])
                self.ln_fm(cacc, "cacc", 2, P["conv_lng"], P["conv_lnb"], TT, [(hcb, "hcb")], func=AF.Silu)
                self.dump("hcb", hcb[:], [128, 2, TT], BF16, [("hcb", 0), ("hcb", 1)])
                for oc in range(2):
                    b = self.bank()
                    self.mm(self.pb[b][:], ("pb", b), [(pw[:, k2, oc * 128:(oc + 1) * 128], hcb[:, k2, :], ["pw", ("hcb", k2)]) for k2 in range(2)])
                    em.op("act", lambda e, oc=oc, b=b: e.copy(out=ybuf[:, 6 + oc, :], in_=self.pb[b][:]), reads=[("pb", b)], writes=[("ybuf", 6 + oc)])
                for s4 in range(4):
                    ts = slice(s4 * 128, (s4 + 1) * 128)
                    for pr in range(2):
                        b = self.bank()
                        self.mm(self.pb[b][:, 0:128], ("pb", b), [(vpad[s4][hh][:, pr, :], WsT[:, 2 * pr + hh, :], [("vpad", s4, hh), "WsT"]) for hh in range(2)])
                        em.op("dve", lambda e, pr=pr, b=b: e.tensor_tensor(out=mtmp, in0=self.pb[b][:, 0:128], in1=Btab[:, pr, :], op=ALU.add), reads=[("pb", b), "Btab"], writes=[("ln_tmp", 0)])
                        em.op("dve", lambda e, pr=pr, ts=ts: e.tensor_tensor(out=ybuf[:, 4 + pr, ts], in0=mtmp, in1=u_sb[:, pr, ts], op=ALU.mult), reads=[("ln_tmp", 0), ("u_sb", pr)], writes=[("ybuf", 4 + pr)])
                em.dma("sp", self.Y[:, 0:2, c0:c0 + TT], ybuf[:, 0:2, :], reads=[("ybuf", 0), ("ybuf", 1)], writes=[("Y", t)])
                em.dma("sp", self.Y[:, 4:8, c0:c0 + TT], ybuf[:, 4:8, :], reads=[("ybuf", c) for c in range(4, 8)], writes=[("Y", t)])
            for kt in range(4):
                b = self.bank()
                self.mm(self.pb[b][:, 0:64], ("pb", b), [(N["vcT"][:, kt * 128:(kt + 1) * 128], self.identb[0:64, 0:64], ["vcT", "identb"])])
                em.op("act", lambda e, kt=kt, b=b: e.copy(out=N["vca"][:, kt, 0:64], in_=self.pb[b][:, 0:64]), reads=[("pb", b)], writes=["vca"])

    def phase_b(self, l, N):
        nc, em, I, T = self.nc, self.em, self.I, self.T
        NKT = T // 128
        CB = self.CB
        identb = self.identb
        with ExitStack() as st:
            sb = lambda n, s, d: self.sb(st, n, s, d)
            cbt = sb("cbt", [128, self.CBW], BF16)
            em.dma("pool", cbt[:], I["cb"], writes=["cbt"])
            qts = [sb("qts%d" % i, [64, 4, 128], BF16) for i in range(2)]
            PT = [sb("PT%d" % i, [128, 512], BF16) for i in range(3)]
            lsb = sb("lsb", [128, 3, 4], F32)
            coef = sb("coef", [128, 3, 4], F32)
            imps = sb("imps", [128, 128], F32)
            sc = sb("sc", [128, 128], F32)
            sc2 = sb("sc2", [128, 128], F32)
            top8 = sb("top8", [128, 8], F32)
            thr = sb("thr", [128, 1], F32)
            negsel = sb("negsel", [128, 128], BF16)
            negselT = sb("negselT", [128, 128], BF16)
            osb = sb("osb", [128, 256], F32)
            obf = sb("obf", [128, 256], BF16)
            ynsa = sb("ynsa", [128, 2, TT], BF16)
            ACC = {"c": 0, "s": 1, "w": 2}
            IMPB = 3
            self.rot = [4, 5, 6, 7]
            pti = [0]

            def bc(ap2d):
                return ap2d[:, None, :].broadcast_to([128, 4, 128])

            def pair(br, sl, kT_ap, kkeys, masks, v_ap, vkeys, first, last, imp_rhs=None):
                b = self.bank()
                S = self.pb[b][:].rearrange("p (h q) -> p h q", h=4)
                n = 1 + len(masks)
                em.op("pe", lambda e: e.matmul(S, lhsT=kT_ap, rhs=qts[sl][:], start=True, stop=(n == 1)), reads=kkeys + [("qts", sl)], writes=[("pb", b)])
                for mi, (ml, mr, mk) in enumerate(masks):
                    em.op("pe", lambda e, ml=ml, mr=mr, mi=mi: e.matmul(S, lhsT=ml, rhs=mr, start=False, stop=(mi == n - 2)), reads=mk, writes=[("pb", b)])
                ps = pti[0] % 3
                pti[0] += 1
                if self.cur_i == self.dbg.get("DI", -1):
                    t_ = self.sb(st, "Sd", [128, 512], F32)
                    em.op("dve", lambda e, b=b, t_=t_: e.tensor_copy(out=t_[:], in_=self.pb[b][:]), reads=[("pb", b)], writes=["Sd" + br])
                    self.dump("b_S" + br, t_[:], [128, 512], F32, ["Sd" + br])
                em.op("act", lambda e, ps=ps, b=b: e.activation(out=PT[ps][:], in_=self.pb[b][:], func=AF.Exp, scale=0.125), reads=[("pb", b)], writes=[("PT", ps)])
                if self.cur_i == self.dbg.get("DI", -1):
                    self.dump("b_PT" + br, PT[ps][:], [128, 512], BF16, [("PT", ps)])
                ab = ACC[br]

                def stage2():
                    em.op("pe", lambda e, ps=ps: e.matmul(self.pb[ab][0:65, :], lhsT=v_ap, rhs=PT[ps][:], start=first, stop=last),
                          reads=[("PT", ps)] + vkeys, writes=[("acc", br)])
                    if imp_rhs is not None:
                        for h in range(4):
                            em.op("pe", lambda e, h=h, ps=ps: e.matmul(self.pb[IMPB][:, h * 128:(h + 1) * 128], lhsT=PT[ps][:, h * 128:(h + 1) * 128], rhs=imp_rhs, start=(first and h == 0), stop=last, skip_group_check=True),
                                  reads=[("PT", ps), "cbt"], writes=["imp"])
                pending.append(stage2)
                while len(pending) > 1:
                    pending.pop(0)()

            pending = []

            def flush():
                while pending:
                    pending.pop(0)()

            oT = [sb("oT%d" % i_, [65, 512], F32) for i_ in range(3)]

            def finalize(br):
                ab = ACC[br]
                em.op("act", lambda e: e.copy(out=oT[ab][:], in_=self.pb[ab][0:65, :]), reads=[("acc", br)], writes=[("oT", ab)])
                for h in range(4):
                    em.op("pe", lambda e, h=h: e.matmul(self.pb[ab][:, h * 65:(h + 1) * 65], lhsT=oT[ab][:, h * 128:(h + 1) * 128], rhs=self.ident[0:65, 0:65], start=(h == 0), stop=(h == 3), skip_group_check=True),
                          reads=[("oT", ab), "ident"], writes=[("acc", br)])

            for i in range(self.NQ):
                self.cur_i = i
                sl = i % 2
                q0 = i * 128
                em.dma("sp", qts[sl][:], self.QT[:, :, q0:q0 + 128], reads=[("QT", i // 4)], writes=[("qts", sl)])
                ktl = (8 * i + 6) // 128
                ip = i - 16 * ktl
                kts = list(range(ktl + 1))
                for kt in kts:
                    masks = []
                    off = 8 * i - 128 * kt
                    if off <= 128:
                        g = CB["Gm"] + (off // 8) * 128
                        masks.append((identb[:], bc(cbt[:, g:g + 128]), ["identb", "cbt"]))
                    m0 = CB["Mimp"] + kt * 128
                    pair("c", sl, N["kcT"][:, kt * 128:(kt + 1) * 128], ["kcT"], masks, N["vca"][:, kt, :], ["vca"], kt == 0, kt == kts[-1], imp_rhs=cbt[:, m0:m0 + 128])
                wk = list(range(max(0, i - 4), i + 1))
                for kt in wk:
                    masks = []
                    if kt == i - 4:
                        masks.append((identb[:], bc(cbt[:, CB["Wlow"]:CB["Wlow"] + 128]), ["identb", "cbt"]))
                    if kt == i:
                        masks.append((identb[:], bc(cbt[:, CB["Caus"]:CB["Caus"] + 128]), ["identb", "cbt"]))
                    pair("w", sl, N["KwinT"][:, kt * 128:(kt + 1) * 128], [("KwinT", kt // 4)], masks, N["Vwin"][:, kt, :], ["Vwin"], kt == wk[0], kt == wk[-1])
                DI = self.dbg.get("DI", -1)
                if i == DI:
                    self.dump("b_qts", qts[sl][:], [64, 4, 128], BF16, [("qts", sl)])
                    self.dump("b_KwinT", N["KwinT"][:], [64, T], BF16, [("KwinT", t_) for t_ in range(self.NT)])
                    self.dump("b_kcT", N["kcT"][:], [64, 512], BF16, ["kcT"])
                    self.dump("b_vca", N["vca"][:], [128, 4, 65], BF16, ["vca"])
                    self.dump("b_Vwin", N["Vwin"][:], [128, self.NQ, 65], BF16, ["Vwin"])
                    self.dump("b_accc", self.pbcopy(st, 0, [("acc", "c")]), [128, 512], F32, ["pbc0"])
                    self.dump("b_accw", self.pbcopy(st, 2, [("acc", "w")]), [128, 512], F32, ["pbc2"])
                    self.dump("b_imp", self.pbcopy(st, 3, ["imp"]), [128, 512], F32, ["pbc3"])
                flush()
                finalize("c")
                finalize("w")
                accc = self.pb[0][:, 0:260].rearrange("p (h d) -> p h d", d=65)
                em.op("dve", lambda e: e.tensor_single_scalar(out=lsb[:, 0, :], in_=accc[:, :, 64], scalar=1e-30, op=ALU.max), reads=[("acc", "c")], writes=[("lsb", 0)])
                em.op("dve", lambda e: e.reciprocal(out=lsb[:, 0, :], in_=lsb[:, 0, :]), reads=[("lsb", 0)], writes=[("lsb", 0)])
                for h in range(4):
                    if h == 0:
                        em.op("dve", lambda e: e.tensor_single_scalar(out=imps[:], in_=self.pb[IMPB][:, 0:128], scalar=lsb[:, 0, 0:1], op=ALU.mult), reads=["imp", ("lsb", 0)], writes=["imps"])
                    else:
                        em.op("dve", lambda e, h=h: e.scalar_tensor_tensor(out=imps[:], in0=self.pb[IMPB][:, h * 128:(h + 1) * 128], scalar=lsb[:, 0, h:h + 1], in1=imps[:], op0=ALU.mult, op1=ALU.add),
                              reads=["imp", ("lsb", 0), "imps"], writes=["imps"])
                w0 = 126 - 2 * i
                em.op("dve", lambda e, w0=w0: e.tensor_tensor(out=sc[:], in0=imps[:], in1=self.cf[:, CF["CW"] + w0:CF["CW"] + w0 + 128], op=ALU.add), reads=["imps", "ident"], writes=["sc"])
                em.op("dve", lambda e: e.tensor_single_scalar(out=sc[:, 0:1], in_=sc[:, 0:1], scalar=1e4, op=ALU.add), reads=["sc"], writes=["sc"])
                em.op("dve", lambda e, w0=w0: e.tensor_tensor(out=sc[:], in0=sc[:], in1=self.cf[:, CF["VW"] + w0:CF["VW"] + w0 + 128], op=ALU.mult), reads=["sc", "ident"], writes=["sc"])
                em.op("dve", lambda e: e.max(out=top8[:], in_=sc[:]), reads=["sc"], writes=["top8"])
                em.op("dve", lambda e: e.match_replace(out=sc2[:], in_to_replace=top8[:], in_values=sc[:], imm_value=-1.0), reads=["sc", "top8"], writes=["sc2"])
                em.op("dve", lambda e: e.max(out=top8[:], in_=sc2[:]), reads=["sc2"], writes=["top8"])
                em.op("dve", lambda e: e.tensor_single_scalar(out=thr[:], in_=top8[:, 7:8], scalar=0.5, op=ALU.max), reads=["top8"], writes=["thr"])
                em.op("dve", lambda e: e.tensor_scalar(out=negsel[:], in0=sc[:], scalar1=thr[:, 0:1], scalar2=NEGM, op0=ALU.is_lt, op1=ALU.mult), reads=["sc", "thr"], writes=["negsel"])
                b = self.bank()
                em.op("pe", lambda e, b=b: e.matmul(self.pb[b][:, 0:128], lhsT=negsel[:], rhs=identb[:], start=True, stop=True), reads=["negsel", "identb"], writes=[("pb", b)])
                em.op("act", lambda e, b=b: e.copy(out=negselT[:], in_=self.pb[b][:, 0:128]), reads=[("pb", b)], writes=["negselT"])
                if i == DI:
                    self.dump("b_imps", imps[:], [128, 128], F32, ["imps"])
                    self.dump("b_sc", sc[:], [128, 128], F32, ["sc"])
                    self.dump("b_thr", thr[:], [128, 1], F32, ["thr"])
                    self.dump("b_negselT", negselT[:], [128, 128], BF16, ["negselT"])
                    self.dump("b_lsb", lsb[:], [128, 3, 4], F32, [("lsb", 0)])
                for kt in range(i + 1):
                    e0 = CB["E"] + kt * 128
                    masks = [(cbt[:, e0:e0 + 128], bc(negselT[:]), ["cbt", "negselT"])]
                    if kt == i:
                        masks.append((identb[:], bc(cbt[:, CB["Caus"]:CB["Caus"] + 128]), ["identb", "cbt"]))
                    pair("s", sl, N["KselT"][:, kt * 128:(kt + 1) * 128], [("KselT", kt // 4)], masks, N["Vsel"][:, kt, :], ["Vsel"], kt == 0, kt == i)
                flush()
                finalize("s")
                for bi, br in enumerate(("s", "w")):
                    av = self.pb[ACC[br]][:, 0:260].rearrange("p (h d) -> p h d", d=65)
                    em.op("dve", lambda e, av=av, bi=bi: e.tensor_single_scalar(out=lsb[:, bi + 1, :], in_=av[:, :, 64], scalar=1e-30, op=ALU.max), reads=[("acc", br)], writes=[("lsb", bi + 1)])
                    em.op("dve", lambda e, bi=bi: e.reciprocal(out=lsb[:, bi + 1, :], in_=lsb[:, bi + 1, :]), reads=[("lsb", bi + 1)], writes=[("lsb", bi + 1)])
                gv = N["gates"][:, i, :].rearrange("p (h b) -> p b h", b=3)
                em.op("dve", lambda e, gv=gv: e.tensor_tensor(out=coef[:], in0=lsb[:], in1=gv, op=ALU.mult), reads=[("lsb", 0), ("lsb", 1), ("lsb", 2), "gates"], writes=["coef"])
                for h in range(4):
                    for bi, br in enumerate(("c", "s", "w")):
                        av = self.pb[ACC[br]][:, h * 65:h * 65 + 64]
                        oh = osb[:, h * 64:(h + 1) * 64]
                        if bi == 0:
                            em.op("dve", lambda e, av=av, oh=oh, bi=bi, h=h: e.tensor_single_scalar(out=oh, in_=av, scalar=coef[:, bi, h:h + 1], op=ALU.mult), reads=[("acc", br), "coef"], writes=[("osb", h)])
                        else:
                            em.op("dve", lambda e, av=av, oh=oh, bi=bi, h=h: e.scalar_tensor_tensor(out=oh, in0=av, scalar=coef[:, bi, h:h + 1], in1=oh, op0=ALU.mult, op1=ALU.add),
                                  reads=[("acc", br), "coef", ("osb", h)], writes=[("osb", h)])
                if i == DI:
                    self.dump("b_accs", self.pbcopy(st, 1, [("acc", "s")]), [128, 512], F32, ["pbc1"])
                    self.dump("b_coef", coef[:], [128, 3, 4], F32, ["coef"])
                    self.dump("b_osb", osb[:], [128, 256], F32, [("osb", h) for h in range(4)])
                em.op("act", lambda e: e.copy(out=obf[:], in_=osb[:]), reads=[("osb", h) for h in range(4)], writes=["obf"])
                for c in range(2):
                    b = self.bank()
                    em.op("pe", lambda e, c=c, b=b: e.matmul(self.pb[b][:, 0:128], lhsT=obf[:, c * 128:(c + 1) * 128], rhs=identb[:], start=True, stop=True), reads=["obf", "identb"], writes=[("pb", b)])
                    em.op("act", lambda e, c=c, b=b, i=i: e.copy(out=ynsa[:, c, (i % 4) * 128:(i % 4 + 1) * 128], in_=self.pb[b][:, 0:128]), reads=[("pb", b)], writes=["ynsa"])
                if i % 4 == 3:
                    t = i // 4
                    em.dma("sp", self.Y[:, 2:4, t * TT:(t + 1) * TT], ynsa[:], reads=["ynsa"], writes=[("Y", t)])
            self.rot = list(range(8))

    def phase_c(self, l, xcur, xkey, Ysrc):
        nc, em, I, T = self.nc, self.em, self.I, self.T
        with ExitStack() as st:
            sb = lambda n, s, d: self.sb(st, n, s, d)
            KxT = sb("KxT", [128, NCH, MEM], BF16)
            Vx = sb("Vx", [128, 2, D], BF16)

            def wload(dst, src, key):
                em.dma("pool", dst[:], src.rearrange("(c p) n -> p c n", p=128), writes=[key])
            with ExitStack() as st2:
                wkv = self.sb(st2, "wkv", [128, NCH, 2 * D], BF16)
                memT = self.sb(st2, "memT", [128, NCH, MEM], BF16)
                wload(wkv, I["xkv_w"][l], "wkv")
                wload(memT, I["memT"], "memT")
                for oc in range(NCH):
                    b = self.bank()
                    self.mm(self.pb[b][:, :MEM], ("pb", b), [(wkv[:, k, oc * 128:(oc + 1) * 128], memT[:, k, :], ["wkv", "memT"]) for k in range(NCH)])
                    em.op("act", lambda e, oc=oc, b=b: e.copy(out=KxT[:, oc, :], in_=self.pb[b][:, :MEM]), reads=[("pb", b)], writes=["KxT"])
                for mt in range(2):
                    for hf in range(2):
                        b = self.bank()
                        self.mm(self.pb[b][:], ("pb", b), [(memT[:, k, mt * 128:(mt + 1) * 128], wkv[:, k, D + hf * 512:D + (hf + 1) * 512], ["wkv", "memT"]) for k in range(NCH)])
                        em.op("act", lambda e, mt=mt, hf=hf, b=b: e.copy(out=Vx[:, mt, hf * 512:(hf + 1) * 512], in_=self.pb[b][:]), reads=[("pb", b)], writes=["Vx"])
            em.barrier()
            wout = sb("wout", [128, NCH, D], BF16)
            wq = sb("wq", [128, NCH, D], BF16)
            wo = sb("wo", [128, NCH, D], BF16)
            self.lnw = {"sq": sb("lnsq", [128, NCH, TT], BF16), "rb": sb("lnrb", [128, NCH, TT], BF16),
                        "mean": sb("lnmean", [128, TT], F32), "rstd": sb("lnrstd", [128, TT], F32), "tmp": sb("lntmp", [128, TT], F32), "tmp2": sb("lntmp2", [128, TT], F32)}
            ybf = [sb("ybf%d" % i, [128, NCH, TT], BF16) for i in range(2)]
            xs = [sb("xs%d" % i, [128, NCH, TT], F32) for i in range(2)]
            r = sb("r", [128, NCH, TT], F32)
            x1 = sb("x1", [128, NCH, TT], F32)
            qx = sb("qx", [128, NCH, TT], BF16)
            pT = sb("pT", [128, 2, TT], BF16)
            rl = sb("rl", [128, TT], F32)
            ox = sb("ox", [128, NCH, TT], BF16)
            x2 = sb("x2", [128, NCH, TT], F32)
            wload(wout, I["w_out"][l], "wout")
            wload(wq, I["xq_w"][l], "wq")
            wload(wo, I["xo_w"][l], "wo")

            P = PCOL
            for t in range(self.NT):
                sl = t % 2
                c0 = t * TT
                em.dma("sp", ybf[sl][:], Ysrc[:, :, c0:c0 + TT], reads=[("Y", t)], writes=[("ybf", sl)] + [(("x1b", sl), c) for c in range(NCH)])
                x1b = ybf[sl]
                em.dma("sp", xs[sl][:], xcur[:, c0:c0 + TT].rearrange("(c p) t -> p c t", p=128), reads=[(xkey, t)], writes=[("xs", sl)])
                for oc in range(NCH):
                    b = self.bank()
                    self.mm(self.pb[b][:], ("pb", b), [(wout[:, k, oc * 128:(oc + 1) * 128], ybf[sl][:, k, :], ["wout", ("ybf", sl)]) for k in range(NCH)])
                    em.op("dve", lambda e, oc=oc, b=b, sl=sl: e.scalar_tensor_tensor(out=r[:, oc, :], in0=xs[sl][:, oc, :], scalar=ALPHA, in1=self.pb[b][:], op0=ALU.mult, op1=ALU.add),
                          reads=[("xs", sl), ("pb", b)], writes=[("r", oc)])
                self.ln_fm(r, "r", NCH, P["ln1g"], P["ln1b"], TT, [(x1, "x1"), (x1b, ("x1b", sl))])
                for oc in range(NCH):
                    b = self.bank()
                    self.mm(self.pb[b][:], ("pb", b), [(wq[:, k, oc * 128:(oc + 1) * 128], x1b[:, k, :], ["wq", (("x1b", sl), k)]) for k in range(NCH)])
                    em.op("act", lambda e, oc=oc, b=b: e.copy(out=qx[:, oc, :], in_=self.pb[b][:]), reads=[("pb", b)], writes=[("qx", oc)])
                for h in range(4):
                    for mt in range(2):
                        b = self.bank()
                        self.mm(self.pb[b][:], ("pb", b), [(KxT[:, 2 * h + dc, mt * 128:(mt + 1) * 128], qx[:, 2 * h + dc, :], ["KxT", ("qx", 2 * h + dc)]) for dc in range(2)])
                        em.op("act", lambda e, mt=mt, b=b: e.activation(out=pT[:, mt, :], in_=self.pb[b][:], func=AF.Exp, scale=1.0 / 16.0),
                              reads=[("pb", b)], writes=[("pT", mt)])
                    b = self.bank()
                    self.mm(self.pb[b][:], ("pb", b), [(self.ones1[:], pT[:, mt, :], ["ones", ("pT", mt)]) for mt in range(2)])
                    em.op("dve", lambda e, b=b: e.reciprocal(out=rl[:], in_=self.pb[b][:]), reads=[("pb", b)], writes=["rl"])
                    for dc in range(2):
                        b = self.bank()
                        self.mm(self.pb[b][:], ("pb", b), [(Vx[:, mt, h * 256 + dc * 128:h * 256 + (dc + 1) * 128], pT[:, mt, :], ["Vx", ("pT", mt)]) for mt in range(2)])
                        em.op("dve", lambda e, h=h, dc=dc, b=b: e.tensor_tensor(out=ox[:, 2 * h + dc, :], in0=self.pb[b][:], in1=rl[:], op=ALU.mult),
                              reads=[("pb", b), "rl"], writes=[("ox", 2 * h + dc)])
                for oc in range(NCH):
                    b = self.bank()
                    self.mm(self.pb[b][:], ("pb", b), [(wo[:, k, oc * 128:(oc + 1) * 128], ox[:, k, :], ["wo", ("ox", k)]) for k in range(NCH)])
                    em.op("dve", lambda e, oc=oc, b=b: e.scalar_tensor_tensor(out=r[:, oc, :], in0=x1[:, oc, :], scalar=ALPHA, in1=self.pb[b][:], op0=ALU.mult, op1=ALU.add),
                          reads=[("x1", oc), ("pb", b)], writes=[("r", oc)])
                self.ln_fm(r, "r", NCH, P["ln2g"], P["ln2b"], TT, [(x2, "x2")])
                em.dma("sp", self.XA[:, c0:c0 + TT].rearrange("(c p) t -> p c t", p=128), x2[:], reads=[("x2", c) for c in range(NCH)], writes=[("XA", t)])

    def phase_d(self, l, xdst, dkey):
        nc, em, I, T = self.nc, self.em, self.I, self.T
        moe = (l % 2 == 1)
        li = l // 2
        ST = min(1024 if moe else 2048, T)
        nsub = ST // TT
        GC = 4
        with ExitStack() as st:
            sb = lambda n, s, d: self.sb(st, n, s, d)
            self.lnw = {"sq": sb("lnsq", [128, NCH, TT], BF16), "rb": sb("lnrb", [128, NCH, TT], BF16),
                        "mean": sb("lnmean", [128, TT], F32), "rstd": sb("lnrstd", [128, TT], F32), "tmp": sb("lntmp", [128, TT], F32), "tmp2": sb("lntmp2", [128, TT], F32)}
            xb = sb("xb", [128, NCH, ST], BF16)
            acc = sb("acc", [128, NCH, ST], F32)
            w13 = [sb("w13_%d" % i, [128, NCH, 2, GC * 128], BF16) for i in range(2)]
            w2 = [sb("w2_%d" % i, [128, GC, D], BF16) for i in range(2)]
            hT = sb("hT", [128, GC, TT], BF16)
            sg = sb("sg", [128, TT], F32)
            xf = None if moe else sb("xf", [128, NCH, TT], F32)
            if moe:
                xsc = sb("xsc", [128, NCH, ST], BF16)
                rw = sb("rw", [128, NCH, NEXP], F32)
                xf32 = sb("xf32", [128, NCH, ST], F32)
                lg = sb("lg", [128, ST // 128, NEXP], F32)
                top = sb("top", [128, 8], F32)
                nm1 = sb("nm1", [128, 1], F32)
                den = sb("den", [128, 1], F32)
                gate = sb("gate", [128, ST // 128, NEXP], F32)
                gbc = sb("gbc", [128, TT], F32)
                em.dma("sp", rw[:], I["router_w"][li].rearrange("(c p) n -> p c n", p=128), writes=["rw"])
            nff = (D_FFE if moe else D_FF) // 128
            groups = [(g0, min(GC, nff - g0)) for g0 in range(0, nff, GC)]
            FF = D_FFE if moe else D_FF
            gi = 0
            for s0 in range(0, T, ST):
                tiles = list(range(s0 // TT, (s0 + ST) // TT))
                em.dma("pool", xb[:], self.XA[:, s0:s0 + ST].rearrange("(c p) t -> p c t", p=128), reads=[("XA", t) for t in tiles], writes=["xb"])
                if moe:
                    em.dma("sp", xf32[:], self.XA[:, s0:s0 + ST].rearrange("(c p) t -> p c t", p=128), reads=[("XA", t) for t in tiles], writes=["xf32"])
                    for j in range(ST // 128):
                        b = self.bank()
                        self.mm(self.pb[b][:, :NEXP], ("pb", b), [(xf32[:, k, j * 128:(j + 1) * 128], rw[:, k, :], ["xf32", "rw"]) for k in range(NCH)])
                        em.op("dve", lambda e, j=j, b=b: e.tensor_copy(out=lg[:, j, :], in_=self.pb[b][:, :NEXP]), reads=[("pb", b)], writes=["lg"])
                        em.op("dve", lambda e, j=j: e.max(out=top[:], in_=lg[:, j, :]), reads=["lg"], writes=["top"])
                        em.op("dve", lambda e: e.tensor_single_scalar(out=nm1[:], in_=top[:, 0:1], scalar=-1.0, op=ALU.mult), reads=["top"], writes=["nm1"])
                        em.op("act", lambda e, j=j: e.activation(out=gate[:, j, :], in_=lg[:, j, :], func=AF.Exp, bias=nm1[:], scale=1.0), reads=["lg", "nm1"], writes=["gate"])
                        em.op("dve", lambda e, j=j: e.scalar_tensor_tensor(out=gate[:, j, :], in0=lg[:, j, :], scalar=top[:, 1:2], in1=gate[:, j, :], op0=ALU.is_ge, op1=ALU.mult),
                              reads=["lg", "top", "gate"], writes=["gate"])
                        em.op("dve", lambda e, j=j: e.tensor_reduce(out=den[:], in_=gate[:, j, :], axis=AX.X, op=ALU.add), reads=["gate"], writes=["den"])
                        em.op("dve", lambda e: e.reciprocal(out=den[:], in_=den[:]), reads=["den"], writes=["den"])
                        em.op("dve", lambda e, j=j: e.tensor_single_scalar(out=gate[:, j, :], in_=gate[:, j, :], scalar=den[:, 0:1], op=ALU.mult), reads=["gate", "den"], writes=["gate"])
                for ex in range(NEXP if moe else 1):
                    if moe:
                        w13src, w2src = I["exp_w13"][li, ex], I["exp_w2"][li, ex]
                        for su in range(nsub):
                            b = self.bank()
                            for jj in range(4):
                                j = su * 4 + jj
                                em.op("pe", lambda e, j=j, jj=jj, b=b, ex=ex: e.matmul(self.pb[b][:, jj * 128:(jj + 1) * 128], lhsT=gate[:, j, ex:ex + 1].broadcast_to([128, 128]), rhs=self.ident, start=True, stop=True),
                                      reads=["gate", "ident"], writes=[("pb", b)])
                            em.op("act", lambda e, b=b: e.copy(out=gbc[:], in_=self.pb[b][:]), reads=[("pb", b)], writes=["gbc"])
                            for c in range(NCH):
                                em.op("dve", lambda e, c=c, su=su: e.tensor_tensor(out=xsc[:, c, su * TT:(su + 1) * TT], in0=xf32[:, c, su * TT:(su + 1) * TT], in1=gbc[:], op=ALU.mult),
                                      reads=["xf32", "gbc"], writes=[("xsc", su)])
                    else:
                        w13src, w2src = I["ffn_w13"][li], I["ffn_w2"][li]
                    for (g0, gn) in groups:
                        ws = gi % 2
                        gi += 1
                        for hf in range(2):
                            em.dma("pool", w13[ws][:, :, hf, :gn * 128], w13src[:, hf * FF + g0 * 128:hf * FF + (g0 + gn) * 128].rearrange("(c p) n -> p c n", p=128), writes=[("w13", ws)])
                        em.dma("pool", w2[ws][:, :gn, :], w2src[g0 * 128:(g0 + gn) * 128, :].rearrange("(c p) n -> p c n", p=128), writes=[("w2", ws)])
                        for su in range(nsub):
                            ts = slice(su * TT, (su + 1) * TT)
                            for j in range(gn):
                                bg, bu = self.bank(), self.bank()
                                self.mm(self.pb[bg][:], ("pb", bg), [(w13[ws][:, k, 0, j * 128:(j + 1) * 128], xb[:, k, ts], [("w13", ws), "xb"]) for k in range(NCH)])
                                usrc = xsc if moe else xb
                                ukey = ("xsc", su) if moe else "xb"
                                self.mm(self.pb[bu][:], ("pb", bu), [(w13[ws][:, k, 1, j * 128:(j + 1) * 128], usrc[:, k, ts], [("w13", ws), ukey]) for k in range(NCH)])
                                em.op("act", lambda e, bg=bg: e.activation(out=sg[:], in_=self.pb[bg][:], func=AF.Silu), reads=[("pb", bg)], writes=["sg"])
                                em.op("dve", lambda e, j=j, bu=bu: e.tensor_tensor(out=hT[:, j, :], in0=self.pb[bu][:], in1=sg[:], op=ALU.mult), reads=[("pb", bu), "sg"], writes=[("hT", j)])
                            first = (ex == 0 and g0 == 0)
                            for oc in range(NCH):
                                b = self.bank()
                                self.mm(self.pb[b][:], ("pb", b), [(w2[ws][:, j, oc * 128:(oc + 1) * 128], hT[:, j, :], [("w2", ws), ("hT", j)]) for j in range(gn)])
                                if first:
                                    em.op("act", lambda e, oc=oc, b=b, ts=ts: e.copy(out=acc[:, oc, ts], in_=self.pb[b][:]), reads=[("pb", b)], writes=[("acc", su, oc)])
                                else:
                                    em.op("dve", lambda e, oc=oc, b=b, ts=ts: e.tensor_tensor(out=acc[:, oc, ts], in0=acc[:, oc, ts], in1=self.pb[b][:], op=ALU.add),
                                          reads=[("pb", b), ("acc", su, oc)], writes=[("acc", su, oc)])
                P = PCOL
                for su in range(nsub):
                    t = s0 // TT + su
                    c0 = t * TT
                    ts = slice(su * TT, (su + 1) * TT)
                    if moe:
                        xv, xk = xf32[:, :, ts], "xf32"
                    else:
                        em.dma("sp", xf[:], self.XA[:, c0:c0 + TT].rearrange("(c p) t -> p c t", p=128), reads=[("XA", t)], writes=["xf32"])
                        xv, xk = xf[:], "xf32"
                    rv = acc[:, :, ts]
                    for oc in range(NCH):
                        em.op("dve", lambda e, oc=oc, xv=xv, rv=rv: e.scalar_tensor_tensor(out=rv[:, oc, :], in0=xv[:, oc, :], scalar=ALPHA, in1=rv[:, oc, :], op0=ALU.mult, op1=ALU.add),
                              reads=[xk, ("acc", su, oc)], writes=[("acc", su, oc), ("rr", oc)])
                    self.ln_fm(rv, "rr", NCH, P["ln3g"], P["ln3b"], TT, [(xv, "x3")])
                    em.dma("sp", xdst[:, c0:c0 + TT].rearrange("(c p) t -> p c t", p=128), xv, reads=[("x3", c) for c in range(NCH)] + [xk], writes=[(dkey, t), xk])


def _chunkcols(v, nch):
    return np.ascontiguousarray(np.asarray(v, np.float32).reshape(nch, 128).T)


def pack_params(inp):
    P = np.zeros((DEPTH, 128, NP), np.float32)
    for l in range(DEPTH):
        for n, src in (("ln1g", "ln1_g"), ("ln1b", "ln1_b"), ("ln2g", "ln2_g"), ("ln2b", "ln2_b"), ("ln3g", "ln3_g"), ("ln3b", "ln3_b")):
            P[l, :, PCOL[n]:PCOL[n] + 8] = _chunkcols(inp[src][l], 8)
        P[l, :, PCOL["pool_scale"]:PCOL["pool_scale"] + 2] = _chunkcols(inp["pool_scale"][l], 2)
        cw = np.asarray(inp["conv_w"][l], np.float32)
        for c in range(2):
            P[l, :, PCOL["conv_w"] + c * 31:PCOL["conv_w"] + (c + 1) * 31] = cw[:, c * 128:(c + 1) * 128].T
        P[l, :, PCOL["conv_b"]:PCOL["conv_b"] + 2] = _chunkcols(inp["conv_b"][l], 2)
        P[l, :, PCOL["conv_lng"]:PCOL["conv_lng"] + 2] = _chunkcols(inp["conv_ln_g"][l], 2)
        P[l, :, PCOL["conv_lnb"]:PCOL["conv_lnb"] + 2] = _chunkcols(inp["conv_ln_b"][l], 2)
        P[l, 0:64, PCOL["peT"]:PCOL["peT"] + 32] = np.asarray(inp["cmp_pe_k"][l], np.float32).T
        P[l, 0:64, PCOL["peT"] + 32:PCOL["peT"] + 64] = np.asarray(inp["cmp_pe_v"][l], np.float32).T
    return P


def const_f32():
    c = np.zeros((128, CF32_W), np.float32)
    c[:, 0:128] = np.eye(128, dtype=np.float32)
    c[:, 128:256] = np.triu(np.ones((128, 128), np.float32))
    wins = (2, 4, 8, 16)
    for p in range(128):
        for pr in range(2):
            w = wins[2 * pr + (1 if p >= 64 else 0)]
            c[p, CF["invw"] + pr] = 1.0 / w
            for t in range(16):
                c[p, CF["invc"] + pr * 16 + t] = 1.0 / min(t + 1, w)
        cq = 1 if p >= 64 else 0
        for m in range(256):
            rel = m - 126
            c[p, CF["VW"] + m] = 1.0 if rel <= cq else 0.0
            c[p, CF["CW"] + m] = 1.0 + (1e4 if (rel == cq or rel == cq - 1) else 0.0)
    return c


def const_cb(T):
    NKT = T // 128
    cb = np.zeros((128, (23 + NKT) * 128), np.float32)
    n = np.arange(128)[:, None]
    q = np.arange(128)[None, :]
    for oi in range(17):
        off = 8 * oi
        cb[:, oi * 128:(oi + 1) * 128] = np.where(16 * (n - off) + 31 <= q, 0.0, NEGM)
    for kt in range(4):
        nn = 128 * kt + n
        j = q
        cb[:, (17 + kt) * 128:(18 + kt) * 128] = ((nn >= 4 * j - 1) & (nn <= 4 * j + 3)).astype(np.float32)
    cb[:, 21 * 128:22 * 128] = np.where(n > q, NEGM, 0.0)
    cb[:, 22 * 128:23 * 128] = np.where(n <= q, NEGM, 0.0)
    for kt in range(NKT):
        cb[:, (23 + kt) * 128:(24 + kt) * 128] = (n == 2 * kt + q // 64).astype(np.float32)
    return cb


def core_inputs(inp, b):
    m = {}
    m["xT"] = np.ascontiguousarray(np.asarray(inp["x"][b], np.float32).T)
    m["memT"] = np.ascontiguousarray(np.asarray(inp["mem"][b], np.float32).T)
    for k in ("w_in", "w_out", "xq_w", "xkv_w", "xo_w", "ffn_w13", "ffn_w2", "router_w", "exp_w13", "exp_w2",
              "cmp_k_w1", "cmp_k_w2", "cmp_v_w1", "cmp_v_w2", "conv_pw", "pool_w", "sgu_b", "sgu_ln_g", "sgu_ln_b"):
        m[k] = np.ascontiguousarray(np.asarray(inp[k], np.float32))
    m["sgu_wT"] = np.ascontiguousarray(np.asarray(inp["sgu_w"], np.float32).transpose(0, 1, 3, 2))
    return m


_CACHE = {}


def kernel(**inp):
    T = 8192
    if "nc" not in _CACHE:
        _CACHE["nc"] = Builder(T, DEPTH).build()
    nc = _CACHE["nc"]
    params = pack_params(inp)
    cf = const_f32()
    cb = const_cb(T)
    in_maps = []
    for c in range(4):
        m = core_inputs(inp, c)
        m["params"] = params
        m["cf32"] = cf
        m["cb"] = cb
        in_maps.append(m)
    res = run_bass_kernel_spmd(nc, in_maps, core_ids=[0, 1, 2, 3])
    out = np.stack([np.ascontiguousarray(np.asarray(res.results[c]["outT"]).T) for c in range(4)])
    return out.astype(np.float32)
```

```python
import numpy as np
from contextlib import ExitStack
import concourse.bass as bass
import concourse.mybir as mybir
from concourse.bass_utils import run_bass_kernel_spmd

F32 = mybir.dt.float32
BF16 = mybir.dt.bfloat16
ALU = mybir.AluOpType
AF = mybir.ActivationFunctionType
AX = mybir.AxisListType

D = 1024
NCH = 8
DEPTH = 4
MEM = 256
D_IN = 1932
D_FF = 2816
D_FFE = 3584
NEXP = 8
ALPHA = (2 * DEPTH) ** 0.25
EPS = 1e-5
NEGM = -30000.0
TT = 512
GELU_NATIVE = True

ENGS = ("pe", "act", "dve", "pool", "sp")
N_DMA_SEMS = 40


class Em:
    def __init__(self, nc, stack):
        self.nc = nc
        self.prog = {e: [] for e in ENGS}
        self.cnt = {e: 0 for e in ENGS}
        self.waited = {e: {} for e in ENGS}
        self.res = {}
        self.sem = {e: stack.enter_context(nc.semaphore("s_" + e)) for e in ENGS}
        self.dsem = [stack.enter_context(nc.semaphore("d%d" % i)) for i in range(N_DMA_SEMS)]
        self.ndma = 0
        self.nwaits = 0
        self.dlast = {}

    def _st(self, key):
        st = self.res.get(key)
        if st is None:
            st = {"w": None, "r": {}}
            self.res[key] = st
        return st

    def _need(self, eng, tok, same_ok):
        if tok is None:
            return
        if tok[0] == "e":
            _, e2, k = tok
            if e2 == eng and same_ok:
                return
            if self.waited[eng].get(e2, 0) >= k:
                return
            self.waited[eng][e2] = k
            self.prog[eng].append(("wait", self.sem[e2], k))
            self.nwaits += 1
        else:
            _, si, target = tok
            if self.waited[eng].get(("d", si), 0) >= target:
                return
            self.waited[eng][("d", si)] = target
            self.prog[eng].append(("wait", self.dsem[si], target))
            self.nwaits += 1

    def _deps(self, eng, reads, writes):
        for r in reads:
            self._need(eng, self._st(r)["w"], same_ok=(eng == "pe"))
        for w in writes:
            st = self._st(w)
            self._need(eng, st["w"], same_ok=True)
            for tok in st["r"].values():
                self._need(eng, tok, same_ok=True)

    def _commit(self, tok, rkey, reads, writes):
        for r in reads:
            self._st(r)["r"][rkey] = tok
        for w in writes:
            st = self._st(w)
            st["w"] = tok
            st["r"] = {}

    @staticmethod
    def _excl(reads, writes):
        ex = [k for k in reads if (isinstance(k, tuple) and k[0] in ("pb", "acc")) or k == "imp"]
        if not ex:
            return reads, writes
        return [k for k in reads if k not in ex], list(writes) + [k for k in ex if k not in writes]

    def op(self, eng, fn, reads=(), writes=()):
        reads, writes = self._excl(list(reads), list(writes))
        self._deps(eng, reads, writes)
        self.cnt[eng] += 1
        tok = ("e", eng, self.cnt[eng])
        self.prog[eng].append(("op", fn, self.sem[eng]))
        self._commit(tok, eng, reads, writes)
        return tok

    def dma(self, q, out, in_, reads=(), writes=(), **kw):
        k = self.ndma
        self.ndma += 1
        si = k % N_DMA_SEMS
        target = 16 * (k // N_DMA_SEMS + 1)
        if target > 16:
            self._need(q, ("d", si, target - 16), same_ok=False)
        self._deps(q, reads, writes)
        tok = ("d", si, target)
        self.dlast[si] = tok
        self.prog[q].append(("dma", out, in_, self.dsem[si], kw))
        self._commit(tok, ("dq", si), reads, writes)
        return tok

    def barrier(self, engs=ENGS):
        for e in engs:
            for e2 in ENGS:
                if e2 != e and self.cnt[e2] > 0:
                    self._need(e, ("e", e2, self.cnt[e2]), same_ok=False)
            for tok in self.dlast.values():
                self._need(e, tok, same_ok=False)

    def replay(self):
        nc = self.nc
        engmap = {"pe": "tensor", "act": "scalar", "dve": "vector", "pool": "gpsimd", "sp": "sync"}
        with nc.Block() as block:
            for e in ENGS:
                prog = self.prog[e]

                def body(engobj, prog=prog):
                    for it in prog:
                        if it[0] == "wait":
                            engobj.wait_ge(it[1], it[2])
                        elif it[0] == "op":
                            it[1](engobj).then_inc(it[2], 1)
                        else:
                            engobj.dma_start(out=it[1], in_=it[2], **it[4]).then_inc(it[3], 16)

                getattr(block, engmap[e])(body)


PCOL = {}
_off = 0
for _n, _w in [("ln1g", 8), ("ln1b", 8), ("ln2g", 8), ("ln2b", 8), ("ln3g", 8), ("ln3b", 8),
               ("pool_scale", 2), ("conv_w", 62), ("conv_b", 2), ("conv_lng", 2), ("conv_lnb", 2),
               ("peT", 64)]:
    PCOL[_n] = _off
    _off += _w
NP = _off
CF = {"ident": 0, "triu": 128, "invw": 256, "invc": 258, "VW": 290, "CW": 546}
CF32_W = 802


class Builder:
    def __init__(self, T, depth, dbg=None):
        self.T = T
        self.depth = depth
        self.dbg = dbg or {}
        self.NT = T // TT
        self.NQ = T // 128
        self.nc = bass.Bass("TRN2", target_bir_lowering=False)
        self.uid = 0
        self.dumped = set()

    def sb(self, st, name, shape, dt):
        self.uid += 1
        return st.enter_context(self.nc.sbuf_tensor("%s_%d" % (name, self.uid), shape, dt))

    def din(self, name, shape, dt=F32):
        return self.nc.dram_tensor(name, list(shape), dt, kind="ExternalInput").ap()

    def dscr(self, name, shape, dt):
        return self.nc.dram_tensor(name, list(shape), dt, kind="Internal").ap()

    def mm(self, ps_ap, pskey, pairs):
        n = len(pairs)
        for i, (l, r, rd) in enumerate(pairs):
            self.em.op("pe", lambda e, l=l, r=r, i=i: e.matmul(ps_ap, lhsT=l, rhs=r, start=(i == 0), stop=(i == n - 1)),
                       reads=rd, writes=[pskey])

    def dump(self, name, ap, shape, dt, keys):
        if "dumps" not in self.dbg or name in self.dumped:
            return
        self.dumped.add(name)
        d = self.nc.dram_tensor("dbg_" + name, list(shape), dt, kind="ExternalOutput").ap()
        self.em.dma("sp", d, ap, reads=keys, writes=["dbg_" + name])

    def pbcopy(self, st, bank, keys):
        t = self.sb(st, "pbc", [128, 512], F32)
        self.em.op("dve", lambda e: e.memset(t[:], 0.0), writes=["pbc%d" % bank])
        n = 512 if bank == 3 else 260
        self.em.op("act", lambda e: e.copy(out=t[:, 0:n], in_=self.pb[bank][:, 0:n]), reads=keys, writes=["pbc%d" % bank])
        return t[:]

    def bank(self):
        b = self.rot[self.roti % len(self.rot)]
        self.roti += 1
        return b

    def ln_fm(self, r, rkey, nch, gcol, bcol, N, outs, func=AF.Identity):
        em = self.em
        W = self.lnw
        ones = self.ones_d[nch * 128]
        for c in range(nch):
            em.op("act", lambda e, c=c: e.activation(out=W["sq"][:, c, :N], in_=r[:, c, :N], func=AF.Square),
                  reads=[(rkey, c)], writes=[("ln_sq", c)])
            em.op("pool", lambda e, c=c: e.tensor_copy(out=W["rb"][:, c, :N], in_=r[:, c, :N]),
                  reads=[(rkey, c)], writes=[("ln_rb", c)])
        b0, b1 = self.bank(), self.bank()
        self.mm(self.pb[b0][:, :N], ("pb", b0), [(ones[:], W["rb"][:, c, :N], ["ones", ("ln_rb", c)]) for c in range(nch)])
        self.mm(self.pb[b1][:, :N], ("pb", b1), [(ones[:], W["sq"][:, c, :N], ["ones", ("ln_sq", c)]) for c in range(nch)])
        mean, rstd, tmp = W["mean"], W["rstd"], W["tmp"]
        em.op("dve", lambda e: e.tensor_copy(out=mean[:, :N], in_=self.pb[b0][:, :N]), reads=[("pb", b0)], writes=["ln_mean"])
        em.op("dve", lambda e: e.tensor_tensor(out=tmp[:, :N], in0=mean[:, :N], in1=mean[:, :N], op=ALU.mult), reads=["ln_mean"], writes=[("ln_tmp", 0)])
        em.op("dve", lambda e: e.tensor_tensor(out=rstd[:, :N], in0=self.pb[b1][:, :N], in1=tmp[:, :N], op=ALU.subtract),
              reads=[("pb", b1), ("ln_tmp", 0)], writes=["ln_rstd"])
        em.op("act", lambda e: e.activation(out=rstd[:, :N], in_=rstd[:, :N], func=AF.Sqrt, bias=self.eps_sb[:], scale=1.0),
              reads=["ln_rstd", "consts"], writes=["ln_rstd"])
        em.op("dve", lambda e: e.reciprocal(out=rstd[:, :N], in_=rstd[:, :N]), reads=["ln_rstd"], writes=["ln_rstd"])
        P = self.par
        for c in range(nch):
            tmpc = W["tmp"] if c % 2 == 0 else W["tmp2"]
            tk = ("ln_tmp", c % 2)
            em.op("dve", lambda e, c=c, tmpc=tmpc: e.tensor_tensor(out=tmpc[:, :N], in0=r[:, c, :N], in1=mean[:, :N], op=ALU.subtract),
                  reads=[(rkey, c), "ln_mean"], writes=[tk])
            em.op("dve", lambda e, c=c, tmpc=tmpc: e.tensor_tensor(out=tmpc[:, :N], in0=tmpc[:, :N], in1=rstd[:, :N], op=ALU.mult),
                  reads=[tk, "ln_rstd"], writes=[tk])
            if func != AF.Identity:
                em.op("act", lambda e, c=c, tmpc=tmpc: e.activation(out=tmpc[:, :N], in_=tmpc[:, :N], func=AF.Identity,
                                                                   scale=P[:, gcol + c:gcol + c + 1], bias=P[:, bcol + c:bcol + c + 1]),
                      reads=[tk, "par"], writes=[tk])
                for (ot, okey) in outs:
                    em.op("act", lambda e, c=c, ot=ot, tmpc=tmpc: e.activation(out=ot[:, c, :N], in_=tmpc[:, :N], func=func), reads=[tk], writes=[(okey, c)])
                continue
            for (ot, okey) in outs:
                em.op("act", lambda e, c=c, ot=ot, tmpc=tmpc: e.activation(out=ot[:, c, :N], in_=tmpc[:, :N], func=func,
                                                                          scale=P[:, gcol + c:gcol + c + 1], bias=P[:, bcol + c:bcol + c + 1]),
                      reads=[tk, "par"], writes=[(okey, c)])

    def build(self):
        nc = self.nc
        T, L = self.T, self.depth
        I = {}
        I["xT"] = self.din("xT", [D, T])
        I["memT"] = self.din("memT", [D, MEM])
        I["w_in"] = self.din("w_in", [DEPTH, D, D_IN])
        I["w_out"] = self.din("w_out", [DEPTH, D, D])
        I["xq_w"] = self.din("xq_w", [DEPTH, D, D])
        I["xkv_w"] = self.din("xkv_w", [DEPTH, D, 2 * D])
        I["xo_w"] = self.din("xo_w", [DEPTH, D, D])
        I["ffn_w13"] = self.din("ffn_w13", [2, D, 2 * D_FF])
        I["ffn_w2"] = self.din("ffn_w2", [2, D_FF, D])
        I["router_w"] = self.din("router_w", [2, D, NEXP])
        I["exp_w13"] = self.din("exp_w13", [2, NEXP, D, 2 * D_FFE])
        I["exp_w2"] = self.din("exp_w2", [2, NEXP, D_FFE, D])
        I["params"] = self.din("params", [DEPTH, 128, NP])
        for nm in ("cmp_k", "cmp_v"):
            I[nm + "_w1"] = self.din(nm + "_w1", [DEPTH, 2048, 256])
            I[nm + "_w2"] = self.din(nm + "_w2", [DEPTH, 256, 64])
        I["conv_pw"] = self.din("conv_pw", [DEPTH, 256, 256])
        I["pool_w"] = self.din("pool_w", [DEPTH, 4, 64, 64])
        I["sgu_wT"] = self.din("sgu_wT", [DEPTH, 4, 128, 128])
        I["sgu_b"] = self.din("sgu_b", [DEPTH, 4, 128])
        I["sgu_ln_g"] = self.din("sgu_ln_g", [DEPTH, 256])
        I["sgu_ln_b"] = self.din("sgu_ln_b", [DEPTH, 256])
        I["cf32"] = self.din("cf32", [128, CF32_W])
        NKT = T // 128
        self.CB = {"Gm": 0, "Mimp": 17 * 128, "Caus": 21 * 128, "Wlow": 22 * 128, "E": 23 * 128}
        self.CBW = (23 + NKT) * 128
        I["cb"] = self.din("cb", [128, self.CBW])
        self.I = I
        self.out = nc.dram_tensor("outT", [D, T], F32, kind="ExternalOutput").ap()
        self.XA = self.dscr("XA", [D, T], F32)
        self.XB = self.dscr("XB", [D, T], F32)
        if "dump_y" in self.dbg:
            self.Y = nc.dram_tensor("Y", [128, NCH, T], BF16, kind="ExternalOutput").ap()
        else:
            self.Y = self.dscr("Y", [128, NCH, T], BF16)
        self.QT = self.dscr("QT", [64, 4, T], BF16)
        if "y_in" in self.dbg:
            self.Yin = self.din("y_in", [128, NCH, T], BF16)

        with ExitStack() as st:
            self.em = em = Em(nc, st)
            self.pb = [st.enter_context(nc.psum_tensor("pb%d" % i, [128, 512], F32)) for i in range(8)]
            self.rot = list(range(8))
            self.roti = 0
            self.ones_d = {1024: self.sb(st, "ones1024", [128, 128], BF16), 256: self.sb(st, "ones256", [128, 128], BF16)}
            self.ones1 = self.sb(st, "ones1", [128, 128], BF16)
            self.eps_sb = self.sb(st, "eps", [128, 1], F32)
            self.cf = self.sb(st, "cf", [128, CF32_W], F32)
            em.dma("sp", self.cf[:], I["cf32"], writes=["ident"])
            self.ident = self.cf[:, 0:128]
            self.identb = self.sb(st, "identb", [128, 128], BF16)
            em.op("act", lambda e: e.copy(out=self.identb[:], in_=self.cf[:, 0:128]), reads=["ident"], writes=["identb"])
            em.op("dve", lambda e: e.memset(self.ones_d[1024][:], 1.0 / 1024), writes=["ones"])
            em.op("dve", lambda e: e.memset(self.ones_d[256][:], 1.0 / 256), writes=["ones"])
            em.op("dve", lambda e: e.memset(self.ones1[:], 1.0), writes=["ones"])
            em.op("dve", lambda e: e.memset(self.eps_sb[:], EPS), writes=["consts"])
            self.par = self.sb(st, "par", [128, NP], F32)
            for l in range(L):
                em.dma("sp", self.par[:], I["params"][l], writes=["par"])
                xcur = I["xT"] if l == 0 else self.XB
                xcur_key = "xT" if l == 0 else "XB"
                last = (l == L - 1)
                if "y_in" not in self.dbg:
                    with ExitStack() as stn:
                        NQ = self.NQ
                        N = {"KselT": self.sb(stn, "KselT", [64, T], BF16), "KwinT": self.sb(stn, "KwinT", [64, T], BF16),
                             "Vsel": self.sb(stn, "Vsel", [128, NQ, 65], BF16), "Vwin": self.sb(stn, "Vwin", [128, NQ, 65], BF16),
                             "gates": self.sb(stn, "gates", [128, NQ, 12], F32), "kcT": self.sb(stn, "kcT", [64, 512], BF16),
                             "vcT": self.sb(stn, "vcT", [64, 512], BF16), "vca": self.sb(stn, "vca", [128, 4, 65], BF16)}
                        self.phase_a(l, xcur, xcur_key, N)
                        em.barrier()
                        if "skip_b" not in self.dbg:
                            self.phase_b(l, N)
                            em.barrier()
                Ysrc = self.Yin if "y_in" in self.dbg else self.Y
                self.phase_c(l, xcur, xcur_key, Ysrc)
                em.barrier()
                self.phase_d(l, self.out if last else self.XB, "out" if last else "XB")
                em.barrier()
            em.barrier(engs=("sp",))
            em.replay()
        return nc

    def gelu(self, x_ap, tmp_ap, xkey, tkey):
        em = self.em
        if GELU_NATIVE:
            em.op("act", lambda e: e.activation(out=x_ap, in_=x_ap, func=AF.Gelu_apprx_tanh), reads=[xkey], writes=[xkey])
            return
        em.op("dve", lambda e: e.tensor_tensor(out=tmp_ap, in0=x_ap, in1=x_ap, op=ALU.mult), reads=[xkey], writes=[tkey])
        em.op("dve", lambda e: e.tensor_scalar(out=tmp_ap, in0=tmp_ap, scalar1=0.044715, scalar2=1.0, op0=ALU.mult, op1=ALU.add), reads=[tkey], writes=[tkey])
        em.op("dve", lambda e: e.tensor_tensor(out=tmp_ap, in0=tmp_ap, in1=x_ap, op=ALU.mult), reads=[xkey, tkey], writes=[tkey])
        em.op("act", lambda e: e.activation(out=tmp_ap, in_=tmp_ap, func=AF.Sigmoid, scale=1.5957691216057308), reads=[tkey], writes=[tkey])
        em.op("dve", lambda e: e.tensor_tensor(out=x_ap, in0=tmp_ap, in1=x_ap, op=ALU.mult), reads=[xkey, tkey], writes=[xkey])

    def phase_a(self, l, xcur, xkey, N):
        nc, em, I, T = self.nc, self.em, self.I, self.T
        P = PCOL
        par = self.par
        with ExitStack() as st:
            sb = lambda n, s, d: self.sb(st, n, s, d)
            win = sb("win", [128, NCH, D_IN], BF16)
            xbf = [sb("xbf0", [128, NCH, TT], BF16)]
            w1 = [sb("w1_%d" % i, [64, 32, 256], BF16) for i in range(2)]
            w2c = [sb("w2c_%d" % i, [128, 2, 64], BF16) for i in range(2)]
            peT = sb("peT", [64, 64], BF16)
            cb = sb("cb", [128, 4], F32)
            cmp_h = [sb("cmp_h%d" % i, [64, 16 + TT], BF16) for i in range(2)]
            hid = sb("hid", [128, 2, 32], BF16)
            hpre = sb("hpre", [128, 32], F32)
            htmp = sb("htmp", [128, 32], F32)
            a_h = sb("a_h", [128, 2, 16 + TT], F32)
            S1 = sb("S1", [128, 16 + TT], F32)
            S2 = sb("S2", [128, 16 + TT], F32)
            Ssel = sb("Ssel", [128, 16 + TT], F32)
            dpool = sb("dpool", [128, 2, TT], BF16)
            PW = sb("PW", [128, 2, 128], BF16)
            u_sb = sb("u_sb", [128, 2, TT], F32)
            gtmp = sb("gtmp", [128, TT if not GELU_NATIVE else 2], F32)
            h_h = sb("h_h", [128, 2, 32 + TT], BF16)
            Dg = sb("Dg", [128, 2, 31, 128], BF16)
            cacc = sb("cacc", [128, 2, TT], F32)
            hcb = sb("hcb", [128, 2, TT], BF16)
            pw = sb("pw", [128, 2, 256], BF16)
            self.lnw = {"sq": sb("lnsq", [128, 2, TT], BF16), "rb": sb("lnrb", [128, 2, TT], BF16),
                        "mean": sb("lnmean", [128, TT], F32), "rstd": sb("lnrstd", [128, TT], F32), "tmp": sb("lntmp", [128, TT], F32), "tmp2": sb("lntmp2", [128, TT], F32)}
            sgm = self.lnw["tmp2"]
            mtmp = self.lnw["tmp"][:, 0:128]
            ybuf = sb("ybuf", [128, NCH, TT], BF16)
            qst = sb("qst", [64, 4, TT], BF16)
            vg = sb("vg", [128, 256], F32)
            vt = sb("vt", [128, 256], F32)
            vs1 = sb("vs1", [128, 1], F32)
            vs2 = sb("vs2", [128, 1], F32)
            vpad = [[sb("vpad%d_%d" % (s4, i), [128, 2, 128], BF16) for i in range(2)] for s4 in range(4)]
            WsTf = Ssel[:, 0:512].rearrange("p (g i) -> p g i", g=4)
            WsT = sb("WsT", [128, 4, 128], BF16)
            Btab = sb("Btab", [128, 2, 128], F32)
            Gbc = sb("Gbc", [128, 256], F32)
            Bbc = sb("Bbc", [128, 256], F32)

            em.dma("pool", win[:], I["w_in"][l].rearrange("(c p) n -> p c n", p=128), writes=["win"])
            for i, nm in enumerate(("cmp_k", "cmp_v")):
                em.dma("pool", w1[i][:], I[nm + "_w1"][l].rearrange("(p d) n -> d p n", d=64), writes=[("w1", i)])
                em.dma("pool", w2c[i][:], I[nm + "_w2"][l].rearrange("(c p) n -> p c n", p=128), writes=[("w2c", i)])
            em.dma("pool", pw[:], I["conv_pw"][l].rearrange("(c p) n -> p c n", p=128), writes=["pw"])
            em.op("dve", lambda e: e.memset(PW[:], 0.0), writes=["PW"])
            for c in range(2):
                for k in range(31):
                    em.op("dve", lambda e, c=c, k=k: e.tensor_single_scalar(out=Dg[:, c, k, :], in_=self.identb[:], scalar=par[:, P["conv_w"] + c * 31 + k:P["conv_w"] + c * 31 + k + 1], op=ALU.mult),
                          reads=["identb", "par"], writes=["Dg"])
            for g in range(4):
                em.dma("pool", PW[(g % 2) * 64:(g % 2) * 64 + 64, g // 2, (g % 2) * 64:(g % 2) * 64 + 64], I["pool_w"][l, g], writes=["PW"])
            em.dma("sp", WsTf, I["sgu_wT"][l].rearrange("g j i -> j g i"), writes=["WsTf", "Ssel_lo", "Ssel_hi"])
            for g in range(4):
                em.op("dve", lambda e, g=g: e.tensor_tensor(out=WsT[:, g, :], in0=WsTf[:, g, :], in1=self.cf[:, CF["triu"]:CF["triu"] + 128], op=ALU.mult),
                      reads=["WsTf", "ident"], writes=["WsT"])
                em.dma("sp", Btab[(g % 2) * 64:(g % 2) * 64 + 64, g // 2, :], I["sgu_b"][l, g].partition_broadcast(64), writes=["Btab"])
            em.dma("sp", Gbc[:], I["sgu_ln_g"][l].partition_broadcast(128), writes=["Gbc"])
            em.dma("sp", Bbc[:], I["sgu_ln_b"][l].partition_broadcast(128), writes=["Gbc"])
            em.op("act", lambda e: e.copy(out=peT[:], in_=par[0:64, P["peT"]:P["peT"] + 64]), reads=["par"], writes=["peT"])
            for i in range(2):
                for hc in range(2):
                    b = self.bank()
                    self.mm(self.pb[b][:, 0:1], ("pb", b), [(w1[i][:, p, hc * 128:(hc + 1) * 128], peT[:, i * 32 + p:i * 32 + p + 1], [("w1", i), "peT"]) for p in range(32)])
                    em.op("act", lambda e, i=i, hc=hc, b=b: e.copy(out=cb[:, i * 2 + hc:i * 2 + hc + 1], in_=self.pb[b][:, 0:1]), reads=[("pb", b)], writes=["cb"])
            em.op("dve", lambda e: e.memset(a_h[:, :, 0:16], 0.0), writes=["a_halo"])
            em.op("dve", lambda e: e.memset(h_h[:, :, 0:32], 0.0), writes=["h_halo0", "h_halo1"])
            for i in range(2):
                em.op("dve", lambda e, i=i: e.memset(cmp_h[i][:, 0:16], 0.0), writes=[("cmp_halo", i)])
                for s4 in range(4):
                    em.op("dve", lambda e, i=i, s4=s4: e.memset(vpad[s4][i][:], 0.0), writes=[("vpad", s4, i)])
            em.op("dve", lambda e: e.memset(N["kcT"][:], 0.0), writes=["kcT"])
            em.op("dve", lambda e: e.memset(N["vcT"][:], 0.0), writes=["vcT"])
            em.op("dve", lambda e: e.memset(N["Vsel"][:, :, 64:65], 1.0), writes=["Vsel"])
            em.op("dve", lambda e: e.memset(N["Vwin"][:, :, 64:65], 1.0), writes=["Vwin"])
            em.op("dve", lambda e: e.memset(N["vca"][:, :, 64:65], 1.0), writes=["vca"])

            def fm(col, width, xb, xk):
                b = self.bank()
                self.mm(self.pb[b][0:width, :], ("pb", b), [(win[:, k, col:col + width], xb[:, k, :], ["win", xk]) for k in range(NCH)])
                return b

            for t in range(self.NT):
                sl = 0
                c0 = t * TT
                xb, xk = xbf[sl], ("xbf", sl)
                em.dma("pool", xb[:], xcur[:, c0:c0 + TT].rearrange("(c p) t -> p c t", p=128), reads=[(xkey, t)], writes=[xk])
                for pr in range(2):
                    b = fm(pr * 128, 128, xb, xk)
                    em.op("act", lambda e, pr=pr, b=b: e.copy(out=a_h[:, pr, 16:], in_=self.pb[b][:]), reads=[("pb", b)], writes=[("a_h", pr)])
                for pr in range(2):
                    A = a_h[:, pr, :]
                    rdA = [("a_h", pr), "a_halo"]
                    n = 16 + TT
                    lo, hi = slice(0, 64), slice(64, 128)
                    if pr == 0:
                        em.op("dve", lambda e, A=A: e.tensor_tensor(out=Ssel[lo, 1:n], in0=A[lo, 1:n], in1=A[lo, 0:n - 1], op=ALU.add), reads=rdA, writes=["Ssel_lo"])
                        em.op("dve", lambda e, A=A: e.tensor_tensor(out=S1[hi, 1:n], in0=A[hi, 1:n], in1=A[hi, 0:n - 1], op=ALU.add), reads=rdA, writes=["S1_hi"])
                        em.op("dve", lambda e: e.tensor_tensor(out=Ssel[hi, 3:n], in0=S1[hi, 3:n], in1=S1[hi, 1:n - 2], op=ALU.add), reads=["S1_hi"], writes=["Ssel_hi"])
                    else:
                        em.op("dve", lambda e, A=A: e.tensor_tensor(out=S1[:, 1:n], in0=A[:, 1:n], in1=A[:, 0:n - 1], op=ALU.add), reads=rdA, writes=["S1_hi", "S1_lo"])
                        em.op("dve", lambda e: e.tensor_tensor(out=S2[:, 3:n], in0=S1[:, 3:n], in1=S1[:, 1:n - 2], op=ALU.add), reads=["S1_hi", "S1_lo"], writes=["S2"])
                        em.op("dve", lambda e: e.tensor_tensor(out=Ssel[lo, 7:n], in0=S2[lo, 7:n], in1=S2[lo, 3:n - 4], op=ALU.add), reads=["S2"], writes=["Ssel_lo"])
                        em.op("dve", lambda e: e.tensor_tensor(out=S1[hi, 7:n], in0=S2[hi, 7:n], in1=S2[hi, 3:n - 4], op=ALU.add), reads=["S2"], writes=["S1_hi"])
                        em.op("dve", lambda e: e.tensor_tensor(out=Ssel[hi, 15:n], in0=S1[hi, 15:n], in1=S1[hi, 7:n - 8], op=ALU.add), reads=["S1_hi"], writes=["Ssel_hi"])
                    em.op("dve", lambda e, pr=pr, A=A: e.scalar_tensor_tensor(out=dpool[:, pr, :], in0=Ssel[:, 16:], scalar=self.cf[:, CF["invw"] + pr:CF["invw"] + pr + 1], in1=A[:, 16:], op0=ALU.mult, op1=ALU.subtract),
                          reads=["Ssel_lo", "Ssel_hi", "ident"] + rdA, writes=[("dpool", pr)])
                    if t == 0:
                        em.op("dve", lambda e, pr=pr: e.tensor_tensor(out=Ssel[:, 0:16], in0=Ssel[:, 16:32], in1=self.cf[:, CF["invc"] + pr * 16:CF["invc"] + pr * 16 + 16], op=ALU.mult),
                              reads=["Ssel_lo", "Ssel_hi", "ident", ("dpool", pr)], writes=["Ssel_lo", "Ssel_hi"])
                        em.op("dve", lambda e, pr=pr, A=A: e.tensor_tensor(out=dpool[:, pr, 0:16], in0=Ssel[:, 0:16], in1=A[:, 16:32], op=ALU.subtract),
                              reads=["Ssel_lo", "Ssel_hi", ("dpool", pr)] + rdA, writes=[("dpool", pr)])
                    b = self.bank()
                    self.mm(self.pb[b][:], ("pb", b), [(PW[:, pr, :], dpool[:, pr, :], ["PW", ("dpool", pr)])])
                    em.op("act", lambda e, pr=pr, b=b: e.activation(out=ybuf[:, pr, :], in_=self.pb[b][:], func=AF.Identity, scale=par[:, P["pool_scale"] + pr:P["pool_scale"] + pr + 1]),
                          reads=[("pb", b), "par"], writes=[("ybuf", pr)])
                em.op("dve", lambda e: e.tensor_copy(out=a_h[:, :, 0:16], in_=a_h[:, :, TT:TT + 16]), reads=[("a_h", 0), ("a_h", 1)], writes=["a_halo"])
                for pr in range(2):
                    b = fm(908 + pr * 128, 128, xb, xk)
                    em.op("act", lambda e, pr=pr, b=b: e.copy(out=u_sb[:, pr, :], in_=self.pb[b][:]), reads=[("pb", b)], writes=[("u_sb", pr)])
                    self.gelu(u_sb[:, pr, :], gtmp[:], ("u_sb", pr), "gtmp")
                self.dump("u_sb", u_sb[:], [128, 2, TT], F32, [("u_sb", 0), ("u_sb", 1)])
                for s4 in range(4):
                    ts = slice(s4 * 128, (s4 + 1) * 128)
                    b = self.bank()
                    self.mm(self.pb[b][:, 0:256], ("pb", b), [(xb[:, k, ts], win[:, k, 1164:1420], ["win", xk]) for k in range(NCH)])
                    em.op("act", lambda e, b=b: e.copy(out=vg[:], in_=self.pb[b][:, 0:256]), reads=[("pb", b)], writes=["vg"])
                    self.dump("vg_pre", vg[:], [128, 256], F32, ["vg"])
                    self.gelu(vg[:], vt[:], "vg", "vt")
                    self.dump("vg_gelu", vg[:], [128, 256], F32, ["vg"])
                    em.op("dve", lambda e: e.tensor_reduce(out=vs1[:], in_=vg[:], axis=AX.X, op=ALU.add), reads=["vg"], writes=["vs1"])
                    em.op("dve", lambda e: e.tensor_single_scalar(out=vs1[:], in_=vs1[:], scalar=-1.0 / 256, op=ALU.mult), reads=["vs1"], writes=["vs1"])
                    em.op("dve", lambda e: e.tensor_single_scalar(out=vg[:], in_=vg[:], scalar=vs1[:, 0:1], op=ALU.add), reads=["vg", "vs1"], writes=["vg"])
                    em.op("dve", lambda e: e.tensor_tensor(out=vt[:], in0=vg[:], in1=vg[:], op=ALU.mult), reads=["vg"], writes=["vt"])
                    em.op("dve", lambda e: e.tensor_reduce(out=vs2[:], in_=vt[:], axis=AX.X, op=ALU.add), reads=["vt"], writes=["vs2"])
                    em.op("dve", lambda e: e.tensor_scalar(out=vs2[:], in0=vs2[:], scalar1=1.0 / 256, scalar2=EPS, op0=ALU.mult, op1=ALU.add), reads=["vs2"], writes=["vs2"])
                    em.op("act", lambda e: e.activation(out=vs2[:], in_=vs2[:], func=AF.Sqrt), reads=["vs2"], writes=["vs2"])
                    em.op("dve", lambda e: e.reciprocal(out=vs2[:], in_=vs2[:]), reads=["vs2"], writes=["vs2"])
                    em.op("dve", lambda e: e.scalar_tensor_tensor(out=vg[:], in0=vg[:], scalar=vs2[:, 0:1], in1=Gbc[:], op0=ALU.mult, op1=ALU.mult), reads=["vg", "vs2", "Gbc"], writes=["vg"])
                    self.dump("vg_ln", vg[:], [128, 256], F32, ["vg"])
                    self.dump("vs2", vs2[:], [128, 1], F32, ["vs2"])
                    for g in range(4):
                        em.op("dve", lambda e, g=g, s4=s4: e.tensor_tensor(out=vpad[s4][g % 2][:, g // 2, (g % 2) * 64:(g % 2) * 64 + 64], in0=vg[:, g * 64:(g + 1) * 64], in1=Bbc[:, g * 64:(g + 1) * 64], op=ALU.add),
                              reads=["vg", "Gbc"], writes=[("vpad", s4, g % 2)])
                for h in range(4):
                    b = fm(256 + h * 64, 64, xb, xk)
                    em.op("act", lambda e, h=h, b=b: e.copy(out=qst[:, h, :], in_=self.pb[b][0:64, :]), reads=[("pb", b)], writes=["qst"])
                em.dma("sp", self.QT[:, :, c0:c0 + TT], qst[:], reads=["qst"], writes=[("QT", t)])
                for (col, dst, key) in ((640, N["KselT"], "KselT"), (768, N["KwinT"], "KwinT")):
                    b = fm(col, 64, xb, xk)
                    em.op("act", lambda e, dst=dst, b=b, c0=c0: e.copy(out=dst[:, c0:c0 + TT], in_=self.pb[b][0:64, :]), reads=[("pb", b)], writes=[(key, t)])
                for i in range(2):
                    b = fm(512 + i * 64, 64, xb, xk)
                    em.op("act", lambda e, i=i, b=b: e.copy(out=cmp_h[i][:, 16:], in_=self.pb[b][0:64, :]), reads=[("pb", b)], writes=[("cmp_h", i)])
                j0 = 1 if t == 0 else 0
                nb = 32 - j0
                col0 = 0 if t == 0 else 32 * t - 1
                for i in range(2):
                    for hc in range(2):
                        b = self.bank()
                        self.mm(self.pb[b][:, 0:nb], ("pb", b),
                                [(w1[i][:, p, hc * 128:(hc + 1) * 128], cmp_h[i][:, 16 * j0 + p:16 * j0 + p + 16 * (nb - 1) + 1:16], [("w1", i), ("cmp_h", i), ("cmp_halo", i)]) for p in range(32)])
                        em.op("dve", lambda e, i=i, hc=hc, b=b, nb=nb: e.tensor_single_scalar(out=hpre[:, 0:nb], in_=self.pb[b][:, 0:nb], scalar=cb[:, i * 2 + hc:i * 2 + hc + 1], op=ALU.add),
                              reads=[("pb", b), "cb"], writes=["hpre"])
                        self.gelu(hpre[:, 0:nb], htmp[:, 0:nb], "hpre", "htmp")
                        em.op("act", lambda e, hc=hc, nb=nb: e.copy(out=hid[:, hc, 0:nb], in_=hpre[:, 0:nb]), reads=["hpre"], writes=[("hid", hc)])
                    b = self.bank()
                    self.mm(self.pb[b][0:64, 0:nb], ("pb", b), [(w2c[i][:, hc, :], hid[:, hc, 0:nb], [("w2c", i), ("hid", hc)]) for hc in range(2)])
                    dst, key = (N["kcT"], "kcT") if i == 0 else (N["vcT"], "vcT")
                    em.op("act", lambda e, dst=dst, b=b, col0=col0, nb=nb: e.copy(out=dst[:, col0:col0 + nb], in_=self.pb[b][0:64, 0:nb]), reads=[("pb", b)], writes=[key])
                    em.op("dve", lambda e, i=i: e.tensor_copy(out=cmp_h[i][:, 0:16], in_=cmp_h[i][:, TT:TT + 16]), reads=[("cmp_h", i)], writes=[("cmp_halo", i)])
                for s4 in range(4):
                    qt = t * 4 + s4
                    ts = slice(s4 * 128, (s4 + 1) * 128)
                    b = self.bank()
                    self.mm(self.pb[b][:, 0:204], ("pb", b), [(xb[:, k, ts], win[:, k, 704:908], ["win", xk]) for k in range(NCH)])
                    em.op("act", lambda e, qt=qt, b=b: e.copy(out=N["Vsel"][:, qt, 0:64], in_=self.pb[b][:, 0:64]), reads=[("pb", b)], writes=["Vsel"])
                    em.op("act", lambda e, qt=qt, b=b: e.copy(out=N["Vwin"][:, qt, 0:64], in_=self.pb[b][:, 128:192]), reads=[("pb", b)], writes=["Vwin"])
                    em.op("act", lambda e, qt=qt, b=b: e.activation(out=N["gates"][:, qt, :], in_=self.pb[b][:, 192:204], func=AF.Sigmoid), reads=[("pb", b)], writes=["gates"])
                for c in range(2):
                    ba = fm(1420 + c * 128, 128, xb, xk)
                    bg = fm(1676 + c * 128, 128, xb, xk)
                    em.op("act", lambda e, bg=bg: e.activation(out=sgm[:], in_=self.pb[bg][:], func=AF.Sigmoid), reads=[("pb", bg)], writes=[("ln_tmp", 1)])
                    em.op("dve", lambda e, c=c, ba=ba: e.tensor_tensor(out=h_h[:, c, 32:], in0=self.pb[ba][:], in1=sgm[:], op=ALU.mult), reads=[("pb", ba), ("ln_tmp", 1)], writes=[("h_h", c)])
                    ceng = "dve"
                    b = self.bank()
                    self.mm(self.pb[b][:], ("pb", b), [(Dg[:, c, k, :], h_h[:, c, 2 + k:2 + k + TT], ["Dg", ("h_h", c), ("h_halo%d" % c)]) for k in range(31)])
                    em.op("act", lambda e, c=c, b=b: e.activation(out=cacc[:, c, :], in_=self.pb[b][:], func=AF.Identity, bias=par[:, P["conv_b"] + c:P["conv_b"] + c + 1], scale=1.0),
                          reads=[("pb", b), "par"], writes=[("cacc", c)])
                    em.op(ceng, lambda e, c=c: e.tensor_copy(out=h_h[:, c, 0:32], in_=h_h[:, c, TT:TT + 32]), reads=[("h_h", c)], writes=[("h_halo%d" % c)])
                self.dump("h_h", h_h[:], [128, 2, 32 + TT], BF16, [("h_h", 0), ("h_h", 1)])
                self.dump("cacc", cacc[:], [128, 2, TT], F32, [("cacc", 0), ("cacc", 1)])
                self.ln_fm(cacc, "cacc", 2, P["conv_lng"], P["conv_lnb"], TT, [(hcb, "hcb")], func=AF.Silu)
                self.dump("hcb", hcb[:], [128, 2, TT], BF16, [("hcb", 0), ("hcb", 1)])
                for oc in range(2):
                    b = self.bank()
                    self.mm(self.pb[b][:], ("pb", b), [(pw[:, k2, oc * 128:(oc + 1) * 128], hcb[:, k2, :], ["pw", ("hcb", k2)]) for k2 in range(2)])
                    em.op("act", lambda e, oc=oc, b=b: e.copy(out=ybuf[:, 6 + oc, :], in_=self.pb[b][:]), reads=[("pb", b)], writes=[("ybuf", 6 + oc)])
                for s4 in range(4):
                    ts = slice(s4 * 128, (s4 + 1) * 128)
                    for pr in range(2):
                        b = self.bank()
                        self.mm(self.pb[b][:, 0:128], ("pb", b), [(vpad[s4][hh][:, pr, :], WsT[:, 2 * pr + hh, :], [("vpad", s4, hh), "WsT"]) for hh in range(2)])
                        em.op("dve", lambda e, pr=pr, b=b: e.tensor_tensor(out=mtmp, in0=self.pb[b][:, 0:128], in1=Btab[:, pr, :], op=ALU.add), reads=[("pb", b), "Btab"], writes=[("ln_tmp", 0)])
                        em.op("dve", lambda e, pr=pr, ts=ts: e.tensor_tensor(out=ybuf[:, 4 + pr, ts], in0=mtmp, in1=u_sb[:, pr, ts], op=ALU.mult), reads=[("ln_tmp", 0), ("u_sb", pr)], writes=[("ybuf", 4 + pr)])
                em.dma("sp", self.Y[:, 0:2, c0:c0 + TT], ybuf[:, 0:2, :], reads=[("ybuf", 0), ("ybuf", 1)], writes=[("Y", t)])
                em.dma("sp", self.Y[:, 4:8, c0:c0 + TT], ybuf[:, 4:8, :], reads=[("ybuf", c) for c in range(4, 8)], writes=[("Y", t)])
            for kt in range(4):
                b = self.bank()
                self.mm(self.pb[b][:, 0:64], ("pb", b), [(N["vcT"][:, kt * 128:(kt + 1) * 128], self.identb[0:64, 0:64], ["vcT", "identb"])])
                em.op("act", lambda e, kt=kt, b=b: e.copy(out=N["vca"][:, kt, 0:64], in_=self.pb[b][:, 0:64]), reads=[("pb", b)], writes=["vca"])

    def phase_b(self, l, N):
        nc, em, I, T = self.nc, self.em, self.I, self.T
        NKT = T // 128
        CB = self.CB
        identb = self.identb
        with ExitStack() as st:
            sb = lambda n, s, d: self.sb(st, n, s, d)
            cbt = sb("cbt", [128, self.CBW], BF16)
            em.dma("pool", cbt[:], I["cb"], writes=["cbt"])
            qts = [sb("qts%d" % i, [64, 4, 128], BF16) for i in range(2)]
            PT = [sb("PT%d" % i, [128, 512], BF16) for i in range(4)]
            lsb = sb("lsb", [128, 3, 4], F32)
            coef = sb("coef", [128, 3, 4], F32)
            imps = sb("imps", [128, 128], F32)
            sc = sb("sc", [128, 128], F32)
            sc2 = sb("sc2", [128, 128], F32)
            top8 = sb("top8", [128, 8], F32)
            thr = sb("thr", [128, 1], F32)
            negsel = sb("negsel", [128, 128], BF16)
            negselT = sb("negselT", [128, 128], BF16)
            osb = sb("osb", [128, 256], F32)
            obf = sb("obf", [128, 256], BF16)
            ynsa = sb("ynsa", [128, 2, TT], BF16)
            ACC = {"c": 0, "s": 1, "w": 2}
            IMPB = 3
            self.rot = [4, 5, 6, 7]
            pti = [0]

            def bc(ap2d):
                return ap2d[:, None, :].broadcast_to([128, 4, 128])

            def pair(br, sl, kT_ap, kkeys, masks, v_ap, vkeys, first, last, imp_rhs=None):
                b = self.bank()
                S = self.pb[b][:].rearrange("p (h q) -> p h q", h=4)
                n = 1 + len(masks)
                em.op("pe", lambda e: e.matmul(S, lhsT=kT_ap, rhs=qts[sl][:], start=True, stop=(n == 1)), reads=kkeys + [("qts", sl)], writes=[("pb", b)])
                for mi, (ml, mr, mk) in enumerate(masks):
                    em.op("pe", lambda e, ml=ml, mr=mr, mi=mi: e.matmul(S, lhsT=ml, rhs=mr, start=False, stop=(mi == n - 2)), reads=mk, writes=[("pb", b)])
                ps = pti[0] % 4
                pti[0] += 1
                if self.cur_i == self.dbg.get("DI", -1):
                    t_ = self.sb(st, "Sd", [128, 512], F32)
                    em.op("dve", lambda e, b=b, t_=t_: e.tensor_copy(out=t_[:], in_=self.pb[b][:]), reads=[("pb", b)], writes=["Sd" + br])
                    self.dump("b_S" + br, t_[:], [128, 512], F32, ["Sd" + br])
                em.op("act", lambda e, ps=ps, b=b: e.activation(out=PT[ps][:], in_=self.pb[b][:], func=AF.Exp, scale=0.125), reads=[("pb", b)], writes=[("PT", ps)])
                if self.cur_i == self.dbg.get("DI", -1):
                    self.dump("b_PT" + br, PT[ps][:], [128, 512], BF16, [("PT", ps)])
                ab = ACC[br]

                def stage2():
                    em.op("pe", lambda e, ps=ps: e.matmul(self.pb[ab][0:65, :], lhsT=v_ap, rhs=PT[ps][:], start=first, stop=last),
                          reads=[("PT", ps)] + vkeys, writes=[("acc", br)])
                    if imp_rhs is not None:
                        for h in range(4):
                            em.op("pe", lambda e, h=h, ps=ps: e.matmul(self.pb[IMPB][:, h * 128:(h + 1) * 128], lhsT=PT[ps][:, h * 128:(h + 1) * 128], rhs=imp_rhs, start=(first and h == 0), stop=last, skip_group_check=True),
                                  reads=[("PT", ps), "cbt"], writes=["imp"])
                pending.append(stage2)
                while len(pending) > 2:
                    pending.pop(0)()

            pending = []

            def flush():
                while pending:
                    pending.pop(0)()

            oT = [sb("oT%d" % i_, [65, 512], F32) for i_ in range(3)]

            def finalize(br):
                ab = ACC[br]
                em.op("act", lambda e: e.copy(out=oT[ab][:], in_=self.pb[ab][0:65, :]), reads=[("acc", br)], writes=[("oT", ab)])
                for h in range(4):
                    em.op("pe", lambda e, h=h: e.matmul(self.pb[ab][:, h * 65:(h + 1) * 65], lhsT=oT[ab][:, h * 128:(h + 1) * 128], rhs=self.ident[0:65, 0:65], start=(h == 0), stop=(h == 3), skip_group_check=True),
                          reads=[("oT", ab), "ident"], writes=[("acc", br)])

            for i in range(self.NQ):
                self.cur_i = i
                DI = self.dbg.get("DI", -1)
                sl = i % 2
                q0 = i * 128
                em.dma("sp", qts[sl][:], self.QT[:, :, q0:q0 + 128], reads=[("QT", i // 4)], writes=[("qts", sl)])
                ktl = (8 * i + 6) // 128
                ip = i - 16 * ktl
                kts = list(range(ktl + 1))
                for kt in kts:
                    masks = []
                    off = 8 * i - 128 * kt
                    if off <= 128:
                        g = CB["Gm"] + (off // 8) * 128
                        masks.append((identb[:], bc(cbt[:, g:g + 128]), ["identb", "cbt"]))
                    m0 = CB["Mimp"] + kt * 128
                    pair("c", sl, N["kcT"][:, kt * 128:(kt + 1) * 128], ["kcT"], masks, N["vca"][:, kt, :], ["vca"], kt == 0, kt == kts[-1], imp_rhs=cbt[:, m0:m0 + 128])
                flush()
                finalize("c")
                accc = self.pb[0][:, 0:260].rearrange("p (h d) -> p h d", d=65)
                em.op("dve", lambda e: e.tensor_single_scalar(out=lsb[:, 0, :], in_=accc[:, :, 64], scalar=1e-30, op=ALU.max), reads=[("acc", "c")], writes=[("lsb", 0)])
                em.op("dve", lambda e: e.reciprocal(out=lsb[:, 0, :], in_=lsb[:, 0, :]), reads=[("lsb", 0)], writes=[("lsb", 0)])
                for h in range(4):
                    if h == 0:
                        em.op("dve", lambda e: e.tensor_single_scalar(out=imps[:], in_=self.pb[IMPB][:, 0:128], scalar=lsb[:, 0, 0:1], op=ALU.mult), reads=["imp", ("lsb", 0)], writes=["imps"])
                    else:
                        em.op("dve", lambda e, h=h: e.scalar_tensor_tensor(out=imps[:], in0=self.pb[IMPB][:, h * 128:(h + 1) * 128], scalar=lsb[:, 0, h:h + 1], in1=imps[:], op0=ALU.mult, op1=ALU.add),
                              reads=["imp", ("lsb", 0), "imps"], writes=["imps"])
                w0 = 126 - 2 * i
                em.op("dve", lambda e, w0=w0: e.tensor_tensor(out=sc[:], in0=imps[:], in1=self.cf[:, CF["CW"] + w0:CF["CW"] + w0 + 128], op=ALU.add), reads=["imps", "ident"], writes=["sc"])
                em.op("dve", lambda e: e.tensor_single_scalar(out=sc[:, 0:1], in_=sc[:, 0:1], scalar=1e4, op=ALU.add), reads=["sc"], writes=["sc"])
                em.op("dve", lambda e, w0=w0: e.tensor_tensor(out=sc[:], in0=sc[:], in1=self.cf[:, CF["VW"] + w0:CF["VW"] + w0 + 128], op=ALU.mult), reads=["sc", "ident"], writes=["sc"])
                em.op("dve", lambda e: e.max(out=top8[:], in_=sc[:]), reads=["sc"], writes=["top8"])
                em.op("dve", lambda e: e.match_replace(out=sc2[:], in_to_replace=top8[:], in_values=sc[:], imm_value=-1.0), reads=["sc", "top8"], writes=["sc2"])
                em.op("dve", lambda e: e.max(out=top8[:], in_=sc2[:]), reads=["sc2"], writes=["top8"])
                em.op("dve", lambda e: e.tensor_single_scalar(out=thr[:], in_=top8[:, 7:8], scalar=0.5, op=ALU.max), reads=["top8"], writes=["thr"])
                em.op("dve", lambda e: e.tensor_scalar(out=negsel[:], in0=sc[:], scalar1=thr[:, 0:1], scalar2=NEGM, op0=ALU.is_lt, op1=ALU.mult), reads=["sc", "thr"], writes=["negsel"])
                b = self.bank()
                em.op("pe", lambda e, b=b: e.matmul(self.pb[b][:, 0:128], lhsT=negsel[:], rhs=identb[:], start=True, stop=True), reads=["negsel", "identb"], writes=[("pb", b)])
                em.op("act", lambda e, b=b: e.copy(out=negselT[:], in_=self.pb[b][:, 0:128]), reads=[("pb", b)], writes=["negselT"])
                if i == DI:
                    self.dump("b_imps", imps[:], [128, 128], F32, ["imps"])
                    self.dump("b_sc", sc[:], [128, 128], F32, ["sc"])
                    self.dump("b_thr", thr[:], [128, 1], F32, ["thr"])
                    self.dump("b_negselT", negselT[:], [128, 128], BF16, ["negselT"])
                    self.dump("b_lsb", lsb[:], [128, 3, 4], F32, [("lsb", 0)])
                wk = list(range(max(0, i - 4), i + 1))
                for kt in wk:
                    masks = []
                    if kt == i - 4:
                        masks.append((identb[:], bc(cbt[:, CB["Wlow"]:CB["Wlow"] + 128]), ["identb", "cbt"]))
                    if kt == i:
                        masks.append((identb[:], bc(cbt[:, CB["Caus"]:CB["Caus"] + 128]), ["identb", "cbt"]))
                    pair("w", sl, N["KwinT"][:, kt * 128:(kt + 1) * 128], [("KwinT", kt // 4)], masks, N["Vwin"][:, kt, :], ["Vwin"], kt == wk[0], kt == wk[-1])
                DI = self.dbg.get("DI", -1)
                if i == DI:
                    self.dump("b_qts", qts[sl][:], [64, 4, 128], BF16, [("qts", sl)])
                    self.dump("b_KwinT", N["KwinT"][:], [64, T], BF16, [("KwinT", t_) for t_ in range(self.NT)])
                    self.dump("b_kcT", N["kcT"][:], [64, 512], BF16, ["kcT"])
                    self.dump("b_vca", N["vca"][:], [128, 4, 65], BF16, ["vca"])
                    self.dump("b_Vwin", N["Vwin"][:], [128, self.NQ, 65], BF16, ["Vwin"])
                    self.dump("b_accc", self.pbcopy(st, 0, [("acc", "c")]), [128, 512], F32, ["pbc0"])
                    self.dump("b_accw", self.pbcopy(st, 2, [("acc", "w")]), [128, 512], F32, ["pbc2"])
                    self.dump("b_imp", self.pbcopy(st, 3, ["imp"]), [128, 512], F32, ["pbc3"])
                for kt in range(i + 1):
                    e0 = CB["E"] + kt * 128
                    masks = [(cbt[:, e0:e0 + 128], bc(negselT[:]), ["cbt", "negselT"])]
                    if kt == i:
                        masks.append((identb[:], bc(cbt[:, CB["Caus"]:CB["Caus"] + 128]), ["identb", "cbt"]))
                    pair("s", sl, N["KselT"][:, kt * 128:(kt + 1) * 128], [("KselT", kt // 4)], masks, N["Vsel"][:, kt, :], ["Vsel"], kt == 0, kt == i)
                flush()
                finalize("w")
                finalize("s")
                for bi, br in enumerate(("s", "w")):
                    av = self.pb[ACC[br]][:, 0:260].rearrange("p (h d) -> p h d", d=65)
                    em.op("dve", lambda e, av=av, bi=bi: e.tensor_single_scalar(out=lsb[:, bi + 1, :], in_=av[:, :, 64], scalar=1e-30, op=ALU.max), reads=[("acc", br)], writes=[("lsb", bi + 1)])
                    em.op("dve", lambda e, bi=bi: e.reciprocal(out=lsb[:, bi + 1, :], in_=lsb[:, bi + 1, :]), reads=[("lsb", bi + 1)], writes=[("lsb", bi + 1)])
                gv = N["gates"][:, i, :].rearrange("p (h b) -> p b h", b=3)
                em.op("dve", lambda e, gv=gv: e.tensor_tensor(out=coef[:], in0=lsb[:], in1=gv, op=ALU.mult), reads=[("lsb", 0), ("lsb", 1), ("lsb", 2), "gates"], writes=["coef"])
                for h in range(4):
                    for bi, br in enumerate(("c", "s", "w")):
                        av = self.pb[ACC[br]][:, h * 65:h * 65 + 64]
                        oh = osb[:, h * 64:(h + 1) * 64]
                        if bi == 0:
                            em.op("dve", lambda e, av=av, oh=oh, bi=bi, h=h: e.tensor_single_scalar(out=oh, in_=av, scalar=coef[:, bi, h:h + 1], op=ALU.mult), reads=[("acc", br), "coef"], writes=[("osb", h)])
                        else:
                            em.op("dve", lambda e, av=av, oh=oh, bi=bi, h=h: e.scalar_tensor_tensor(out=oh, in0=av, scalar=coef[:, bi, h:h + 1], in1=oh, op0=ALU.mult, op1=ALU.add),
                                  reads=[("acc", br), "coef", ("osb", h)], writes=[("osb", h)])
                if i == DI:
                    self.dump("b_accs", self.pbcopy(st, 1, [("acc", "s")]), [128, 512], F32, ["pbc1"])
                    self.dump("b_coef", coef[:], [128, 3, 4], F32, ["coef"])
                    self.dump("b_osb", osb[:], [128, 256], F32, [("osb", h) for h in range(4)])
                em.op("act", lambda e: e.copy(out=obf[:], in_=osb[:]), reads=[("osb", h) for h in range(4)], writes=["obf"])
                for c in range(2):
                    b = self.bank()
                    em.op("pe", lambda e, c=c, b=b: e.matmul(self.pb[b][:, 0:128], lhsT=obf[:, c * 128:(c + 1) * 128], rhs=identb[:], start=True, stop=True), reads=["obf", "identb"], writes=[("pb", b)])
                    em.op("act", lambda e, c=c, b=b, i=i: e.copy(out=ynsa[:, c, (i % 4) * 128:(i % 4 + 1) * 128], in_=self.pb[b][:, 0:128]), reads=[("pb", b)], writes=["ynsa"])
                if i % 4 == 3:
                    t = i // 4
                    em.dma("sp", self.Y[:, 2:4, t * TT:(t + 1) * TT], ynsa[:], reads=["ynsa"], writes=[("Y", t)])
            self.rot = list(range(8))

    def phase_c(self, l, xcur, xkey, Ysrc):
        nc, em, I, T = self.nc, self.em, self.I, self.T
        with ExitStack() as st:
            sb = lambda n, s, d: self.sb(st, n, s, d)
            KxT = sb("KxT", [128, NCH, MEM], BF16)
            Vx = sb("Vx", [128, 2, D], BF16)

            def wload(dst, src, key):
                em.dma("pool", dst[:], src.rearrange("(c p) n -> p c n", p=128), writes=[key])
            with ExitStack() as st2:
                wkv = self.sb(st2, "wkv", [128, NCH, 2 * D], BF16)
                memT = self.sb(st2, "memT", [128, NCH, MEM], BF16)
                wload(wkv, I["xkv_w"][l], "wkv")
                wload(memT, I["memT"], "memT")
                for oc in range(NCH):
                    b = self.bank()
                    self.mm(self.pb[b][:, :MEM], ("pb", b), [(wkv[:, k, oc * 128:(oc + 1) * 128], memT[:, k, :], ["wkv", "memT"]) for k in range(NCH)])
                    em.op("act", lambda e, oc=oc, b=b: e.copy(out=KxT[:, oc, :], in_=self.pb[b][:, :MEM]), reads=[("pb", b)], writes=["KxT"])
                for mt in range(2):
                    for hf in range(2):
                        b = self.bank()
                        self.mm(self.pb[b][:], ("pb", b), [(memT[:, k, mt * 128:(mt + 1) * 128], wkv[:, k, D + hf * 512:D + (hf + 1) * 512], ["wkv", "memT"]) for k in range(NCH)])
                        em.op("act", lambda e, mt=mt, hf=hf, b=b: e.copy(out=Vx[:, mt, hf * 512:(hf + 1) * 512], in_=self.pb[b][:]), reads=[("pb", b)], writes=["Vx"])
            em.barrier()
            wout = sb("wout", [128, NCH, D], BF16)
            wq = sb("wq", [128, NCH, D], BF16)
            wo = sb("wo", [128, NCH, D], BF16)
            self.lnw = {"sq": sb("lnsq", [128, NCH, TT], BF16), "rb": sb("lnrb", [128, NCH, TT], BF16),
                        "mean": sb("lnmean", [128, TT], F32), "rstd": sb("lnrstd", [128, TT], F32), "tmp": sb("lntmp", [128, TT], F32), "tmp2": sb("lntmp2", [128, TT], F32)}
            ybf = [sb("ybf%d" % i, [128, NCH, TT], BF16) for i in range(2)]
            xs = [sb("xs%d" % i, [128, NCH, TT], F32) for i in range(2)]
            r = sb("r", [128, NCH, TT], F32)
            x1 = sb("x1", [128, NCH, TT], F32)
            qx = sb("qx", [128, NCH, TT], BF16)
            pT = sb("pT", [128, 2, TT], BF16)
            rl = sb("rl", [128, TT], F32)
            ox = sb("ox", [128, NCH, TT], BF16)
            x2 = sb("x2", [128, NCH, TT], F32)
            wload(wout, I["w_out"][l], "wout")
            wload(wq, I["xq_w"][l], "wq")
            wload(wo, I["xo_w"][l], "wo")

            P = PCOL
            for t in range(self.NT):
                sl = t % 2
                c0 = t * TT
                em.dma("sp", ybf[sl][:], Ysrc[:, :, c0:c0 + TT], reads=[("Y", t)], writes=[("ybf", sl)] + [(("x1b", sl), c) for c in range(NCH)])
                x1b = ybf[sl]
                em.dma("sp", xs[sl][:], xcur[:, c0:c0 + TT].rearrange("(c p) t -> p c t", p=128), reads=[(xkey, t)], writes=[("xs", sl)])
                for oc in range(NCH):
                    b = self.bank()
                    self.mm(self.pb[b][:], ("pb", b), [(wout[:, k, oc * 128:(oc + 1) * 128], ybf[sl][:, k, :], ["wout", ("ybf", sl)]) for k in range(NCH)])
                    em.op("dve", lambda e, oc=oc, b=b, sl=sl: e.scalar_tensor_tensor(out=r[:, oc, :], in0=xs[sl][:, oc, :], scalar=ALPHA, in1=self.pb[b][:], op0=ALU.mult, op1=ALU.add),
                          reads=[("xs", sl), ("pb", b)], writes=[("r", oc)])
                self.ln_fm(r, "r", NCH, P["ln1g"], P["ln1b"], TT, [(x1, "x1"), (x1b, ("x1b", sl))])
                for oc in range(NCH):
                    b = self.bank()
                    self.mm(self.pb[b][:], ("pb", b), [(wq[:, k, oc * 128:(oc + 1) * 128], x1b[:, k, :], ["wq", (("x1b", sl), k)]) for k in range(NCH)])
                    em.op("act", lambda e, oc=oc, b=b: e.copy(out=qx[:, oc, :], in_=self.pb[b][:]), reads=[("pb", b)], writes=[("qx", oc)])
                for h in range(4):
                    for mt in range(2):
                        b = self.bank()
                        self.mm(self.pb[b][:], ("pb", b), [(KxT[:, 2 * h + dc, mt * 128:(mt + 1) * 128], qx[:, 2 * h + dc, :], ["KxT", ("qx", 2 * h + dc)]) for dc in range(2)])
                        em.op("act", lambda e, mt=mt, b=b: e.activation(out=pT[:, mt, :], in_=self.pb[b][:], func=AF.Exp, scale=1.0 / 16.0),
                              reads=[("pb", b)], writes=[("pT", mt)])
                    b = self.bank()
                    self.mm(self.pb[b][:], ("pb", b), [(self.ones1[:], pT[:, mt, :], ["ones", ("pT", mt)]) for mt in range(2)])
                    em.op("dve", lambda e, b=b: e.reciprocal(out=rl[:], in_=self.pb[b][:]), reads=[("pb", b)], writes=["rl"])
                    for dc in range(2):
                        b = self.bank()
                        self.mm(self.pb[b][:], ("pb", b), [(Vx[:, mt, h * 256 + dc * 128:h * 256 + (dc + 1) * 128], pT[:, mt, :], ["Vx", ("pT", mt)]) for mt in range(2)])
                        em.op("dve", lambda e, h=h, dc=dc, b=b: e.tensor_tensor(out=ox[:, 2 * h + dc, :], in0=self.pb[b][:], in1=rl[:], op=ALU.mult),
                              reads=[("pb", b), "rl"], writes=[("ox", 2 * h + dc)])
                for oc in range(NCH):
                    b = self.bank()
                    self.mm(self.pb[b][:], ("pb", b), [(wo[:, k, oc * 128:(oc + 1) * 128], ox[:, k, :], ["wo", ("ox", k)]) for k in range(NCH)])
                    em.op("dve", lambda e, oc=oc, b=b: e.scalar_tensor_tensor(out=r[:, oc, :], in0=x1[:, oc, :], scalar=ALPHA, in1=self.pb[b][:], op0=ALU.mult, op1=ALU.add),
                          reads=[("x1", oc), ("pb", b)], writes=[("r", oc)])
                self.ln_fm(r, "r", NCH, P["ln2g"], P["ln2b"], TT, [(x2, "x2")])
                em.dma("sp", self.XA[:, c0:c0 + TT].rearrange("(c p) t -> p c t", p=128), x2[:], reads=[("x2", c) for c in range(NCH)], writes=[("XA", t)])

    def phase_d(self, l, xdst, dkey):
        nc, em, I, T = self.nc, self.em, self.I, self.T
        moe = (l % 2 == 1)
        li = l // 2
        ST = min(1024 if moe else 2048, T)
        nsub = ST // TT
        GC = 4
        with ExitStack() as st:
            sb = lambda n, s, d: self.sb(st, n, s, d)
            self.lnw = {"sq": sb("lnsq", [128, NCH, TT], BF16), "rb": sb("lnrb", [128, NCH, TT], BF16),
                        "mean": sb("lnmean", [128, TT], F32), "rstd": sb("lnrstd", [128, TT], F32), "tmp": sb("lntmp", [128, TT], F32), "tmp2": sb("lntmp2", [128, TT], F32)}
            xb = sb("xb", [128, NCH, ST], BF16)
            acc = sb("acc", [128, NCH, ST], F32)
            w13 = [sb("w13_%d" % i, [128, NCH, 2, GC * 128], BF16) for i in range(2)]
            w2 = [sb("w2_%d" % i, [128, GC, D], BF16) for i in range(2)]
            hT = sb("hT", [128, GC, TT], BF16)
            sg = sb("sg", [128, TT], F32)
            xf = None if moe else sb("xf", [128, NCH, TT], F32)
            if moe:
                xsc = sb("xsc", [128, NCH, ST], BF16)
                rw = sb("rw", [128, NCH, NEXP], F32)
                xf32 = sb("xf32", [128, NCH, ST], F32)
                lg = sb("lg", [128, ST // 128, NEXP], F32)
                top = sb("top", [128, 8], F32)
                nm1 = sb("nm1", [128, 1], F32)
                den = sb("den", [128, 1], F32)
                gate = sb("gate", [128, ST // 128, NEXP], F32)
                gbc = sb("gbc", [128, TT], F32)
                em.dma("sp", rw[:], I["router_w"][li].rearrange("(c p) n -> p c n", p=128), writes=["rw"])
            nff = (D_FFE if moe else D_FF) // 128
            groups = [(g0, min(GC, nff - g0)) for g0 in range(0, nff, GC)]
            FF = D_FFE if moe else D_FF
            gi = 0
            for s0 in range(0, T, ST):
                tiles = list(range(s0 // TT, (s0 + ST) // TT))
                em.dma("pool", xb[:], self.XA[:, s0:s0 + ST].rearrange("(c p) t -> p c t", p=128), reads=[("XA", t) for t in tiles], writes=["xb"])
                if moe:
                    em.dma("sp", xf32[:], self.XA[:, s0:s0 + ST].rearrange("(c p) t -> p c t", p=128), reads=[("XA", t) for t in tiles], writes=["xf32"])
                    for j in range(ST // 128):
                        b = self.bank()
                        self.mm(self.pb[b][:, :NEXP], ("pb", b), [(xf32[:, k, j * 128:(j + 1) * 128], rw[:, k, :], ["xf32", "rw"]) for k in range(NCH)])
                        em.op("dve", lambda e, j=j, b=b: e.tensor_copy(out=lg[:, j, :], in_=self.pb[b][:, :NEXP]), reads=[("pb", b)], writes=["lg"])
                        em.op("dve", lambda e, j=j: e.max(out=top[:], in_=lg[:, j, :]), reads=["lg"], writes=["top"])
                        em.op("dve", lambda e: e.tensor_single_scalar(out=nm1[:], in_=top[:, 0:1], scalar=-1.0, op=ALU.mult), reads=["top"], writes=["nm1"])
                        em.op("act", lambda e, j=j: e.activation(out=gate[:, j, :], in_=lg[:, j, :], func=AF.Exp, bias=nm1[:], scale=1.0), reads=["lg", "nm1"], writes=["gate"])
                        em.op("dve", lambda e, j=j: e.scalar_tensor_tensor(out=gate[:, j, :], in0=lg[:, j, :], scalar=top[:, 1:2], in1=gate[:, j, :], op0=ALU.is_ge, op1=ALU.mult),
                              reads=["lg", "top", "gate"], writes=["gate"])
                        em.op("dve", lambda e, j=j: e.tensor_reduce(out=den[:], in_=gate[:, j, :], axis=AX.X, op=ALU.add), reads=["gate"], writes=["den"])
                        em.op("dve", lambda e: e.reciprocal(out=den[:], in_=den[:]), reads=["den"], writes=["den"])
                        em.op("dve", lambda e, j=j: e.tensor_single_scalar(out=gate[:, j, :], in_=gate[:, j, :], scalar=den[:, 0:1], op=ALU.mult), reads=["gate", "den"], writes=["gate"])
                for ex in range(NEXP if moe else 1):
                    if moe:
                        w13src, w2src = I["exp_w13"][li, ex], I["exp_w2"][li, ex]
                        for su in range(nsub):
                            b = self.bank()
                            for jj in range(4):
                                j = su * 4 + jj
                                em.op("pe", lambda e, j=j, jj=jj, b=b, ex=ex: e.matmul(self.pb[b][:, jj * 128:(jj + 1) * 128], lhsT=gate[:, j, ex:ex + 1].broadcast_to([128, 128]), rhs=self.ident, start=True, stop=True),
                                      reads=["gate", "ident"], writes=[("pb", b)])
                            em.op("act", lambda e, b=b: e.copy(out=gbc[:], in_=self.pb[b][:]), reads=[("pb", b)], writes=["gbc"])
                            for c in range(NCH):
                                em.op("dve", lambda e, c=c, su=su: e.tensor_tensor(out=xsc[:, c, su * TT:(su + 1) * TT], in0=xf32[:, c, su * TT:(su + 1) * TT], in1=gbc[:], op=ALU.mult),
                                      reads=["xf32", "gbc"], writes=[("xsc", su)])
                    else:
                        w13src, w2src = I["ffn_w13"][li], I["ffn_w2"][li]
                    for (g0, gn) in groups:
                        ws = gi % 2
                        gi += 1
                        for hf in range(2):
                            em.dma("pool", w13[ws][:, :, hf, :gn * 128], w13src[:, hf * FF + g0 * 128:hf * FF + (g0 + gn) * 128].rearrange("(c p) n -> p c n", p=128), writes=[("w13", ws)])
                        em.dma("pool", w2[ws][:, :gn, :], w2src[g0 * 128:(g0 + gn) * 128, :].rearrange("(c p) n -> p c n", p=128), writes=[("w2", ws)])
                        for su in range(nsub):
                            ts = slice(su * TT, (su + 1) * TT)
                            for j in range(gn):
                                bg, bu = self.bank(), self.bank()
                                self.mm(self.pb[bg][:], ("pb", bg), [(w13[ws][:, k, 0, j * 128:(j + 1) * 128], xb[:, k, ts], [("w13", ws), "xb"]) for k in range(NCH)])
                                usrc = xsc if moe else xb
                                ukey = ("xsc", su) if moe else "xb"
                                self.mm(self.pb[bu][:], ("pb", bu), [(w13[ws][:, k, 1, j * 128:(j + 1) * 128], usrc[:, k, ts], [("w13", ws), ukey]) for k in range(NCH)])
                                em.op("act", lambda e, bg=bg: e.activation(out=sg[:], in_=self.pb[bg][:], func=AF.Silu), reads=[("pb", bg)], writes=["sg"])
                                em.op("dve", lambda e, j=j, bu=bu: e.tensor_tensor(out=hT[:, j, :], in0=self.pb[bu][:], in1=sg[:], op=ALU.mult), reads=[("pb", bu), "sg"], writes=[("hT", j)])
                            first = (ex == 0 and g0 == 0)
                            for oc in range(NCH):
                                b = self.bank()
                                self.mm(self.pb[b][:], ("pb", b), [(w2[ws][:, j, oc * 128:(oc + 1) * 128], hT[:, j, :], [("w2", ws), ("hT", j)]) for j in range(gn)])
                                if first:
                                    em.op("act", lambda e, oc=oc, b=b, ts=ts: e.copy(out=acc[:, oc, ts], in_=self.pb[b][:]), reads=[("pb", b)], writes=[("acc", su, oc)])
                                else:
                                    em.op("dve", lambda e, oc=oc, b=b, ts=ts: e.tensor_tensor(out=acc[:, oc, ts], in0=acc[:, oc, ts], in1=self.pb[b][:], op=ALU.add),
                                          reads=[("pb", b), ("acc", su, oc)], writes=[("acc", su, oc)])
                P = PCOL
                for su in range(nsub):
                    t = s0 // TT + su
                    c0 = t * TT
                    ts = slice(su * TT, (su + 1) * TT)
                    if moe:
                        xv, xk = xf32[:, :, ts], "xf32"
                    else:
                        em.dma("sp", xf[:], self.XA[:, c0:c0 + TT].rearrange("(c p) t -> p c t", p=128), reads=[("XA", t)], writes=["xf32"])
                        xv, xk = xf[:], "xf32"
                    rv = acc[:, :, ts]
                    for oc in range(NCH):
                        em.op("dve", lambda e, oc=oc, xv=xv, rv=rv: e.scalar_tensor_tensor(out=rv[:, oc, :], in0=xv[:, oc, :], scalar=ALPHA, in1=rv[:, oc, :], op0=ALU.mult, op1=ALU.add),
                              reads=[xk, ("acc", su, oc)], writes=[("acc", su, oc), ("rr", oc)])
                    self.ln_fm(rv, "rr", NCH, P["ln3g"], P["ln3b"], TT, [(xv, "x3")])
                    em.dma("sp", xdst[:, c0:c0 + TT].rearrange("(c p) t -> p c t", p=128), xv, reads=[("x3", c) for c in range(NCH)] + [xk], writes=[(dkey, t), xk])


def _chunkcols(v, nch):
    return np.ascontiguousarray(np.asarray(v, np.float32).reshape(nch, 128).T)


def pack_params(inp):
    P = np.zeros((DEPTH, 128, NP), np.float32)
    for l in range(DEPTH):
        for n, src in (("ln1g", "ln1_g"), ("ln1b", "ln1_b"), ("ln2g", "ln2_g"), ("ln2b", "ln2_b"), ("ln3g", "ln3_g"), ("ln3b", "ln3_b")):
            P[l, :, PCOL[n]:PCOL[n] + 8] = _chunkcols(inp[src][l], 8)
        P[l, :, PCOL["pool_scale"]:PCOL["pool_scale"] + 2] = _chunkcols(inp["pool_scale"][l], 2)
        cw = np.asarray(inp["conv_w"][l], np.float32)
        for c in range(2):
            P[l, :, PCOL["conv_w"] + c * 31:PCOL["conv_w"] + (c + 1) * 31] = cw[:, c * 128:(c + 1) * 128].T
        P[l, :, PCOL["conv_b"]:PCOL["conv_b"] + 2] = _chunkcols(inp["conv_b"][l], 2)
        P[l, :, PCOL["conv_lng"]:PCOL["conv_lng"] + 2] = _chunkcols(inp["conv_ln_g"][l], 2)
        P[l, :, PCOL["conv_lnb"]:PCOL["conv_lnb"] + 2] = _chunkcols(inp["conv_ln_b"][l], 2)
        P[l, 0:64, PCOL["peT"]:PCOL["peT"] + 32] = np.asarray(inp["cmp_pe_k"][l], np.float32).T
        P[l, 0:64, PCOL["peT"] + 32:PCOL["peT"] + 64] = np.asarray(inp["cmp_pe_v"][l], np.float32).T
    return P


def const_f32():
    c = np.zeros((128, CF32_W), np.float32)
    c[:, 0:128] = np.eye(128, dtype=np.float32)
    c[:, 128:256] = np.triu(np.ones((128, 128), np.float32))
    wins = (2, 4, 8, 16)
    for p in range(128):
        for pr in range(2):
            w = wins[2 * pr + (1 if p >= 64 else 0)]
            c[p, CF["invw"] + pr] = 1.0 / w
            for t in range(16):
                c[p, CF["invc"] + pr * 16 + t] = 1.0 / min(t + 1, w)
        cq = 1 if p >= 64 else 0
        for m in range(256):
            rel = m - 126
            c[p, CF["VW"] + m] = 1.0 if rel <= cq else 0.0
            c[p, CF["CW"] + m] = 1.0 + (1e4 if (rel == cq or rel == cq - 1) else 0.0)
    return c


def const_cb(T):
    NKT = T // 128
    cb = np.zeros((128, (23 + NKT) * 128), np.float32)
    n = np.arange(128)[:, None]
    q = np.arange(128)[None, :]
    for oi in range(17):
        off = 8 * oi
        cb[:, oi * 128:(oi + 1) * 128] = np.where(16 * (n - off) + 31 <= q, 0.0, NEGM)
    for kt in range(4):
        nn = 128 * kt + n
        j = q
        cb[:, (17 + kt) * 128:(18 + kt) * 128] = ((nn >= 4 * j - 1) & (nn <= 4 * j + 3)).astype(np.float32)
    cb[:, 21 * 128:22 * 128] = np.where(n > q, NEGM, 0.0)
    cb[:, 22 * 128:23 * 128] = np.where(n <= q, NEGM, 0.0)
    for kt in range(NKT):
        cb[:, (23 + kt) * 128:(24 + kt) * 128] = (n == 2 * kt + q // 64).astype(np.float32)
    return cb


def core_inputs(inp, b):
    m = {}
    m["xT"] = np.ascontiguousarray(np.asarray(inp["x"][b], np.float32).T)
    m["memT"] = np.ascontiguousarray(np.asarray(inp["mem"][b], np.float32).T)
    for k in ("w_in", "w_out", "xq_w", "xkv_w", "xo_w", "ffn_w13", "ffn_w2", "router_w", "exp_w13", "exp_w2",
              "cmp_k_w1", "cmp_k_w2", "cmp_v_w1", "cmp_v_w2", "conv_pw", "pool_w", "sgu_b", "sgu_ln_g", "sgu_ln_b"):
        m[k] = np.ascontiguousarray(np.asarray(inp[k], np.float32))
    m["sgu_wT"] = np.ascontiguousarray(np.asarray(inp["sgu_w"], np.float32).transpose(0, 1, 3, 2))
    return m


_CACHE = {}


def kernel(**inp):
    T = 8192
    if "nc" not in _CACHE:
        _CACHE["nc"] = Builder(T, DEPTH).build()
    nc = _CACHE["nc"]
    params = pack_params(inp)
    cf = const_f32()
    cb = const_cb(T)
    in_maps = []
    for c in range(4):
        m = core_inputs(inp, c)
        m["params"] = params
        m["cf32"] = cf
        m["cb"] = cb
        in_maps.append(m)
    res = run_bass_kernel_spmd(nc, in_maps, core_ids=[0, 1, 2, 3])
    out = np.stack([np.ascontiguousarray(np.asarray(res.results[c]["outT"]).T) for c in range(4)])
    return out.astype(np.float32)
```

```python
import numpy as np
from contextlib import ExitStack
import concourse.bass as bass
import concourse.mybir as mybir
from concourse.bass_utils import run_bass_kernel_spmd

F32 = mybir.dt.float32
BF16 = mybir.dt.bfloat16
ALU = mybir.AluOpType
AF = mybir.ActivationFunctionType
AX = mybir.AxisListType

D = 1024
NCH = 8
DEPTH = 4
MEM = 256
D_IN = 1932
D_FF = 2816
D_FFE = 3584
NEXP = 8
ALPHA = (2 * DEPTH) ** 0.25
EPS = 1e-5
NEGM = -30000.0
TT = 512
GELU_NATIVE = True

ENGS = ("pe", "act", "dve", "pool", "sp")
N_DMA_SEMS = 40


class Em:
    def __init__(self, nc, stack):
        self.nc = nc
        self.prog = {e: [] for e in ENGS}
        self.cnt = {e: 0 for e in ENGS}
        self.waited = {e: {} for e in ENGS}
        self.res = {}
        self.sem = {e: stack.enter_context(nc.semaphore("s_" + e)) for e in ENGS}
        self.dsem = [stack.enter_context(nc.semaphore("d%d" % i)) for i in range(N_DMA_SEMS)]
        self.ndma = 0
        self.nwaits = 0
        self.dlast = {}

    def _st(self, key):
        st = self.res.get(key)
        if st is None:
            st = {"w": None, "r": {}}
            self.res[key] = st
        return st

    def _need(self, eng, tok, same_ok):
        if tok is None:
            return
        if tok[0] == "e":
            _, e2, k = tok
            if e2 == eng and same_ok:
                return
            if self.waited[eng].get(e2, 0) >= k:
                return
            self.waited[eng][e2] = k
            self.prog[eng].append(("wait", self.sem[e2], k))
            self.nwaits += 1
        else:
            _, si, target = tok
            if self.waited[eng].get(("d", si), 0) >= target:
                return
            self.waited[eng][("d", si)] = target
            self.prog[eng].append(("wait", self.dsem[si], target))
            self.nwaits += 1

    def _deps(self, eng, reads, writes):
        for r in reads:
            self._need(eng, self._st(r)["w"], same_ok=(eng == "pe"))
        for w in writes:
            st = self._st(w)
            self._need(eng, st["w"], same_ok=True)
            for tok in st["r"].values():
                self._need(eng, tok, same_ok=True)

    def _commit(self, tok, rkey, reads, writes):
        for r in reads:
            self._st(r)["r"][rkey] = tok
        for w in writes:
            st = self._st(w)
            st["w"] = tok
            st["r"] = {}

    @staticmethod
    def _excl(reads, writes):
        ex = [k for k in reads if (isinstance(k, tuple) and k[0] in ("pb", "acc")) or k == "imp"]
        if not ex:
            return reads, writes
        return [k for k in reads if k not in ex], list(writes) + [k for k in ex if k not in writes]

    def op(self, eng, fn, reads=(), writes=()):
        reads, writes = self._excl(list(reads), list(writes))
        self._deps(eng, reads, writes)
        self.cnt[eng] += 1
        tok = ("e", eng, self.cnt[eng])
        self.prog[eng].append(("op", fn, self.sem[eng]))
        self._commit(tok, eng, reads, writes)
        return tok

    def dma(self, q, out, in_, reads=(), writes=(), **kw):
        k = self.ndma
        self.ndma += 1
        si = k % N_DMA_SEMS
        target = 16 * (k // N_DMA_SEMS + 1)
        if target > 16:
            self._need(q, ("d", si, target - 16), same_ok=False)
        self._deps(q, reads, writes)
        tok = ("d", si, target)
        self.dlast[si] = tok
        self.prog[q].append(("dma", out, in_, self.dsem[si], kw))
        self._commit(tok, ("dq", si), reads, writes)
        return tok

    def barrier(self, engs=ENGS):
        for e in engs:
            for e2 in ENGS:
                if e2 != e and self.cnt[e2] > 0:
                    self._need(e, ("e", e2, self.cnt[e2]), same_ok=False)
            for tok in self.dlast.values():
                self._need(e, tok, same_ok=False)

    def replay(self):
        nc = self.nc
        engmap = {"pe": "tensor", "act": "scalar", "dve": "vector", "pool": "gpsimd", "sp": "sync"}
        with nc.Block() as block:
            for e in ENGS:
                prog = self.prog[e]

                def body(engobj, prog=prog):
                    for it in prog:
                        if it[0] == "wait":
                            engobj.wait_ge(it[1], it[2])
                        elif it[0] == "op":
                            it[1](engobj).then_inc(it[2], 1)
                        else:
                            engobj.dma_start(out=it[1], in_=it[2], **it[4]).then_inc(it[3], 16)

                getattr(block, engmap[e])(body)


PCOL = {}
_off = 0
for _n, _w in [("ln1g", 8), ("ln1b", 8), ("ln2g", 8), ("ln2b", 8), ("ln3g", 8), ("ln3b", 8),
               ("pool_scale", 2), ("conv_w", 62), ("conv_b", 2), ("conv_lng", 2), ("conv_lnb", 2),
               ("peT", 64)]:
    PCOL[_n] = _off
    _off += _w
NP = _off
CF = {"ident": 0, "triu": 128, "invw": 256, "invc": 258, "VW": 290, "CW": 546}
CF32_W = 802


class Builder:
    def __init__(self, T, depth, dbg=None):
        self.T = T
        self.depth = depth
        self.dbg = dbg or {}
        self.NT = T // TT
        self.NQ = T // 128
        self.nc = bass.Bass("TRN2", target_bir_lowering=False)
        self.uid = 0
        self.dumped = set()

    def sb(self, st, name, shape, dt):
        self.uid += 1
        return st.enter_context(self.nc.sbuf_tensor("%s_%d" % (name, self.uid), shape, dt))

    def din(self, name, shape, dt=F32):
        return self.nc.dram_tensor(name, list(shape), dt, kind="ExternalInput").ap()

    def dscr(self, name, shape, dt):
        return self.nc.dram_tensor(name, list(shape), dt, kind="Internal").ap()

    def mm(self, ps_ap, pskey, pairs):
        n = len(pairs)
        for i, (l, r, rd) in enumerate(pairs):
            self.em.op("pe", lambda e, l=l, r=r, i=i: e.matmul(ps_ap, lhsT=l, rhs=r, start=(i == 0), stop=(i == n - 1)),
                       reads=rd, writes=[pskey])

    def dump(self, name, ap, shape, dt, keys):
        if "dumps" not in self.dbg or name in self.dumped:
            return
        self.dumped.add(name)
        d = self.nc.dram_tensor("dbg_" + name, list(shape), dt, kind="ExternalOutput").ap()
        self.em.dma("sp", d, ap, reads=keys, writes=["dbg_" + name])

    def pbcopy(self, st, bank, keys):
        t = self.sb(st, "pbc", [128, 512], F32)
        self.em.op("dve", lambda e: e.memset(t[:], 0.0), writes=["pbc%d" % bank])
        n = 512 if bank == 3 else 260
        self.em.op("act", lambda e: e.copy(out=t[:, 0:n], in_=self.pb[bank][:, 0:n]), reads=keys, writes=["pbc%d" % bank])
        return t[:]

    def bank(self):
        b = self.rot[self.roti % len(self.rot)]
        self.roti += 1
        return b

    def ln_fm(self, r, rkey, nch, gcol, bcol, N, outs, func=AF.Identity):
        em = self.em
        W = self.lnw
        ones = self.ones_d[nch * 128]
        for c in range(nch):
            em.op("act", lambda e, c=c: e.activation(out=W["sq"][:, c, :N], in_=r[:, c, :N], func=AF.Square),
                  reads=[(rkey, c)], writes=[("ln_sq", c)])
            em.op("pool", lambda e, c=c: e.tensor_copy(out=W["rb"][:, c, :N], in_=r[:, c, :N]),
                  reads=[(rkey, c)], writes=[("ln_rb", c)])
        b0, b1 = self.bank(), self.bank()
        self.mm(self.pb[b0][:, :N], ("pb", b0), [(ones[:], W["rb"][:, c, :N], ["ones", ("ln_rb", c)]) for c in range(nch)])
        self.mm(self.pb[b1][:, :N], ("pb", b1), [(ones[:], W["sq"][:, c, :N], ["ones", ("ln_sq", c)]) for c in range(nch)])
        mean, rstd, tmp = W["mean"], W["rstd"], W["tmp"]
        em.op("dve", lambda e: e.tensor_copy(out=mean[:, :N], in_=self.pb[b0][:, :N]), reads=[("pb", b0)], writes=["ln_mean"])
        em.op("dve", lambda e: e.tensor_tensor(out=tmp[:, :N], in0=mean[:, :N], in1=mean[:, :N], op=ALU.mult), reads=["ln_mean"], writes=[("ln_tmp", 0)])
        em.op("dve", lambda e: e.tensor_tensor(out=rstd[:, :N], in0=self.pb[b1][:, :N], in1=tmp[:, :N], op=ALU.subtract),
              reads=[("pb", b1), ("ln_tmp", 0)], writes=["ln_rstd"])
        em.op("act", lambda e: e.activation(out=rstd[:, :N], in_=rstd[:, :N], func=AF.Sqrt, bias=self.eps_sb[:], scale=1.0),
              reads=["ln_rstd", "consts"], writes=["ln_rstd"])
        em.op("dve", lambda e: e.reciprocal(out=rstd[:, :N], in_=rstd[:, :N]), reads=["ln_rstd"], writes=["ln_rstd"])
        P = self.par
        for c in range(nch):
            tmpc = W["tmp"] if c % 2 == 0 else W["tmp2"]
            tk = ("ln_tmp", c % 2)
            em.op("dve", lambda e, c=c, tmpc=tmpc: e.tensor_tensor(out=tmpc[:, :N], in0=r[:, c, :N], in1=mean[:, :N], op=ALU.subtract),
                  reads=[(rkey, c), "ln_mean"], writes=[tk])
            em.op("dve", lambda e, c=c, tmpc=tmpc: e.tensor_tensor(out=tmpc[:, :N], in0=tmpc[:, :N], in1=rstd[:, :N], op=ALU.mult),
                  reads=[tk, "ln_rstd"], writes=[tk])
            if func != AF.Identity:
                em.op("act", lambda e, c=c, tmpc=tmpc: e.activation(out=tmpc[:, :N], in_=tmpc[:, :N], func=AF.Identity,
                                                                   scale=P[:, gcol + c:gcol + c + 1], bias=P[:, bcol + c:bcol + c + 1]),
                      reads=[tk, "par"], writes=[tk])
                for (ot, okey) in outs:
                    em.op("act", lambda e, c=c, ot=ot, tmpc=tmpc: e.activation(out=ot[:, c, :N], in_=tmpc[:, :N], func=func), reads=[tk], writes=[(okey, c)])
                continue
            for (ot, okey) in outs:
                em.op("act", lambda e, c=c, ot=ot, tmpc=tmpc: e.activation(out=ot[:, c, :N], in_=tmpc[:, :N], func=func,
                                                                          scale=P[:, gcol + c:gcol + c + 1], bias=P[:, bcol + c:bcol + c + 1]),
                      reads=[tk, "par"], writes=[(okey, c)])

    def build(self):
        nc = self.nc
        T, L = self.T, self.depth
        I = {}
        I["xT"] = self.din("xT", [D, T])
        I["memT"] = self.din("memT", [D, MEM])
        I["w_in"] = self.din("w_in", [DEPTH, D, D_IN])
        I["w_out"] = self.din("w_out", [DEPTH, D, D])
        I["xq_w"] = self.din("xq_w", [DEPTH, D, D])
        I["xkv_w"] = self.din("xkv_w", [DEPTH, D, 2 * D])
        I["xo_w"] = self.din("xo_w", [DEPTH, D, D])
        I["ffn_w13"] = self.din("ffn_w13", [2, D, 2 * D_FF])
        I["ffn_w2"] = self.din("ffn_w2", [2, D_FF, D])
        I["router_w"] = self.din("router_w", [2, D, NEXP])
        I["exp_w13"] = self.din("exp_w13", [2, NEXP, D, 2 * D_FFE])
        I["exp_w2"] = self.din("exp_w2", [2, NEXP, D_FFE, D])
        I["params"] = self.din("params", [DEPTH, 128, NP])
        for nm in ("cmp_k", "cmp_v"):
            I[nm + "_w1"] = self.din(nm + "_w1", [DEPTH, 2048, 256])
            I[nm + "_w2"] = self.din(nm + "_w2", [DEPTH, 256, 64])
        I["conv_pw"] = self.din("conv_pw", [DEPTH, 256, 256])
        I["pool_w"] = self.din("pool_w", [DEPTH, 4, 64, 64])
        I["sgu_wT"] = self.din("sgu_wT", [DEPTH, 4, 128, 128])
        I["sgu_b"] = self.din("sgu_b", [DEPTH, 4, 128])
        I["sgu_ln_g"] = self.din("sgu_ln_g", [DEPTH, 256])
        I["sgu_ln_b"] = self.din("sgu_ln_b", [DEPTH, 256])
        I["cf32"] = self.din("cf32", [128, CF32_W])
        NKT = T // 128
        self.CB = {"Gm": 0, "Mimp": 17 * 128, "Caus": 21 * 128, "Wlow": 22 * 128, "E": 23 * 128}
        self.CBW = (23 + NKT) * 128
        I["cb"] = self.din("cb", [128, self.CBW])
        self.I = I
        self.out = nc.dram_tensor("outT", [D, T], F32, kind="ExternalOutput").ap()
        self.XA = self.dscr("XA", [D, T], F32)
        self.XB = self.dscr("XB", [D, T], F32)
        if "dump_y" in self.dbg:
            self.Y = nc.dram_tensor("Y", [128, NCH, T], BF16, kind="ExternalOutput").ap()
        else:
            self.Y = self.dscr("Y", [128, NCH, T], BF16)
        self.QT = self.dscr("QT", [64, 4, T], BF16)
        if "y_in" in self.dbg:
            self.Yin = self.din("y_in", [128, NCH, T], BF16)

        with ExitStack() as st:
            self.em = em = Em(nc, st)
            self.pb = [st.enter_context(nc.psum_tensor("pb%d" % i, [128, 512], F32)) for i in range(8)]
            self.rot = list(range(8))
            self.roti = 0
            self.ones_d = {1024: self.sb(st, "ones1024", [128, 128], BF16), 256: self.sb(st, "ones256", [128, 128], BF16)}
            self.ones1 = self.sb(st, "ones1", [128, 128], BF16)
            self.eps_sb = self.sb(st, "eps", [128, 1], F32)
            self.cf = self.sb(st, "cf", [128, CF32_W], F32)
            em.dma("sp", self.cf[:], I["cf32"], writes=["ident"])
            self.ident = self.cf[:, 0:128]
            self.identb = self.sb(st, "identb", [128, 128], BF16)
            em.op("act", lambda e: e.copy(out=self.identb[:], in_=self.cf[:, 0:128]), reads=["ident"], writes=["identb"])
            em.op("dve", lambda e: e.memset(self.ones_d[1024][:], 1.0 / 1024), writes=["ones"])
            em.op("dve", lambda e: e.memset(self.ones_d[256][:], 1.0 / 256), writes=["ones"])
            em.op("dve", lambda e: e.memset(self.ones1[:], 1.0), writes=["ones"])
            em.op("dve", lambda e: e.memset(self.eps_sb[:], EPS), writes=["consts"])
            self.par = self.sb(st, "par", [128, NP], F32)
            for l in range(L):
                em.dma("sp", self.par[:], I["params"][l], writes=["par"])
                xcur = I["xT"] if l == 0 else self.XB
                xcur_key = "xT" if l == 0 else "XB"
                last = (l == L - 1)
                if "y_in" not in self.dbg:
                    with ExitStack() as stn:
                        NQ = self.NQ
                        N = {"KselT": self.sb(stn, "KselT", [64, T], BF16), "KwinT": self.sb(stn, "KwinT", [64, T], BF16),
                             "Vsel": self.sb(stn, "Vsel", [128, NQ, 65], BF16), "Vwin": self.sb(stn, "Vwin", [128, NQ, 65], BF16),
                             "gates": self.sb(stn, "gates", [128, NQ, 12], F32), "kcT": self.sb(stn, "kcT", [64, 512], BF16),
                             "vcT": self.sb(stn, "vcT", [64, 512], BF16), "vca": self.sb(stn, "vca", [128, 4, 65], BF16)}
                        self.phase_a(l, xcur, xcur_key, N)
                        em.barrier()
                        if "skip_b" not in self.dbg:
                            self.phase_b(l, N)
                            em.barrier()
                Ysrc = self.Yin if "y_in" in self.dbg else self.Y
                self.phase_c(l, xcur, xcur_key, Ysrc)
                em.barrier()
                self.phase_d(l, self.out if last else self.XB, "out" if last else "XB")
                em.barrier()
            em.barrier(engs=("sp",))
            em.replay()
        return nc

    def gelu(self, x_ap, tmp_ap, xkey, tkey):
        em = self.em
        if GELU_NATIVE:
            em.op("act", lambda e: e.activation(out=x_ap, in_=x_ap, func=AF.Gelu_apprx_tanh), reads=[xkey], writes=[xkey])
            return
        em.op("dve", lambda e: e.tensor_tensor(out=tmp_ap, in0=x_ap, in1=x_ap, op=ALU.mult), reads=[xkey], writes=[tkey])
        em.op("dve", lambda e: e.tensor_scalar(out=tmp_ap, in0=tmp_ap, scalar1=0.044715, scalar2=1.0, op0=ALU.mult, op1=ALU.add), reads=[tkey], writes=[tkey])
        em.op("dve", lambda e: e.tensor_tensor(out=tmp_ap, in0=tmp_ap, in1=x_ap, op=ALU.mult), reads=[xkey, tkey], writes=[tkey])
        em.op("act", lambda e: e.activation(out=tmp_ap, in_=tmp_ap, func=AF.Sigmoid, scale=1.5957691216057308), reads=[tkey], writes=[tkey])
        em.op("dve", lambda e: e.tensor_tensor(out=x_ap, in0=tmp_ap, in1=x_ap, op=ALU.mult), reads=[xkey, tkey], writes=[xkey])

    def phase_a(self, l, xcur, xkey, N):
        nc, em, I, T = self.nc, self.em, self.I, self.T
        P = PCOL
        par = self.par
        with ExitStack() as st:
            sb = lambda n, s, d: self.sb(st, n, s, d)
            win = sb("win", [128, NCH, D_IN], BF16)
            xbf = [sb("xbf0", [128, NCH, TT], BF16)]
            w1 = [sb("w1_%d" % i, [64, 32, 256], BF16) for i in range(2)]
            w2c = [sb("w2c_%d" % i, [128, 2, 64], BF16) for i in range(2)]
            peT = sb("peT", [64, 64], BF16)
            cb = sb("cb", [128, 4], F32)
            cmp_h = [sb("cmp_h%d" % i, [64, 16 + TT], BF16) for i in range(2)]
            hid = sb("hid", [128, 2, 32], BF16)
            hpre = sb("hpre", [128, 32], F32)
            htmp = sb("htmp", [128, 32], F32)
            a_h = sb("a_h", [128, 2, 16 + TT], F32)
            S1 = sb("S1", [128, 16 + TT], F32)
            S2 = sb("S2", [128, 16 + TT], F32)
            Ssel = sb("Ssel", [128, 16 + TT], F32)
            dpool = sb("dpool", [128, 2, TT], BF16)
            PW = sb("PW", [128, 2, 128], BF16)
            u_sb = sb("u_sb", [128, 2, TT], F32)
            gtmp = sb("gtmp", [128, TT if not GELU_NATIVE else 2], F32)
            h_h = sb("h_h", [128, 2, 32 + TT], BF16)
            Dg = sb("Dg", [128, 2, 31, 128], BF16)
            cacc = sb("cacc", [128, 2, TT], F32)
            hcb = sb("hcb", [128, 2, TT], BF16)
            pw = sb("pw", [128, 2, 256], BF16)
            self.lnw = {"sq": sb("lnsq", [128, 2, TT], BF16), "rb": sb("lnrb", [128, 2, TT], BF16),
                        "mean": sb("lnmean", [128, TT], F32), "rstd": sb("lnrstd", [128, TT], F32), "tmp": sb("lntmp", [128, TT], F32), "tmp2": sb("lntmp2", [128, TT], F32)}
            sgm = self.lnw["tmp2"]
            mtmp = self.lnw["tmp"][:, 0:128]
            ybuf = sb("ybuf", [128, NCH, TT], BF16)
            qst = sb("qst", [64, 4, TT], BF16)
            vg = sb("vg", [128, 256], F32)
            vt = sb("vt", [128, 256], F32)
            vs1 = sb("vs1", [128, 1], F32)
            vs2 = sb("vs2", [128, 1], F32)
            vpad = [[sb("vpad%d_%d" % (s4, i), [128, 2, 128], BF16) for i in range(2)] for s4 in range(4)]
            WsTf = Ssel[:, 0:512].rearrange("p (g i) -> p g i", g=4)
            WsT = sb("WsT", [128, 4, 128], BF16)
            Btab = sb("Btab", [128, 2, 128], F32)
            Gbc = sb("Gbc", [128, 256], F32)
            Bbc = sb("Bbc", [128, 256], F32)

            em.dma("pool", win[:], I["w_in"][l].rearrange("(c p) n -> p c n", p=128), writes=["win"])
            for i, nm in enumerate(("cmp_k", "cmp_v")):
                em.dma("pool", w1[i][:], I[nm + "_w1"][l].rearrange("(p d) n -> d p n", d=64), writes=[("w1", i)])
                em.dma("pool", w2c[i][:], I[nm + "_w2"][l].rearrange("(c p) n -> p c n", p=128), writes=[("w2c", i)])
            em.dma("pool", pw[:], I["conv_pw"][l].rearrange("(c p) n -> p c n", p=128), writes=["pw"])
            em.op("dve", lambda e: e.memset(PW[:], 0.0), writes=["PW"])
            for c in range(2):
                for k in range(31):
                    em.op("dve", lambda e, c=c, k=k: e.tensor_single_scalar(out=Dg[:, c, k, :], in_=self.identb[:], scalar=par[:, P["conv_w"] + c * 31 + k:P["conv_w"] + c * 31 + k + 1], op=ALU.mult),
                          reads=["identb", "par"], writes=["Dg"])
            for g in range(4):
                em.dma("pool", PW[(g % 2) * 64:(g % 2) * 64 + 64, g // 2, (g % 2) * 64:(g % 2) * 64 + 64], I["pool_w"][l, g], writes=["PW"])
            em.dma("sp", WsTf, I["sgu_wT"][l].rearrange("g j i -> j g i"), writes=["WsTf", "Ssel_lo", "Ssel_hi"])
            for g in range(4):
                em.op("dve", lambda e, g=g: e.tensor_tensor(out=WsT[:, g, :], in0=WsTf[:, g, :], in1=self.cf[:, CF["triu"]:CF["triu"] + 128], op=ALU.mult),
                      reads=["WsTf", "ident"], writes=["WsT"])
                em.dma("sp", Btab[(g % 2) * 64:(g % 2) * 64 + 64, g // 2, :], I["sgu_b"][l, g].partition_broadcast(64), writes=["Btab"])
            em.dma("sp", Gbc[:], I["sgu_ln_g"][l].partition_broadcast(128), writes=["Gbc"])
            em.dma("sp", Bbc[:], I["sgu_ln_b"][l].partition_broadcast(128), writes=["Gbc"])
            em.op("act", lambda e: e.copy(out=peT[:], in_=par[0:64, P["peT"]:P["peT"] + 64]), reads=["par"], writes=["peT"])
            for i in range(2):
                for hc in range(2):
                    b = self.bank()
                    self.mm(self.pb[b][:, 0:1], ("pb", b), [(w1[i][:, p, hc * 128:(hc + 1) * 128], peT[:, i * 32 + p:i * 32 + p + 1], [("w1", i), "peT"]) for p in range(32)])
                    em.op("act", lambda e, i=i, hc=hc, b=b: e.copy(out=cb[:, i * 2 + hc:i * 2 + hc + 1], in_=self.pb[b][:, 0:1]), reads=[("pb", b)], writes=["cb"])
            em.op("dve", lambda e: e.memset(a_h[:, :, 0:16], 0.0), writes=["a_halo"])
            em.op("dve", lambda e: e.memset(h_h[:, :, 0:32], 0.0), writes=["h_halo0", "h_halo1"])
            for i in range(2):
                em.op("dve", lambda e, i=i: e.memset(cmp_h[i][:, 0:16], 0.0), writes=[("cmp_halo", i)])
                for s4 in range(4):
                    em.op("dve", lambda e, i=i, s4=s4: e.memset(vpad[s4][i][:], 0.0), writes=[("vpad", s4, i)])
            em.op("dve", lambda e: e.memset(N["kcT"][:], 0.0), writes=["kcT"])
            em.op("dve", lambda e: e.memset(N["vcT"][:], 0.0), writes=["vcT"])
            em.op("dve", lambda e: e.memset(N["Vsel"][:, :, 64:65], 1.0), writes=["Vsel"])
            em.op("dve", lambda e: e.memset(N["Vwin"][:, :, 64:65], 1.0), writes=["Vwin"])
            em.op("dve", lambda e: e.memset(N["vca"][:, :, 64:65], 1.0), writes=["vca"])

            def fm(col, width, xb, xk):
                b = self.bank()
                self.mm(self.pb[b][0:width, :], ("pb", b), [(win[:, k, col:col + width], xb[:, k, :], ["win", xk]) for k in range(NCH)])
                return b

            for t in range(self.NT):
                sl = 0
                c0 = t * TT
                xb, xk = xbf[sl], ("xbf", sl)
                em.dma("pool", xb[:], xcur[:, c0:c0 + TT].rearrange("(c p) t -> p c t", p=128), reads=[(xkey, t)], writes=[xk])
                for pr in range(2):
                    b = fm(pr * 128, 128, xb, xk)
                    em.op("act", lambda e, pr=pr, b=b: e.copy(out=a_h[:, pr, 16:], in_=self.pb[b][:]), reads=[("pb", b)], writes=[("a_h", pr)])
                for pr in range(2):
                    A = a_h[:, pr, :]
                    rdA = [("a_h", pr), "a_halo"]
                    n = 16 + TT
                    lo, hi = slice(0, 64), slice(64, 128)
                    if pr == 0:
                        em.op("dve", lambda e, A=A: e.tensor_tensor(out=Ssel[lo, 1:n], in0=A[lo, 1:n], in1=A[lo, 0:n - 1], op=ALU.add), reads=rdA, writes=["Ssel_lo"])
                        em.op("dve", lambda e, A=A: e.tensor_tensor(out=S1[hi, 1:n], in0=A[hi, 1:n], in1=A[hi, 0:n - 1], op=ALU.add), reads=rdA, writes=["S1_hi"])
                        em.op("dve", lambda e: e.tensor_tensor(out=Ssel[hi, 3:n], in0=S1[hi, 3:n], in1=S1[hi, 1:n - 2], op=ALU.add), reads=["S1_hi"], writes=["Ssel_hi"])
                    else:
                        em.op("dve", lambda e, A=A: e.tensor_tensor(out=S1[:, 1:n], in0=A[:, 1:n], in1=A[:, 0:n - 1], op=ALU.add), reads=rdA, writes=["S1_hi", "S1_lo"])
                        em.op("dve", lambda e: e.tensor_tensor(out=S2[:, 3:n], in0=S1[:, 3:n], in1=S1[:, 1:n - 2], op=ALU.add), reads=["S1_hi", "S1_lo"], writes=["S2"])
                        em.op("dve", lambda e: e.tensor_tensor(out=Ssel[lo, 7:n], in0=S2[lo, 7:n], in1=S2[lo, 3:n - 4], op=ALU.add), reads=["S2"], writes=["Ssel_lo"])
                        em.op("dve", lambda e: e.tensor_tensor(out=S1[hi, 7:n], in0=S2[hi, 7:n], in1=S2[hi, 3:n - 4], op=ALU.add), reads=["S2"], writes=["S1_hi"])
                        em.op("dve", lambda e: e.tensor_tensor(out=Ssel[hi, 15:n], in0=S1[hi, 15:n], in1=S1[hi, 7:n - 8], op=ALU.add), reads=["S1_hi"], writes=["Ssel_hi"])
                    em.op("dve", lambda e, pr=pr, A=A: e.scalar_tensor_tensor(out=dpool[:, pr, :], in0=Ssel[:, 16:], scalar=self.cf[:, CF["invw"] + pr:CF["invw"] + pr + 1], in1=A[:, 16:], op0=ALU.mult, op1=ALU.subtract),
                          reads=["Ssel_lo", "Ssel_hi", "ident"] + rdA, writes=[("dpool", pr)])
                    if t == 0:
                        em.op("dve", lambda e, pr=pr: e.tensor_tensor(out=Ssel[:, 0:16], in0=Ssel[:, 16:32], in1=self.cf[:, CF["invc"] + pr * 16:CF["invc"] + pr * 16 + 16], op=ALU.mult),
                              reads=["Ssel_lo", "Ssel_hi", "ident", ("dpool", pr)], writes=["Ssel_lo", "Ssel_hi"])
                        em.op("dve", lambda e, pr=pr, A=A: e.tensor_tensor(out=dpool[:, pr, 0:16], in0=Ssel[:, 0:16], in1=A[:, 16:32], op=ALU.subtract),
                              reads=["Ssel_lo", "Ssel_hi", ("dpool", pr)] + rdA, writes=[("dpool", pr)])
                    b = self.bank()
                    self.mm(self.pb[b][:], ("pb", b), [(PW[:, pr, :], dpool[:, pr, :], ["PW", ("dpool", pr)])])
                    em.op("act", lambda e, pr=pr, b=b: e.activation(out=ybuf[:, pr, :], in_=self.pb[b][:], func=AF.Identity, scale=par[:, P["pool_scale"] + pr:P["pool_scale"] + pr + 1]),
                          reads=[("pb", b), "par"], writes=[("ybuf", pr)])
                em.op("dve", lambda e: e.tensor_copy(out=a_h[:, :, 0:16], in_=a_h[:, :, TT:TT + 16]), reads=[("a_h", 0), ("a_h", 1)], writes=["a_halo"])
                for pr in range(2):
                    b = fm(908 + pr * 128, 128, xb, xk)
                    em.op("act", lambda e, pr=pr, b=b: e.copy(out=u_sb[:, pr, :], in_=self.pb[b][:]), reads=[("pb", b)], writes=[("u_sb", pr)])
                    self.gelu(u_sb[:, pr, :], gtmp[:], ("u_sb", pr), "gtmp")
                self.dump("u_sb", u_sb[:], [128, 2, TT], F32, [("u_sb", 0), ("u_sb", 1)])
                for s4 in range(4):
                    ts = slice(s4 * 128, (s4 + 1) * 128)
                    b = self.bank()
                    self.mm(self.pb[b][:, 0:256], ("pb", b), [(xb[:, k, ts], win[:, k, 1164:1420], ["win", xk]) for k in range(NCH)])
                    em.op("act", lambda e, b=b: e.copy(out=vg[:], in_=self.pb[b][:, 0:256]), reads=[("pb", b)], writes=["vg"])
                    self.dump("vg_pre", vg[:], [128, 256], F32, ["vg"])
                    self.gelu(vg[:], vt[:], "vg", "vt")
                    self.dump("vg_gelu", vg[:], [128, 256], F32, ["vg"])
                    em.op("dve", lambda e: e.tensor_reduce(out=vs1[:], in_=vg[:], axis=AX.X, op=ALU.add), reads=["vg"], writes=["vs1"])
                    em.op("dve", lambda e: e.tensor_single_scalar(out=vs1[:], in_=vs1[:], scalar=-1.0 / 256, op=ALU.mult), reads=["vs1"], writes=["vs1"])
                    em.op("dve", lambda e: e.tensor_single_scalar(out=vg[:], in_=vg[:], scalar=vs1[:, 0:1], op=ALU.add), reads=["vg", "vs1"], writes=["vg"])
                    em.op("dve", lambda e: e.tensor_tensor(out=vt[:], in0=vg[:], in1=vg[:], op=ALU.mult), reads=["vg"], writes=["vt"])
                    em.op("dve", lambda e: e.tensor_reduce(out=vs2[:], in_=vt[:], axis=AX.X, op=ALU.add), reads=["vt"], writes=["vs2"])
                    em.op("dve", lambda e: e.tensor_scalar(out=vs2[:], in0=vs2[:], scalar1=1.0 / 256, scalar2=EPS, op0=ALU.mult, op1=ALU.add), reads=["vs2"], writes=["vs2"])
                    em.op("act", lambda e: e.activation(out=vs2[:], in_=vs2[:], func=AF.Sqrt), reads=["vs2"], writes=["vs2"])
                    em.op("dve", lambda e: e.reciprocal(out=vs2[:], in_=vs2[:]), reads=["vs2"], writes=["vs2"])
                    em.op("dve", lambda e: e.scalar_tensor_tensor(out=vg[:], in0=vg[:], scalar=vs2[:, 0:1], in1=Gbc[:], op0=ALU.mult, op1=ALU.mult), reads=["vg", "vs2", "Gbc"], writes=["vg"])
                    self.dump("vg_ln", vg[:], [128, 256], F32, ["vg"])
                    self.dump("vs2", vs2[:], [128, 1], F32, ["vs2"])
                    for g in range(4):
                        em.op("dve", lambda e, g=g, s4=s4: e.tensor_tensor(out=vpad[s4][g % 2][:, g // 2, (g % 2) * 64:(g % 2) * 64 + 64], in0=vg[:, g * 64:(g + 1) * 64], in1=Bbc[:, g * 64:(g + 1) * 64], op=ALU.add),
                              reads=["vg", "Gbc"], writes=[("vpad", s4, g % 2)])
                for h in range(4):
                    b = fm(256 + h * 64, 64, xb, xk)
                    em.op("act", lambda e, h=h, b=b: e.copy(out=qst[:, h, :], in_=self.pb[b][0:64, :]), reads=[("pb", b)], writes=["qst"])
                em.dma("sp", self.QT[:, :, c0:c0 + TT], qst[:], reads=["qst"], writes=[("QT", t)])
                for (col, dst, key) in ((640, N["KselT"], "KselT"), (768, N["KwinT"], "KwinT")):
                    b = fm(col, 64, xb, xk)
                    em.op("act", lambda e, dst=dst, b=b, c0=c0: e.copy(out=dst[:, c0:c0 + TT], in_=self.pb[b][0:64, :]), reads=[("pb", b)], writes=[(key, t)])
                for i in range(2):
                    b = fm(512 + i * 64, 64, xb, xk)
                    em.op("act", lambda e, i=i, b=b: e.copy(out=cmp_h[i][:, 16:], in_=self.pb[b][0:64, :]), reads=[("pb", b)], writes=[("cmp_h", i)])
                j0 = 1 if t == 0 else 0
                nb = 32 - j0
                col0 = 0 if t == 0 else 32 * t - 1
                for i in range(2):
                    for hc in range(2):
                        b = self.bank()
                        self.mm(self.pb[b][:, 0:nb], ("pb", b),
                                [(w1[i][:, p, hc * 128:(hc + 1) * 128], cmp_h[i][:, 16 * j0 + p:16 * j0 + p + 16 * (nb - 1) + 1:16], [("w1", i), ("cmp_h", i), ("cmp_halo", i)]) for p in range(32)])
                        em.op("dve", lambda e, i=i, hc=hc, b=b, nb=nb: e.tensor_single_scalar(out=hpre[:, 0:nb], in_=self.pb[b][:, 0:nb], scalar=cb[:, i * 2 + hc:i * 2 + hc + 1], op=ALU.add),
                              reads=[("pb", b), "cb"], writes=["hpre"])
                        self.gelu(hpre[:, 0:nb], htmp[:, 0:nb], "hpre", "htmp")
                        em.op("act", lambda e, hc=hc, nb=nb: e.copy(out=hid[:, hc, 0:nb], in_=hpre[:, 0:nb]), reads=["hpre"], writes=[("hid", hc)])
                    b = self.bank()
                    self.mm(self.pb[b][0:64, 0:nb], ("pb", b), [(w2c[i][:, hc, :], hid[:, hc, 0:nb], [("w2c", i), ("hid", hc)]) for hc in range(2)])
                    dst, key = (N["kcT"], "kcT") if i == 0 else (N["vcT"], "vcT")
                    em.op("act", lambda e, dst=dst, b=b, col0=col0, nb=nb: e.copy(out=dst[:, col0:col0 + nb], in_=self.pb[b][0:64, 0:nb]), reads=[("pb", b)], writes=[key])
                    em.op("dve", lambda e, i=i: e.tensor_copy(out=cmp_h[i][:, 0:16], in_=cmp_h[i][:, TT:TT + 16]), reads=[("cmp_h", i)], writes=[("cmp_halo", i)])
                for s4 in range(4):
                    qt = t * 4 + s4
                    ts = slice(s4 * 128, (s4 + 1) * 128)
                    b = self.bank()
                    self.mm(self.pb[b][:, 0:204], ("pb", b), [(xb[:, k, ts], win[:, k, 704:908], ["win", xk]) for k in range(NCH)])
                    em.op("act", lambda e, qt=qt, b=b: e.copy(out=N["Vsel"][:, qt, 0:64], in_=self.pb[b][:, 0:64]), reads=[("pb", b)], writes=["Vsel"])
                    em.op("act", lambda e, qt=qt, b=b: e.copy(out=N["Vwin"][:, qt, 0:64], in_=self.pb[b][:, 128:192]), reads=[("pb", b)], writes=["Vwin"])
                    em.op("act", lambda e, qt=qt, b=b: e.activation(out=N["gates"][:, qt, :], in_=self.pb[b][:, 192:204], func=AF.Sigmoid), reads=[("pb", b)], writes=["gates"])
                for c in range(2):
                    ba = fm(1420 + c * 128, 128, xb, xk)
                    bg = fm(1676 + c * 128, 128, xb, xk)
                    em.op("act", lambda e, bg=bg: e.activation(out=sgm[:], in_=self.pb[bg][:], func=AF.Sigmoid), reads=[("pb", bg)], writes=[("ln_tmp", 1)])
                    em.op("dve", lambda e, c=c, ba=ba: e.tensor_tensor(out=h_h[:, c, 32:], in0=self.pb[ba][:], in1=sgm[:], op=ALU.mult), reads=[("pb", ba), ("ln_tmp", 1)], writes=[("h_h", c)])
                    ceng = "dve"
                    b = self.bank()
                    self.mm(self.pb[b][:], ("pb", b), [(Dg[:, c, k, :], h_h[:, c, 2 + k:2 + k + TT], ["Dg", ("h_h", c), ("h_halo%d" % c)]) for k in range(31)])
                    em.op("act", lambda e, c=c, b=b: e.activation(out=cacc[:, c, :], in_=self.pb[b][:], func=AF.Identity, bias=par[:, P["conv_b"] + c:P["conv_b"] + c + 1], scale=1.0),
                          reads=[("pb", b), "par"], writes=[("cacc", c)])
                    em.op(ceng, lambda e, c=c: e.tensor_copy(out=h_h[:, c, 0:32], in_=h_h[:, c, TT:TT + 32]), reads=[("h_h", c)], writes=[("h_halo%d" % c)])
                self.dump("h_h", h_h[:], [128, 2, 32 + TT], BF16, [("h_h", 0), ("h_h", 1)])
                self.dump("cacc", cacc[:], [128, 2, TT], F32, [("cacc", 0), ("cacc", 1)])
                self.ln_fm(cacc, "cacc", 2, P["conv_lng"], P["conv_lnb"], TT, [(hcb, "hcb")], func=AF.Silu)
                self.dump("hcb", hcb[:], [128, 2, TT], BF16, [("hcb", 0), ("hcb", 1)])
                for oc in range(2):
                    b = self.bank()
                    self.mm(self.pb[b][:], ("pb", b), [(pw[:, k2, oc * 128:(oc + 1) * 128], hcb[:, k2, :], ["pw", ("hcb", k2)]) for k2 in range(2)])
                    em.op("act", lambda e, oc=oc, b=b: e.copy(out=ybuf[:, 6 + oc, :], in_=self.pb[b][:]), reads=[("pb", b)], writes=[("ybuf", 6 + oc)])
                for s4 in range(4):
                    ts = slice(s4 * 128, (s4 + 1) * 128)
                    for pr in range(2):
                        b = self.bank()
                        self.mm(self.pb[b][:, 0:128], ("pb", b), [(vpad[s4][hh][:, pr, :], WsT[:, 2 * pr + hh, :], [("vpad", s4, hh), "WsT"]) for hh in range(2)])
                        em.op("dve", lambda e, pr=pr, b=b: e.tensor_tensor(out=mtmp, in0=self.pb[b][:, 0:128], in1=Btab[:, pr, :], op=ALU.add), reads=[("pb", b), "Btab"], writes=[("ln_tmp", 0)])
                        em.op("dve", lambda e, pr=pr, ts=ts: e.tensor_tensor(out=ybuf[:, 4 + pr, ts], in0=mtmp, in1=u_sb[:, pr, ts], op=ALU.mult), reads=[("ln_tmp", 0), ("u_sb", pr)], writes=[("ybuf", 4 + pr)])
                em.dma("sp", self.Y[:, 0:2, c0:c0 + TT], ybuf[:, 0:2, :], reads=[("ybuf", 0), ("ybuf", 1)], writes=[("Y", t)])
                em.dma("sp", self.Y[:, 4:8, c0:c0 + TT], ybuf[:, 4:8, :], reads=[("ybuf", c) for c in range(4, 8)], writes=[("Y", t)])
            for kt in range(4):
                b = self.bank()
                self.mm(self.pb[b][:, 0:64], ("pb", b), [(N["vcT"][:, kt * 128:(kt + 1) * 128], self.identb[0:64, 0:64], ["vcT", "identb"])])
                em.op("act", lambda e, kt=kt, b=b: e.copy(out=N["vca"][:, kt, 0:64], in_=self.pb[b][:, 0:64]), reads=[("pb", b)], writes=["vca"])

    def phase_b(self, l, N):
        nc, em, I, T = self.nc, self.em, self.I, self.T
        NKT = T // 128
        CB = self.CB
        identb = self.identb
        with ExitStack() as st:
            sb = lambda n, s, d: self.sb(st, n, s, d)
            cbt = sb("cbt", [128, self.CBW], BF16)
            em.dma("pool", cbt[:], I["cb"], writes=["cbt"])
            qts = [sb("qts%d" % i, [64, 4, 128], BF16) for i in range(2)]
            PT = [sb("PT%d" % i, [128, 512], BF16) for i in range(4)]
            lsb = sb("lsb", [128, 3, 4], F32)
            coef = sb("coef", [128, 3, 4], F32)
            imps = sb("imps", [128, 128], F32)
            sc = sb("sc", [128, 128], F32)
            sc2 = sb("sc2", [128, 128], F32)
            top8 = sb("top8", [128, 8], F32)
            thr = sb("thr", [128, 1], F32)
            negsel = sb("negsel", [128, 128], BF16)
            negselT = sb("negselT", [128, 128], BF16)
            osb = sb("osb", [128, 256], F32)
            obf = sb("obf", [128, 256], BF16)
            ynsa = sb("ynsa", [128, 2, TT], BF16)
            ACC = {"c": 0, "s": 1, "w": 2}
            IMPB = 3
            self.rot = [4, 5, 6, 7]
            pti = [0]

            def bc(ap2d):
                return ap2d[:, None, :].broadcast_to([128, 4, 128])

            def pair(br, sl, kT_ap, kkeys, masks, v_ap, vkeys, first, last, imp_rhs=None):
                b = self.bank()
                S = self.pb[b][:].rearrange("p (h q) -> p h q", h=4)
                n = 1 + len(masks)
                em.op("pe", lambda e: e.matmul(S, lhsT=kT_ap, rhs=qts[sl][:], start=True, stop=(n == 1)), reads=kkeys + [("qts", sl)], writes=[("pb", b)])
                for mi, (ml, mr, mk) in enumerate(masks):
                    em.op("pe", lambda e, ml=ml, mr=mr, mi=mi: e.matmul(S, lhsT=ml, rhs=mr, start=False, stop=(mi == n - 2)), reads=mk, writes=[("pb", b)])
                ps = pti[0] % 4
                pti[0] += 1
                if self.cur_i == self.dbg.get("DI", -1):
                    t_ = self.sb(st, "Sd", [128, 512], F32)
                    em.op("dve", lambda e, b=b, t_=t_: e.tensor_copy(out=t_[:], in_=self.pb[b][:]), reads=[("pb", b)], writes=["Sd" + br])
                    self.dump("b_S" + br, t_[:], [128, 512], F32, ["Sd" + br])
                em.op("act", lambda e, ps=ps, b=b: e.activation(out=PT[ps][:], in_=self.pb[b][:], func=AF.Exp, scale=0.125), reads=[("pb", b)], writes=[("PT", ps)])
                if self.cur_i == self.dbg.get("DI", -1):
                    self.dump("b_PT" + br, PT[ps][:], [128, 512], BF16, [("PT", ps)])
                ab = ACC[br]

                def stage2():
                    for h in range(4):
                        em.op("pe", lambda e, h=h, ps=ps: e.matmul(self.pb[ab][:, h * 65:(h + 1) * 65], lhsT=PT[ps][:, h * 128:(h + 1) * 128], rhs=v_ap, start=(first and h == 0), stop=last, skip_group_check=True),
                              reads=[("PT", ps)] + vkeys, writes=[("acc", br)])
                    if imp_rhs is not None:
                        for h in range(4):
                            em.op("pe", lambda e, h=h, ps=ps: e.matmul(self.pb[IMPB][:, h * 128:(h + 1) * 128], lhsT=PT[ps][:, h * 128:(h + 1) * 128], rhs=imp_rhs, start=(first and h == 0), stop=last, skip_group_check=True),
                                  reads=[("PT", ps), "cbt"], writes=["imp"])
                pending.append(stage2)
                while len(pending) > 2:
                    pending.pop(0)()

            pending = []

            def flush():
                while pending:
                    pending.pop(0)()

            oT = [sb("oT%d" % i_, [65, 512], F32) for i_ in range(3)]

            def finalize(br):
                return
                ab = ACC[br]
                em.op("act", lambda e: e.copy(out=oT[ab][:], in_=self.pb[ab][0:65, :]), reads=[("acc", br)], writes=[("oT", ab)])
                for h in range(4):
                    em.op("pe", lambda e, h=h: e.matmul(self.pb[ab][:, h * 65:(h + 1) * 65], lhsT=oT[ab][:, h * 128:(h + 1) * 128], rhs=self.ident[0:65, 0:65], start=(h == 0), stop=(h == 3), skip_group_check=True),
                          reads=[("oT", ab), "ident"], writes=[("acc", br)])

            for i in range(self.NQ):
                self.cur_i = i
                DI = self.dbg.get("DI", -1)
                sl = i % 2
                q0 = i * 128
                em.dma("sp", qts[sl][:], self.QT[:, :, q0:q0 + 128], reads=[("QT", i // 4)], writes=[("qts", sl)])
                ktl = (8 * i + 6) // 128
                ip = i - 16 * ktl
                kts = list(range(ktl + 1))
                for kt in kts:
                    masks = []
                    off = 8 * i - 128 * kt
                    if off <= 128:
                        g = CB["Gm"] + (off // 8) * 128
                        masks.append((identb[:], bc(cbt[:, g:g + 128]), ["identb", "cbt"]))
                    m0 = CB["Mimp"] + kt * 128
                    pair("c", sl, N["kcT"][:, kt * 128:(kt + 1) * 128], ["kcT"], masks, N["vca"][:, kt, :], ["vca"], kt == 0, kt == kts[-1], imp_rhs=cbt[:, m0:m0 + 128])
                flush()
                finalize("c")
                accc = self.pb[0][:, 0:260].rearrange("p (h d) -> p h d", d=65)
                em.op("dve", lambda e: e.tensor_single_scalar(out=lsb[:, 0, :], in_=accc[:, :, 64], scalar=1e-30, op=ALU.max), reads=[("acc", "c")], writes=[("lsb", 0)])
                em.op("dve", lambda e: e.reciprocal(out=lsb[:, 0, :], in_=lsb[:, 0, :]), reads=[("lsb", 0)], writes=[("lsb", 0)])
                for h in range(4):
                    if h == 0:
                        em.op("dve", lambda e: e.tensor_single_scalar(out=imps[:], in_=self.pb[IMPB][:, 0:128], scalar=lsb[:, 0, 0:1], op=ALU.mult), reads=["imp", ("lsb", 0)], writes=["imps"])
                    else:
                        em.op("dve", lambda e, h=h: e.scalar_tensor_tensor(out=imps[:], in0=self.pb[IMPB][:, h * 128:(h + 1) * 128], scalar=lsb[:, 0, h:h + 1], in1=imps[:], op0=ALU.mult, op1=ALU.add),
                              reads=["imp", ("lsb", 0), "imps"], writes=["imps"])
                w0 = 126 - 2 * i
                em.op("dve", lambda e, w0=w0: e.tensor_tensor(out=sc[:], in0=imps[:], in1=self.cf[:, CF["CW"] + w0:CF["CW"] + w0 + 128], op=ALU.add), reads=["imps", "ident"], writes=["sc"])
                em.op("dve", lambda e: e.tensor_single_scalar(out=sc[:, 0:1], in_=sc[:, 0:1], scalar=1e4, op=ALU.add), reads=["sc"], writes=["sc"])
                em.op("dve", lambda e, w0=w0: e.tensor_tensor(out=sc[:], in0=sc[:], in1=self.cf[:, CF["VW"] + w0:CF["VW"] + w0 + 128], op=ALU.mult), reads=["sc", "ident"], writes=["sc"])
                em.op("dve", lambda e: e.max(out=top8[:], in_=sc[:]), reads=["sc"], writes=["top8"])
                em.op("dve", lambda e: e.match_replace(out=sc2[:], in_to_replace=top8[:], in_values=sc[:], imm_value=-1.0), reads=["sc", "top8"], writes=["sc2"])
                em.op("dve", lambda e: e.max(out=top8[:], in_=sc2[:]), reads=["sc2"], writes=["top8"])
                em.op("dve", lambda e: e.tensor_single_scalar(out=thr[:], in_=top8[:, 7:8], scalar=0.5, op=ALU.max), reads=["top8"], writes=["thr"])
                em.op("dve", lambda e: e.tensor_scalar(out=negsel[:], in0=sc[:], scalar1=thr[:, 0:1], scalar2=NEGM, op0=ALU.is_lt, op1=ALU.mult), reads=["sc", "thr"], writes=["negsel"])
                b = self.bank()
                em.op("pe", lambda e, b=b: e.matmul(self.pb[b][:, 0:128], lhsT=negsel[:], rhs=identb[:], start=True, stop=True), reads=["negsel", "identb"], writes=[("pb", b)])
                em.op("act", lambda e, b=b: e.copy(out=negselT[:], in_=self.pb[b][:, 0:128]), reads=[("pb", b)], writes=["negselT"])
                if i == DI:
                    self.dump("b_imps", imps[:], [128, 128], F32, ["imps"])
                    self.dump("b_sc", sc[:], [128, 128], F32, ["sc"])
                    self.dump("b_thr", thr[:], [128, 1], F32, ["thr"])
                    self.dump("b_negselT", negselT[:], [128, 128], BF16, ["negselT"])
                    self.dump("b_lsb", lsb[:], [128, 3, 4], F32, [("lsb", 0)])
                wk = list(range(max(0, i - 4), i + 1))
                for kt in wk:
                    masks = []
                    if kt == i - 4:
                        masks.append((identb[:], bc(cbt[:, CB["Wlow"]:CB["Wlow"] + 128]), ["identb", "cbt"]))
                    if kt == i:
                        masks.append((identb[:], bc(cbt[:, CB["Caus"]:CB["Caus"] + 128]), ["identb", "cbt"]))
                    pair("w", sl, N["KwinT"][:, kt * 128:(kt + 1) * 128], [("KwinT", kt // 4)], masks, N["Vwin"][:, kt, :], ["Vwin"], kt == wk[0], kt == wk[-1])
                DI = self.dbg.get("DI", -1)
                if i == DI:
                    self.dump("b_qts", qts[sl][:], [64, 4, 128], BF16, [("qts", sl)])
                    self.dump("b_KwinT", N["KwinT"][:], [64, T], BF16, [("KwinT", t_) for t_ in range(self.NT)])
                    self.dump("b_kcT", N["kcT"][:], [64, 512], BF16, ["kcT"])
                    self.dump("b_vca", N["vca"][:], [128, 4, 65], BF16, ["vca"])
                    self.dump("b_Vwin", N["Vwin"][:], [128, self.NQ, 65], BF16, ["Vwin"])
                    self.dump("b_accc", self.pbcopy(st, 0, [("acc", "c")]), [128, 512], F32, ["pbc0"])
                    self.dump("b_accw", self.pbcopy(st, 2, [("acc", "w")]), [128, 512], F32, ["pbc2"])
                    self.dump("b_imp", self.pbcopy(st, 3, ["imp"]), [128, 512], F32, ["pbc3"])
                for kt in range(i + 1):
                    e0 = CB["E"] + kt * 128
                    masks = [(cbt[:, e0:e0 + 128], bc(negselT[:]), ["cbt", "negselT"])]
                    if kt == i:
                        masks.append((identb[:], bc(cbt[:, CB["Caus"]:CB["Caus"] + 128]), ["identb", "cbt"]))
                    pair("s", sl, N["KselT"][:, kt * 128:(kt + 1) * 128], [("KselT", kt // 4)], masks, N["Vsel"][:, kt, :], ["Vsel"], kt == 0, kt == i)
                flush()
                finalize("w")
                finalize("s")
                for bi, br in enumerate(("s", "w")):
                    av = self.pb[ACC[br]][:, 0:260].rearrange("p (h d) -> p h d", d=65)
                    em.op("dve", lambda e, av=av, bi=bi: e.tensor_single_scalar(out=lsb[:, bi + 1, :], in_=av[:, :, 64], scalar=1e-30, op=ALU.max), reads=[("acc", br)], writes=[("lsb", bi + 1)])
                    em.op("dve", lambda e, bi=bi: e.reciprocal(out=lsb[:, bi + 1, :], in_=lsb[:, bi + 1, :]), reads=[("lsb", bi + 1)], writes=[("lsb", bi + 1)])
                gv = N["gates"][:, i, :].rearrange("p (h b) -> p b h", b=3)
                em.op("dve", lambda e, gv=gv: e.tensor_tensor(out=coef[:], in0=lsb[:], in1=gv, op=ALU.mult), reads=[("lsb", 0), ("lsb", 1), ("lsb", 2), "gates"], writes=["coef"])
                for h in range(4):
                    for bi, br in enumerate(("c", "s", "w")):
                        av = self.pb[ACC[br]][:, h * 65:h * 65 + 64]
                        oh = osb[:, h * 64:(h + 1) * 64]
                        if bi == 0:
                            em.op("dve", lambda e, av=av, oh=oh, bi=bi, h=h: e.tensor_single_scalar(out=oh, in_=av, scalar=coef[:, bi, h:h + 1], op=ALU.mult), reads=[("acc", br), "coef"], writes=[("osb", h)])
                        else:
                            em.op("dve", lambda e, av=av, oh=oh, bi=bi, h=h: e.scalar_tensor_tensor(out=oh, in0=av, scalar=coef[:, bi, h:h + 1], in1=oh, op0=ALU.mult, op1=ALU.add),
                                  reads=[("acc", br), "coef", ("osb", h)], writes=[("osb", h)])
                if i == DI:
                    self.dump("b_accs", self.pbcopy(st, 1, [("acc", "s")]), [128, 512], F32, ["pbc1"])
                    self.dump("b_coef", coef[:], [128, 3, 4], F32, ["coef"])
                    self.dump("b_osb", osb[:], [128, 256], F32, [("osb", h) for h in range(4)])
                em.op("act", lambda e: e.copy(out=obf[:], in_=osb[:]), reads=[("osb", h) for h in range(4)], writes=["obf"])
                for c in range(2):
                    b = self.bank()
                    em.op("pe", lambda e, c=c, b=b: e.matmul(self.pb[b][:, 0:128], lhsT=obf[:, c * 128:(c + 1) * 128], rhs=identb[:], start=True, stop=True), reads=["obf", "identb"], writes=[("pb", b)])
                    em.op("act", lambda e, c=c, b=b, i=i: e.copy(out=ynsa[:, c, (i % 4) * 128:(i % 4 + 1) * 128], in_=self.pb[b][:, 0:128]), reads=[("pb", b)], writes=["ynsa"])
                if i % 4 == 3:
                    t = i // 4
                    em.dma("sp", self.Y[:, 2:4, t * TT:(t + 1) * TT], ynsa[:], reads=["ynsa"], writes=[("Y", t)])
            self.rot = list(range(8))

    def phase_c(self, l, xcur, xkey, Ysrc):
        nc, em, I, T = self.nc, self.em, self.I, self.T
        with ExitStack() as st:
            sb = lambda n, s, d: self.sb(st, n, s, d)
            KxT = sb("KxT", [128, NCH, MEM], BF16)
            Vx = sb("Vx", [128, 2, D], BF16)

            def wload(dst, src, key):
                em.dma("pool", dst[:], src.rearrange("(c p) n -> p c n", p=128), writes=[key])
            with ExitStack() as st2:
                wkv = self.sb(st2, "wkv", [128, NCH, 2 * D], BF16)
                memT = self.sb(st2, "memT", [128, NCH, MEM], BF16)
                wload(wkv, I["xkv_w"][l], "wkv")
                wload(memT, I["memT"], "memT")
                for oc in range(NCH):
                    b = self.bank()
                    self.mm(self.pb[b][:, :MEM], ("pb", b), [(wkv[:, k, oc * 128:(oc + 1) * 128], memT[:, k, :], ["wkv", "memT"]) for k in range(NCH)])
                    em.op("act", lambda e, oc=oc, b=b: e.copy(out=KxT[:, oc, :], in_=self.pb[b][:, :MEM]), reads=[("pb", b)], writes=["KxT"])
                for mt in range(2):
                    for hf in range(2):
                        b = self.bank()
                        self.mm(self.pb[b][:], ("pb", b), [(memT[:, k, mt * 128:(mt + 1) * 128], wkv[:, k, D + hf * 512:D + (hf + 1) * 512], ["wkv", "memT"]) for k in range(NCH)])
                        em.op("act", lambda e, mt=mt, hf=hf, b=b: e.copy(out=Vx[:, mt, hf * 512:(hf + 1) * 512], in_=self.pb[b][:]), reads=[("pb", b)], writes=["Vx"])
            em.barrier()
            wout = sb("wout", [128, NCH, D], BF16)
            wq = sb("wq", [128, NCH, D], BF16)
            wo = sb("wo", [128, NCH, D], BF16)
            self.lnw = {"sq": sb("lnsq", [128, NCH, TT], BF16), "rb": sb("lnrb", [128, NCH, TT], BF16),
                        "mean": sb("lnmean", [128, TT], F32), "rstd": sb("lnrstd", [128, TT], F32), "tmp": sb("lntmp", [128, TT], F32), "tmp2": sb("lntmp2", [128, TT], F32)}
            ybf = [sb("ybf%d" % i, [128, NCH, TT], BF16) for i in range(2)]
            xs = [sb("xs%d" % i, [128, NCH, TT], F32) for i in range(2)]
            r = sb("r", [128, NCH, TT], F32)
            x1 = sb("x1", [128, NCH, TT], F32)
            qx = sb("qx", [128, NCH, TT], BF16)
            pT = sb("pT", [128, 2, TT], BF16)
            rl = sb("rl", [128, TT], F32)
            ox = sb("ox", [128, NCH, TT], BF16)
            x2 = sb("x2", [128, NCH, TT], F32)
            wload(wout, I["w_out"][l], "wout")
            wload(wq, I["xq_w"][l], "wq")
            wload(wo, I["xo_w"][l], "wo")

            P = PCOL
            for t in range(self.NT):
                sl = t % 2
                c0 = t * TT
                em.dma("sp", ybf[sl][:], Ysrc[:, :, c0:c0 + TT], reads=[("Y", t)], writes=[("ybf", sl)] + [(("x1b", sl), c) for c in range(NCH)])
                x1b = ybf[sl]
                em.dma("sp", xs[sl][:], xcur[:, c0:c0 + TT].rearrange("(c p) t -> p c t", p=128), reads=[(xkey, t)], writes=[("xs", sl)])
                for oc in range(NCH):
                    b = self.bank()
                    self.mm(self.pb[b][:], ("pb", b), [(wout[:, k, oc * 128:(oc + 1) * 128], ybf[sl][:, k, :], ["wout", ("ybf", sl)]) for k in range(NCH)])
                    em.op("dve", lambda e, oc=oc, b=b, sl=sl: e.scalar_tensor_tensor(out=r[:, oc, :], in0=xs[sl][:, oc, :], scalar=ALPHA, in1=self.pb[b][:], op0=ALU.mult, op1=ALU.add),
                          reads=[("xs", sl), ("pb", b)], writes=[("r", oc)])
                self.ln_fm(r, "r", NCH, P["ln1g"], P["ln1b"], TT, [(x1, "x1"), (x1b, ("x1b", sl))])
                for oc in range(NCH):
                    b = self.bank()
                    self.mm(self.pb[b][:], ("pb", b), [(wq[:, k, oc * 128:(oc + 1) * 128], x1b[:, k, :], ["wq", (("x1b", sl), k)]) for k in range(NCH)])
                    em.op("act", lambda e, oc=oc, b=b: e.copy(out=qx[:, oc, :], in_=self.pb[b][:]), reads=[("pb", b)], writes=[("qx", oc)])
                for h in range(4):
                    for mt in range(2):
                        b = self.bank()
                        self.mm(self.pb[b][:], ("pb", b), [(KxT[:, 2 * h + dc, mt * 128:(mt + 1) * 128], qx[:, 2 * h + dc, :], ["KxT", ("qx", 2 * h + dc)]) for dc in range(2)])
                        em.op("act", lambda e, mt=mt, b=b: e.activation(out=pT[:, mt, :], in_=self.pb[b][:], func=AF.Exp, scale=1.0 / 16.0),
                              reads=[("pb", b)], writes=[("pT", mt)])
                    b = self.bank()
                    self.mm(self.pb[b][:], ("pb", b), [(self.ones1[:], pT[:, mt, :], ["ones", ("pT", mt)]) for mt in range(2)])
                    em.op("dve", lambda e, b=b: e.reciprocal(out=rl[:], in_=self.pb[b][:]), reads=[("pb", b)], writes=["rl"])
                    for dc in range(2):
                        b = self.bank()
                        self.mm(self.pb[b][:], ("pb", b), [(Vx[:, mt, h * 256 + dc * 128:h * 256 + (dc + 1) * 128], pT[:, mt, :], ["Vx", ("pT", mt)]) for mt in range(2)])
                        em.op("dve", lambda e, h=h, dc=dc, b=b: e.tensor_tensor(out=ox[:, 2 * h + dc, :], in0=self.pb[b][:], in1=rl[:], op=ALU.mult),
                              reads=[("pb", b), "rl"], writes=[("ox", 2 * h + dc)])
                for oc in range(NCH):
                    b = self.bank()
                    self.mm(self.pb[b][:], ("pb", b), [(wo[:, k, oc * 128:(oc + 1) * 128], ox[:, k, :], ["wo", ("ox", k)]) for k in range(NCH)])
                    em.op("dve", lambda e, oc=oc, b=b: e.scalar_tensor_tensor(out=r[:, oc, :], in0=x1[:, oc, :], scalar=ALPHA, in1=self.pb[b][:], op0=ALU.mult, op1=ALU.add),
                          reads=[("x1", oc), ("pb", b)], writes=[("r", oc)])
                self.ln_fm(r, "r", NCH, P["ln2g"], P["ln2b"], TT, [(x2, "x2")])
                em.dma("sp", self.XA[:, c0:c0 + TT].rearrange("(c p) t -> p c t", p=128), x2[:], reads=[("x2", c) for c in range(NCH)], writes=[("XA", t)])

    def phase_d(self, l, xdst, dkey):
        nc, em, I, T = self.nc, self.em, self.I, self.T
        moe = (l % 2 == 1)
        li = l // 2
        ST = min(1024 if moe else 2048, T)
        nsub = ST // TT
        GC = 4
        with ExitStack() as st:
            sb = lambda n, s, d: self.sb(st, n, s, d)
            self.lnw = {"sq": sb("lnsq", [128, NCH, TT], BF16), "rb": sb("lnrb", [128, NCH, TT], BF16),
                        "mean": sb("lnmean", [128, TT], F32), "rstd": sb("lnrstd", [128, TT], F32), "tmp": sb("lntmp", [128, TT], F32), "tmp2": sb("lntmp2", [128, TT], F32)}
            xb = sb("xb", [128, NCH, ST], BF16)
            acc = sb("acc", [128, NCH, ST], F32)
            w13 = [sb("w13_%d" % i, [128, NCH, 2, GC * 128], BF16) for i in range(2)]
            w2 = [sb("w2_%d" % i, [128, GC, D], BF16) for i in range(2)]
            hT = sb("hT", [128, GC, TT], BF16)
            sg = sb("sg", [128, TT], F32)
            xf = None if moe else sb("xf", [128, NCH, TT], F32)
            if moe:
                xsc = sb("xsc", [128, NCH, ST], BF16)
                rw = sb("rw", [128, NCH, NEXP], F32)
                xf32 = sb("xf32", [128, NCH, ST], F32)
                lg = sb("lg", [128, ST // 128, NEXP], F32)
                top = sb("top", [128, 8], F32)
                nm1 = sb("nm1", [128, 1], F32)
                den = sb("den", [128, 1], F32)
                gate = sb("gate", [128, ST // 128, NEXP], F32)
                gbc = sb("gbc", [128, TT], F32)
                em.dma("sp", rw[:], I["router_w"][li].rearrange("(c p) n -> p c n", p=128), writes=["rw"])
            nff = (D_FFE if moe else D_FF) // 128
            groups = [(g0, min(GC, nff - g0)) for g0 in range(0, nff, GC)]
            FF = D_FFE if moe else D_FF
            gi = 0
            for s0 in range(0, T, ST):
                tiles = list(range(s0 // TT, (s0 + ST) // TT))
                em.dma("pool", xb[:], self.XA[:, s0:s0 + ST].rearrange("(c p) t -> p c t", p=128), reads=[("XA", t) for t in tiles], writes=["xb"])
                if moe:
                    em.dma("sp", xf32[:], self.XA[:, s0:s0 + ST].rearrange("(c p) t -> p c t", p=128), reads=[("XA", t) for t in tiles], writes=["xf32"])
                    for j in range(ST // 128):
                        b = self.bank()
                        self.mm(self.pb[b][:, :NEXP], ("pb", b), [(xf32[:, k, j * 128:(j + 1) * 128], rw[:, k, :], ["xf32", "rw"]) for k in range(NCH)])
                        em.op("dve", lambda e, j=j, b=b: e.tensor_copy(out=lg[:, j, :], in_=self.pb[b][:, :NEXP]), reads=[("pb", b)], writes=["lg"])
                        em.op("dve", lambda e, j=j: e.max(out=top[:], in_=lg[:, j, :]), reads=["lg"], writes=["top"])
                        em.op("dve", lambda e: e.tensor_single_scalar(out=nm1[:], in_=top[:, 0:1], scalar=-1.0, op=ALU.mult), reads=["top"], writes=["nm1"])
                        em.op("act", lambda e, j=j: e.activation(out=gate[:, j, :], in_=lg[:, j, :], func=AF.Exp, bias=nm1[:], scale=1.0), reads=["lg", "nm1"], writes=["gate"])
                        em.op("dve", lambda e, j=j: e.scalar_tensor_tensor(out=gate[:, j, :], in0=lg[:, j, :], scalar=top[:, 1:2], in1=gate[:, j, :], op0=ALU.is_ge, op1=ALU.mult),
                              reads=["lg", "top", "gate"], writes=["gate"])
                        em.op("dve", lambda e, j=j: e.tensor_reduce(out=den[:], in_=gate[:, j, :], axis=AX.X, op=ALU.add), reads=["gate"], writes=["den"])
                        em.op("dve", lambda e: e.reciprocal(out=den[:], in_=den[:]), reads=["den"], writes=["den"])
                        em.op("dve", lambda e, j=j: e.tensor_single_scalar(out=gate[:, j, :], in_=gate[:, j, :], scalar=den[:, 0:1], op=ALU.mult), reads=["gate", "den"], writes=["gate"])
                for ex in range(NEXP if moe else 1):
                    if moe:
                        w13src, w2src = I["exp_w13"][li, ex], I["exp_w2"][li, ex]
                        for su in range(nsub):
                            b = self.bank()
                            for jj in range(4):
                                j = su * 4 + jj
                                em.op("pe", lambda e, j=j, jj=jj, b=b, ex=ex: e.matmul(self.pb[b][:, jj * 128:(jj + 1) * 128], lhsT=gate[:, j, ex:ex + 1].broadcast_to([128, 128]), rhs=self.ident, start=True, stop=True),
                                      reads=["gate", "ident"], writes=[("pb", b)])
                            em.op("act", lambda e, b=b: e.copy(out=gbc[:], in_=self.pb[b][:]), reads=[("pb", b)], writes=["gbc"])
                            for c in range(NCH):
                                em.op("dve", lambda e, c=c, su=su: e.tensor_tensor(out=xsc[:, c, su * TT:(su + 1) * TT], in0=xf32[:, c, su * TT:(su + 1) * TT], in1=gbc[:], op=ALU.mult),
                                      reads=["xf32", "gbc"], writes=[("xsc", su)])
                    else:
                        w13src, w2src = I["ffn_w13"][li], I["ffn_w2"][li]
                    for (g0, gn) in groups:
                        ws = gi % 2
                        gi += 1
                        for hf in range(2):
                            em.dma("pool", w13[ws][:, :, hf, :gn * 128], w13src[:, hf * FF + g0 * 128:hf * FF + (g0 + gn) * 128].rearrange("(c p) n -> p c n", p=128), writes=[("w13", ws)])
                        em.dma("pool", w2[ws][:, :gn, :], w2src[g0 * 128:(g0 + gn) * 128, :].rearrange("(c p) n -> p c n", p=128), writes=[("w2", ws)])
                        for su in range(nsub):
                            ts = slice(su * TT, (su + 1) * TT)
                            for j in range(gn):
                                bg, bu = self.bank(), self.bank()
                                self.mm(self.pb[bg][:], ("pb", bg), [(w13[ws][:, k, 0, j * 128:(j + 1) * 128], xb[:, k, ts], [("w13", ws), "xb"]) for k in range(NCH)])
                                usrc = xsc if moe else xb
                                ukey = ("xsc", su) if moe else "xb"
                                self.mm(self.pb[bu][:], ("pb", bu), [(w13[ws][:, k, 1, j * 128:(j + 1) * 128], usrc[:, k, ts], [("w13", ws), ukey]) for k in range(NCH)])
                                em.op("act", lambda e, bg=bg: e.activation(out=sg[:], in_=self.pb[bg][:], func=AF.Silu), reads=[("pb", bg)], writes=["sg"])
                                em.op("dve", lambda e, j=j, bu=bu: e.tensor_tensor(out=hT[:, j, :], in0=self.pb[bu][:], in1=sg[:], op=ALU.mult), reads=[("pb", bu), "sg"], writes=[("hT", j)])
                            first = (ex == 0 and g0 == 0)
                            for oc in range(NCH):
                                b = self.bank()
                                self.mm(self.pb[b][:], ("pb", b), [(w2[ws][:, j, oc * 128:(oc + 1) * 128], hT[:, j, :], [("w2", ws), ("hT", j)]) for j in range(gn)])
                                if first:
                                    em.op("act", lambda e, oc=oc, b=b, ts=ts: e.copy(out=acc[:, oc, ts], in_=self.pb[b][:]), reads=[("pb", b)], writes=[("acc", su, oc)])
                                else:
                                    em.op("dve", lambda e, oc=oc, b=b, ts=ts: e.tensor_tensor(out=acc[:, oc, ts], in0=acc[:, oc, ts], in1=self.pb[b][:], op=ALU.add),
                                          reads=[("pb", b), ("acc", su, oc)], writes=[("acc", su, oc)])
                P = PCOL
                for su in range(nsub):
                    t = s0 // TT + su
                    c0 = t * TT
                    ts = slice(su * TT, (su + 1) * TT)
                    if moe:
                        xv, xk = xf32[:, :, ts], "xf32"
                    else:
                        em.dma("sp", xf[:], self.XA[:, c0:c0 + TT].rearrange("(c p) t -> p c t", p=128), reads=[("XA", t)], writes=["xf32"])
                        xv, xk = xf[:], "xf32"
                    rv = acc[:, :, ts]
                    for oc in range(NCH):
                        em.op("dve", lambda e, oc=oc, xv=xv, rv=rv: e.scalar_tensor_tensor(out=rv[:, oc, :], in0=xv[:, oc, :], scalar=ALPHA, in1=rv[:, oc, :], op0=ALU.mult, op1=ALU.add),
                              reads=[xk, ("acc", su, oc)], writes=[("acc", su, oc), ("rr", oc)])
                    self.ln_fm(rv, "rr", NCH, P["ln3g"], P["ln3b"], TT, [(xv, "x3")])
                    em.dma("sp", xdst[:, c0:c0 + TT].rearrange("(c p) t -> p c t", p=128), xv, reads=[("x3", c) for c in range(NCH)] + [xk], writes=[(dkey, t), xk])


def _chunkcols(v, nch):
    return np.ascontiguousarray(np.asarray(v, np.float32).reshape(nch, 128).T)


def pack_params(inp):
    P = np.zeros((DEPTH, 128, NP), np.float32)
    for l in range(DEPTH):
        for n, src in (("ln1g", "ln1_g"), ("ln1b", "ln1_b"), ("ln2g", "ln2_g"), ("ln2b", "ln2_b"), ("ln3g", "ln3_g"), ("ln3b", "ln3_b")):
            P[l, :, PCOL[n]:PCOL[n] + 8] = _chunkcols(inp[src][l], 8)
        P[l, :, PCOL["pool_scale"]:PCOL["pool_scale"] + 2] = _chunkcols(inp["pool_scale"][l], 2)
        cw = np.asarray(inp["conv_w"][l], np.float32)
        for c in range(2):
            P[l, :, PCOL["conv_w"] + c * 31:PCOL["conv_w"] + (c + 1) * 31] = cw[:, c * 128:(c + 1) * 128].T
        P[l, :, PCOL["conv_b"]:PCOL["conv_b"] + 2] = _chunkcols(inp["conv_b"][l], 2)
        P[l, :, PCOL["conv_lng"]:PCOL["conv_lng"] + 2] = _chunkcols(inp["conv_ln_g"][l], 2)
        P[l, :, PCOL["conv_lnb"]:PCOL["conv_lnb"] + 2] = _chunkcols(inp["conv_ln_b"][l], 2)
        P[l, 0:64, PCOL["peT"]:PCOL["peT"] + 32] = np.asarray(inp["cmp_pe_k"][l], np.float32).T
        P[l, 0:64, PCOL["peT"] + 32:PCOL["peT"] + 64] = np.asarray(inp["cmp_pe_v"][l], np.float32).T
    return P


def const_f32():
    c = np.zeros((128, CF32_W), np.float32)
    c[:, 0:128] = np.eye(128, dtype=np.float32)
    c[:, 128:256] = np.triu(np.ones((128, 128), np.float32))
    wins = (2, 4, 8, 16)
    for p in range(128):
        for pr in range(2):
            w = wins[2 * pr + (1 if p >= 64 else 0)]
            c[p, CF["invw"] + pr] = 1.0 / w
            for t in range(16):
                c[p, CF["invc"] + pr * 16 + t] = 1.0 / min(t + 1, w)
        cq = 1 if p >= 64 else 0
        for m in range(256):
            rel = m - 126
            c[p, CF["VW"] + m] = 1.0 if rel <= cq else 0.0
            c[p, CF["CW"] + m] = 1.0 + (1e4 if (rel == cq or rel == cq - 1) else 0.0)
    return c


def const_cb(T):
    NKT = T // 128
    cb = np.zeros((128, (23 + NKT) * 128), np.float32)
    n = np.arange(128)[:, None]
    q = np.arange(128)[None, :]
    for oi in range(17):
        off = 8 * oi
        cb[:, oi * 128:(oi + 1) * 128] = np.where(16 * (n - off) + 31 <= q, 0.0, NEGM)
    for kt in range(4):
        nn = 128 * kt + n
        j = q
        cb[:, (17 + kt) * 128:(18 + kt) * 128] = ((nn >= 4 * j - 1) & (nn <= 4 * j + 3)).astype(np.float32)
    cb[:, 21 * 128:22 * 128] = np.where(n > q, NEGM, 0.0)
    cb[:, 22 * 128:23 * 128] = np.where(n <= q, NEGM, 0.0)
    for kt in range(NKT):
        cb[:, (23 + kt) * 128:(24 + kt) * 128] = (n == 2 * kt + q // 64).astype(np.float32)
    return cb


def core_inputs(inp, b):
    m = {}
    m["xT"] = np.ascontiguousarray(np.asarray(inp["x"][b], np.float32).T)
    m["memT"] = np.ascontiguousarray(np.asarray(inp["mem"][b], np.float32).T)
    for k in ("w_in", "w_out", "xq_w", "xkv_w", "xo_w", "ffn_w13", "ffn_w2", "router_w", "exp_w13", "exp_w2",
              "cmp_k_w1", "cmp_k_w2", "cmp_v_w1", "cmp_v_w2", "conv_pw", "pool_w", "sgu_b", "sgu_ln_g", "sgu_ln_b"):
        m[k] = np.ascontiguousarray(np.asarray(inp[k], np.float32))
    m["sgu_wT"] = np.ascontiguousarray(np.asarray(inp["sgu_w"], np.float32).transpose(0, 1, 3, 2))
    return m


_CACHE = {}


def kernel(**inp):
    T = 8192
    if "nc" not in _CACHE:
        _CACHE["nc"] = Builder(T, DEPTH).build()
    nc = _CACHE["nc"]
    params = pack_params(inp)
    cf = const_f32()
    cb = const_cb(T)
    in_maps = []
    for c in range(4):
        m = core_inputs(inp, c)
        m["params"] = params
        m["cf32"] = cf
        m["cb"] = cb
        in_maps.append(m)
    res = run_bass_kernel_spmd(nc, in_maps, core_ids=[0, 1, 2, 3])
    out = np.stack([np.ascontiguousarray(np.asarray(res.results[c]["outT"]).T) for c in range(4)])
    return out.astype(np.float32)
```
